# Optimizing a Trainium2 kernel written in Bass

```python
import jax, jax.numpy as jnp
from jax import lax
import numpy as np

D_MODEL = 1024
BATCH = 8
SEQ = 2048
DEPTH = 1

MEM_LEN = 256
CONV_GROUPS = 8
CONV_GROUP_DIM = 64
CONV_DIM = CONV_GROUPS * CONV_GROUP_DIM
SB_HEADS = 8
SB_HEAD_DIM = 64
SB_DIM = SB_HEADS * SB_HEAD_DIM
MIX_DIM = CONV_DIM + SB_DIM
IN_PROJ_DIM = 3 * CONV_DIM + 3 * SB_DIM
CONV_K = 3
Q_BLOCK = 128
X_HEADS = 4
X_HEAD_DIM = D_MODEL // X_HEADS
N_EXPERTS = 32
TOP_K = 4
D_EXPERT = D_MODEL
SWIGLU_LIMIT = 7.0
SWIGLU_ALPHA = 1.702
EXPERT_BLOCK = 128
EPS = 1e-5

kernel_name = "hymba_conv_stickbreak_memxattn_moe"


def rmsnorm(x, g):
    x32 = x.astype(jnp.float32)
    y = x32 * lax.rsqrt(jnp.mean(x32 * x32, axis=-1, keepdims=True) + EPS)
    return (y * g.astype(jnp.float32)).astype(x.dtype)


def group_rmsnorm(y, g, n_groups):
    b, s, w = y.shape
    y32 = y.astype(jnp.float32).reshape(b, s, n_groups, w // n_groups)
    y32 = y32 * lax.rsqrt(jnp.mean(y32 * y32, axis=-1, keepdims=True) + EPS)
    return (y32.reshape(b, s, w) * g.astype(jnp.float32)).astype(y.dtype)


def short_gated_conv(b_gate, c_gate, xv, conv_w):
    u = c_gate * xv
    s = u.shape[1]
    up = jnp.pad(u, ((0, 0), (CONV_K - 1, 0), (0, 0)))
    y = conv_w[0] * up[:, 0:s]
    for i in range(1, CONV_K):
        y = y + conv_w[i] * up[:, i:i + s]
    return b_gate * y


def stick_breaking_attention(q, k, v):
    s_len = q.shape[1]
    scale = SB_HEAD_DIM ** -0.5
    outs = []
    for i in range(s_len // Q_BLOCK):
        q0 = i * Q_BLOCK
        kend = q0 + Q_BLOCK
        qb = q[:, q0:kend]
        kb = k[:, :kend]
        vb = v[:, :kend]
        z = jnp.einsum('bqhd,bkhd->bhqk', qb, kb).astype(jnp.float32) * scale
        t_idx = q0 + jnp.arange(Q_BLOCK)[:, None]
        s_idx = jnp.arange(kend)[None, :]
        valid = s_idx < t_idx
        log_keep = jnp.where(valid, jax.nn.log_sigmoid(-z), 0.0)
        rest = lax.cumsum(log_keep, axis=3, reverse=True) - log_keep
        a = jnp.where(valid, jnp.exp(jax.nn.log_sigmoid(z) + rest), 0.0)
        outs.append(jnp.einsum('bhqk,bkhd->bqhd', a.astype(v.dtype), vb))
    return jnp.concatenate(outs, axis=1)


def memory_cross_attention(h, mem_n, w_q, w_kv, w_o):
    b, s, _ = h.shape
    m = mem_n.shape[1]
    q = (h @ w_q).reshape(b, s, X_HEADS, X_HEAD_DIM)
    k, v = jnp.split(mem_n @ w_kv, 2, axis=-1)
    k = k.reshape(b, m, X_HEADS, X_HEAD_DIM)
    v = v.reshape(b, m, X_HEADS, X_HEAD_DIM)
    sc = jnp.einsum('bqhd,bkhd->bhqk', q, k).astype(jnp.float32) * (X_HEAD_DIM ** -0.5)
    p = jax.nn.softmax(sc, axis=-1).astype(v.dtype)
    o = jnp.einsum('bhqk,bkhd->bqhd', p, v).reshape(b, s, D_MODEL)
    return o @ w_o


def moe_clamped_swiglu(h, w_router, b_router, w1, b1, w2, b2):
    bn, s, d = h.shape
    n = bn * s
    hf = h.reshape(n, d)
    logits = hf.astype(jnp.float32) @ w_router.astype(jnp.float32) + b_router.astype(jnp.float32)
    top_v, top_i = lax.top_k(logits, TOP_K)
    gate = jax.nn.softmax(top_v, axis=-1)
    n_assign = n * TOP_K
    flat_e = top_i.reshape(-1)
    flat_w = gate.reshape(-1)
    flat_tok = jnp.repeat(jnp.arange(n, dtype=jnp.int32), TOP_K)
    order = jnp.argsort(flat_e)
    sorted_e = flat_e[order]
    sorted_tok = flat_tok[order]
    sorted_w = flat_w[order]
    counts = jnp.bincount(flat_e, length=N_EXPERTS)
    padded = (counts + EXPERT_BLOCK - 1) // EXPERT_BLOCK * EXPERT_BLOCK
    start_sorted = jnp.cumsum(counts) - counts
    padded_end = jnp.cumsum(padded)
    start_padded = padded_end - padded
    rows = jnp.arange(n_assign, dtype=jnp.int32)
    dest = start_padded[sorted_e] + rows - start_sorted[sorted_e]
    n_rows = ((n_assign + N_EXPERTS * EXPERT_BLOCK + EXPERT_BLOCK - 1) // EXPERT_BLOCK) * EXPERT_BLOCK
    n_blocks = n_rows // EXPERT_BLOCK
    pad_tok = jnp.zeros((n_rows,), jnp.int32).at[dest].set(sorted_tok)
    pad_w = jnp.zeros((n_rows,), jnp.float32).at[dest].set(sorted_w)
    block_start = jnp.arange(n_blocks, dtype=jnp.int32) * EXPERT_BLOCK
    block_e = jnp.minimum(jnp.searchsorted(padded_end, block_start, side='right'), N_EXPERTS - 1)

    def expert_block(args):
        e, tok, wt = args
        xb = hf[tok]
        hid = xb @ w1[e] + b1[e]
        glu, lin = jnp.split(hid, 2, axis=-1)
        glu = jnp.minimum(glu, SWIGLU_LIMIT)
        lin = jnp.clip(lin, -SWIGLU_LIMIT, SWIGLU_LIMIT)
        act = glu * jax.nn.sigmoid(SWIGLU_ALPHA * glu) * (lin + 1.0)
        out = act @ w2[e] + b2[e]
        return out * wt[:, None].astype(out.dtype)

    out_blocks = lax.map(expert_block, (block_e,
                                        pad_tok.reshape(n_blocks, EXPERT_BLOCK),
                                        pad_w.reshape(n_blocks, EXPERT_BLOCK)))
    y = jnp.zeros_like(hf).at[pad_tok].add(out_blocks.reshape(n_rows, d))
    return y.reshape(bn, s, d)


def setup_inputs(seed: int = 0) -> dict:
    key = jax.random.key(seed)
    ks = jax.random.split(key, 24)
    f32 = jnp.float32

    def nrm(k, shape, scale):
        return jax.random.normal(k, shape, f32) * scale

    def gain(k, shape):
        return 1.0 + 0.02 * jax.random.normal(k, shape, f32)

    return {
        "x": nrm(ks[0], (BATCH, SEQ, D_MODEL), 1.0),
        "mem": nrm(ks[1], (BATCH, MEM_LEN, D_MODEL), 1.0),
        "g_mix": gain(ks[2], (DEPTH, D_MODEL)),
        "w_in": nrm(ks[3], (DEPTH, D_MODEL, IN_PROJ_DIM), D_MODEL ** -0.5),
        "conv_w": nrm(ks[4], (DEPTH, CONV_K, CONV_DIM), CONV_K ** -0.5),
        "g_conv_out": gain(ks[5], (DEPTH, CONV_DIM)),
        "g_sb_out": gain(ks[6], (DEPTH, SB_DIM)),
        "w_out": nrm(ks[7], (DEPTH, MIX_DIM, D_MODEL), MIX_DIM ** -0.5),
        "g_xattn": gain(ks[8], (DEPTH, D_MODEL)),
        "g_mem": gain(ks[9], (DEPTH, D_MODEL)),
        "w_q_mem": nrm(ks[10], (DEPTH, D_MODEL, D_MODEL), D_MODEL ** -0.5),
        "w_kv_mem": nrm(ks[11], (DEPTH, D_MODEL, 2 * D_MODEL), D_MODEL ** -0.5),
        "w_o_mem": nrm(ks[12], (DEPTH, D_MODEL, D_MODEL), D_MODEL ** -0.5),
        "g_moe": gain(ks[13], (DEPTH, D_MODEL)),
        "w_router": nrm(ks[14], (DEPTH, D_MODEL, N_EXPERTS), D_MODEL ** -0.5),
        "b_router": nrm(ks[15], (DEPTH, N_EXPERTS), 0.01),
        "w1": nrm(ks[16], (DEPTH, N_EXPERTS, D_MODEL, 2 * D_EXPERT), D_MODEL ** -0.5),
        "b1": nrm(ks[17], (DEPTH, N_EXPERTS, 2 * D_EXPERT), 0.02),
        "w2": nrm(ks[18], (DEPTH, N_EXPERTS, D_EXPERT, D_MODEL), D_EXPERT ** -0.5),
        "b2": nrm(ks[19], (DEPTH, N_EXPERTS, D_MODEL), 0.02),
        "g_final": gain(ks[20], (D_MODEL,)),
    }


def reference(x, mem, g_mix, w_in, conv_w, g_conv_out, g_sb_out, w_out, g_xattn, g_mem,
              w_q_mem, w_kv_mem, w_o_mem, g_moe, w_router, b_router, w1, b1, w2, b2, g_final):
    b, s, _ = x.shape
    splits = [CONV_DIM, 2 * CONV_DIM, 3 * CONV_DIM,
              3 * CONV_DIM + SB_DIM, 3 * CONV_DIM + 2 * SB_DIM]
    for l in range(DEPTH):
        h = rmsnorm(x, g_mix[l])
        proj = h @ w_in[l]
        b_gate, c_gate, xv, q, k, v = jnp.split(proj, splits, axis=-1)
        y_conv = short_gated_conv(b_gate, c_gate, xv, conv_w[l])
        y_sb = stick_breaking_attention(q.reshape(b, s, SB_HEADS, SB_HEAD_DIM),
                                        k.reshape(b, s, SB_HEADS, SB_HEAD_DIM),
                                        v.reshape(b, s, SB_HEADS, SB_HEAD_DIM)).reshape(b, s, SB_DIM)
        y_mix = jnp.concatenate([group_rmsnorm(y_conv, g_conv_out[l], CONV_GROUPS),
                                 group_rmsnorm(y_sb, g_sb_out[l], SB_HEADS)], axis=-1)
        x = x + y_mix @ w_out[l]
        x = x + memory_cross_attention(rmsnorm(x, g_xattn[l]), rmsnorm(mem, g_mem[l]),
                                       w_q_mem[l], w_kv_mem[l], w_o_mem[l])
        x = x + moe_clamped_swiglu(rmsnorm(x, g_moe[l]), w_router[l], b_router[l],
                                   w1[l], b1[l], w2[l], b2[l])
    return rmsnorm(x, g_final)
```

```python
import contextlib
import numpy as np
import concourse.bass as bass
import concourse.mybir as mybir
from concourse.bass_utils import run_bass_kernel_spmd

F32 = mybir.dt.float32
BF16 = mybir.dt.bfloat16
I32 = mybir.dt.int32
U32 = mybir.dt.uint32
U8 = mybir.dt.uint8
AF = mybir.ActivationFunctionType
ALU = mybir.AluOpType

NCORES = 8
S = 2048
D = 1024
NT = S // 128
MEM = 256
NE = 32
C1_LEVEL = 9
ATT_SPLIT = 0
C2_LEVEL = 9
C2_SUB = 9
NE_RUN = 32
SCAT_TEST = False
CAP = 352
EPS = 1e-5

ENGMAP = {"pe": "tensor", "act": "scalar", "dve": "vector", "pool": "gpsimd", "sp": "sync"}


class Sched:
    def __init__(self, nc, es):
        self.nc = nc
        self.es = es
        self.ops = []
        self.last_w = {}
        self.readers = {}
        self.eng_sem = {e: es.enter_context(nc.semaphore("sem_" + e)) for e in ENGMAP}
        self.dma_sem = {}
        self.dma_last = {}

    def op(self, eng, fn, r=(), w=(), dma=None):
        idx = len(self.ops)
        deps = set()
        for k in r:
            p = self.last_w.get(k)
            if p is not None:
                deps.add(p)
        for k in w:
            p = self.last_w.get(k)
            if p is not None:
                deps.add(p)
            deps |= self.readers.get(k, set())
        deps.discard(idx)
        for k in r:
            self.readers.setdefault(k, set()).add(idx)
        for k in w:
            self.last_w[k] = idx
            self.readers[k] = set()
        if dma is not None:
            name, n = dma
            if name not in self.dma_sem:
                self.dma_sem[name] = [self.es.enter_context(self.nc.semaphore("dq_" + name)), 0]
        self.ops.append(dict(eng=eng, fn=fn, deps=deps, dma=dma, token=None, needed=False))
        return idx

    def emit(self):
        ops = self.ops
        for op in ops:
            for d in op["deps"]:
                P = ops[d]
                if P["eng"] == "pe" and op["eng"] == "pe" and P["dma"] is None and op["dma"] is None:
                    continue
                P["needed"] = True
        cnt = {e: 0 for e in ENGMAP}
        for op in ops:
            if op["dma"] is not None:
                name, n = op["dma"]
                ent = self.dma_sem[name]
                ent[1] += 16 * n
                op["token"] = (ent[0], ent[1])
            elif op["needed"]:
                cnt[op["eng"]] += 1
                op["token"] = (self.eng_sem[op["eng"]], cnt[op["eng"]])
        waited = {e: {} for e in ENGMAP}
        nwaits = 0
        for op in ops:
            E = getattr(self.nc, ENGMAP[op["eng"]])
            need = {}
            for d in op["deps"]:
                P = ops[d]
                if P["token"] is None:
                    continue
                if P["eng"] == "pe" and op["eng"] == "pe" and P["dma"] is None and op["dma"] is None:
                    continue
                sem, c = P["token"]
                key = id(sem)
                if key not in need or need[key][1] < c:
                    need[key] = (sem, c)
            for key, (sem, c) in need.items():
                if waited[op["eng"]].get(key, 0) >= c:
                    continue
                E.wait_ge(sem, c)
                nwaits += 1
                waited[op["eng"]][key] = c
            res = op["fn"](E)
            if op["dma"] is not None:
                sem = op["token"][0]
                assert len(res) == op["dma"][1], (len(res), op["dma"])
                for ins in res:
                    ins.then_inc(sem, 16)
            elif op["token"] is not None:
                last = res[-1] if isinstance(res, (list, tuple)) else res
                last.then_inc(op["token"][0], 1)
        return nwaits


def build_program(stop_after=None, use=None):
    nc = bass.Bass("TRN2", target_bir_lowering=False)
    es = contextlib.ExitStack()
    with es:
        _build(nc, es, stop_after, use)
    return nc


def _build(nc, es, stop_after, use):
    def din(name, shape):
        if use is not None and name not in use:
            return None
        return nc.dram_tensor(name, list(shape), F32, kind="ExternalInput").ap()

    x = din("x", [S, D])
    mem = din("mem", [MEM, D])
    g_mix = din("g_mix", [1, D])
    w_in = din("w_in", [D, 3072])
    conv_w = din("conv_w", [3, 512])
    g_conv_out = din("g_conv_out", [1, 512])
    g_sb_out = din("g_sb_out", [1, 512])
    w_out = din("w_out", [D, D])
    g_xattn = din("g_xattn", [1, D])
    g_mem = din("g_mem", [1, D])
    w_q_mem = din("w_q_mem", [D, D])
    w_kv_mem = din("w_kv_mem", [D, 2 * D])
    w_o_mem = din("w_o_mem", [D, D])
    g_moe = din("g_moe", [1, D])
    w_router = din("w_router", [D, NE])
    b_router = din("b_router", [1, NE])
    w1 = din("w1", [max(NE_RUN, 1), D, 2 * D])
    b1 = din("b1", [NE, 2 * D])
    w2 = din("w2", [max(NE_RUN, 1), D, D])
    b2 = din("b2", [NE, D])
    g_final = din("g_final", [1, D])
    out = nc.dram_tensor("out", [S, D], F32, kind="ExternalOutput").ap()

    sc = Sched(nc, es)

    ARENA = 206 * 1024
    arena = es.enter_context(nc.sbuf_tensor("arena", [128, ARENA], U8))
    psum = es.enter_context(nc.psum_tensor("psum", [128, 8, 512], F32))

    def carve(off, nbytes, dt, pat=None, **kw):
        v = arena[:, off:off + nbytes].bitcast(dt)
        if pat is not None:
            v = v.rearrange(pat, **kw)
        return v

    def bank(b):
        return psum[:, b, :]

    K = 1024
    o = 0
    identF = carve(o, 512, F32); o += 512
    onesB = carve(o, 256, BF16); o += 256
    nonesB = carve(o, 256, BF16); o += 256
    ntriB = carve(o, 256, BF16); o += 256
    bdiagB = carve(o, 256, BF16); o += 256
    maskB = carve(o, 4096, BF16, "p (r f) -> p r f", r=4); o += 4096
    iotaI = carve(o, 2048, I32); o += 2048
    gtab = carve(o, 208, F32)
    gmixC = carve(o, 32, F32); o += 32
    gxatC = carve(o, 32, F32); o += 32
    gmemC = carve(o, 32, F32); o += 32
    gmoeC = carve(o, 32, F32); o += 32
    gconvC = carve(o, 16, F32); o += 16
    gsbC = carve(o, 16, F32); o += 16
    convC = carve(o, 48, F32, "p (k m) -> p k m", k=3); o += 48
    ssA = carve(o, 64, F32); o += 64
    rsA = carve(o, 64, F32); o += 64
    ssM = carve(o, 8, F32); o += 8
    rsM = carve(o, 8, F32); o += 8
    identB = carve(o, 256, BF16); o += 256
    triS = carve(o, 256, BF16); o += 256
    iotaE = carve(o, 128, F32); o += 128
    iotaEi = carve(o, 128, I32); o += 128
    brB = carve(o, 128, F32); o += 128
    wr = carve(o, 1024, F32, "p (k e) -> p k e", k=8); o += 1024
    gatesA = carve(o, 256, F32, "p (i k) -> p i k", i=NT); o += 256
    idxA = carve(o, 256, I32, "p (i k) -> p i k", i=NT); o += 256
    ssC = carve(o, 64, F32); o += 64
    rsC = carve(o, 64, F32); o += 64
    gst = carve(o, 512, F32); o += 512
    b1T = iotaI.bitcast(F32).rearrange("p (j e) -> p j e", j=16)
    b2S = maskB.rearrange("p r f -> p (r f)").bitcast(F32)
    assert o <= 12 * K
    XOFF = 12 * K
    X = carve(XOFF, 64 * K, F32, "p (i d) -> p i d", i=NT)
    hT = carve(XOFF, 32 * K, BF16, "p (k t) -> p k t", k=8)
    wsl = [carve(XOFF + 32 * K + j * 8 * K, 8 * K, BF16, "p (k f) -> p k f", k=8) for j in range(3)]
    xs = [carve(XOFF + 56 * K + j * 4 * K, 4 * K, F32) for j in range(2)]
    o = XOFF + 64 * K
    qT = carve(o, 16 * K, BF16, "p (m t) -> p m t", m=4); o += 16 * K
    kT = carve(o, 16 * K, BF16, "p (m t) -> p m t", m=4); o += 16 * K
    vtok = carve(o, 16 * K, BF16, "p (i f) -> p i f", i=NT); o += 16 * K
    ymix = carve(o, 32 * K, BF16, "p (k t) -> p k t", k=8); o += 32 * K
    TOFF = o
    assert TOFF == 156 * K
    xn = [carve(TOFF + j * 4 * K, 4 * K, F32) for j in range(2)]
    junk = carve(TOFF + 8 * K, 2 * K, BF16)
    uT = carve(TOFF + 10 * K, 4 * (S + 2) * 2, BF16, "p (m t) -> p m t", m=4)
    o = TOFF + 10 * K + 4 * (S + 2) * 2
    o = (o + 63) // 64 * 64
    cset = []
    for j in range(2):
        d = {}
        for nm, nb, dt in (("tc", 2 * K, F32), ("t0", 2 * K, F32), ("t1", 2 * K, F32), ("yc", 2 * K, F32),
                           ("sq", 1 * K, BF16), ("rs", 2 * K, F32)):
            d[nm] = carve(o, nb, dt); o += nb
        cset.append(d)
    assert o <= ARENA, o
    o = TOFF
    AT = {}
    AT["e"] = [carve(o + i * 2 * K, 2 * K, BF16, "p (h t) -> p h t", h=2) for i in range(3)]; o += 6 * K
    AT["sp"] = [carve(o + i * 2 * K, 2 * K, BF16, "p (h t) -> p h t", h=2) for i in range(2)]; o += 4 * K
    AT["sacc"] = [carve(o + i * 2 * K, 2 * K, BF16, "p (h t) -> p h t", h=2) for i in range(2)]; o += 4 * K
    AT["p"] = [carve(o + i * 2 * K, 2 * K, BF16, "p (h t) -> p h t", h=2) for i in range(2)]; o += 4 * K
    gn = {}
    gn["yraw"] = carve(o, 2 * K, F32); o += 2 * K
    gn["sq"] = carve(o, K, BF16); o += K
    gn["rs"] = carve(o, 2 * K, F32); o += 2 * K
    o = TOFF + 27 * K
    wo_sl = carve(o, 16 * K, BF16, "p (k f) -> p k f", k=8); o += 16 * K
    assert o <= ARENA

    def c_iota(E):
        return E.iota(iotaI, pattern=[[1, 512]], base=0, channel_multiplier=-1)
    sc.op("pool", c_iota, w=["iota"])

    def c_consts(E):
        r = []
        r.append(E.tensor_scalar(out=identF, in0=iotaI[:, 0:128], scalar1=0.0, scalar2=None, op0=ALU.is_equal))
        r.append(E.memset(onesB, 1.0))
        r.append(E.memset(nonesB, -1.0))
        r.append(E.tensor_scalar(out=ntriB, in0=iotaI[:, 0:128], scalar1=0.0, scalar2=-1.0,
                                 op0=ALU.is_le, op1=ALU.mult))
        r.append(E.memset(bdiagB[0:64, 0:64], 1.0))
        r.append(E.memset(bdiagB[0:64, 64:128], 0.0))
        r.append(E.memset(bdiagB[64:128, 0:64], 0.0))
        r.append(E.memset(bdiagB[64:128, 64:128], 1.0))
        for rr in range(4):
            r.append(E.tensor_scalar(out=maskB[:, rr, :], in0=iotaI, scalar1=float(128 * rr), scalar2=None,
                                     op0=ALU.is_gt))
        r.append(E.memset(uT[:, :, 0:2], 0.0))
        r.append(E.tensor_scalar(out=triS, in0=iotaI[:, 0:128], scalar1=0.0, scalar2=None, op0=ALU.is_gt))
        r.append(E.tensor_copy(out=iotaE, in_=iotaEi))
        return r
    sc.op("pool", lambda E: E.iota(iotaEi, pattern=[[1, 32]], base=0, channel_multiplier=0), w=["iotaE"])
    sc.op("dve", c_consts, r=["iota", "iotaE"], w=["consts1", "uhalo", "maskc"])
    sc.op("dve", lambda E: E.tensor_copy(out=identB, in_=identF), r=["consts1"], w=["consts"])

    def c_gains(E):
        r = []
        srcs = [g_mix.rearrange("o (c p) -> (o c) p", p=128), g_xattn.rearrange("o (c p) -> (o c) p", p=128),
                g_mem.rearrange("o (c p) -> (o c) p", p=128), g_moe.rearrange("o (c p) -> (o c) p", p=128),
                g_conv_out.rearrange("o (c p) -> (o c) p", p=128), g_sb_out.rearrange("o (c p) -> (o c) p", p=128),
                conv_w.rearrange("k (m p) -> (k m) p", p=128)]
        row = 0
        for src in srcs:
            n = src.shape[0]
            r.append(E.dma_start(out=gst[row:row + n, :], in_=src))
            row += n
        return r
    sc.op("sp", c_gains, w=["gst"], dma=("gains", 7))
    sc.op("pe", lambda E: E.transpose(out=bank(7)[:, 0:52], in_=gst[0:52, :], identity=identF[0:52, 0:52]),
          r=["gst", "consts"], w=["ps7"])
    sc.op("dve", lambda E: E.tensor_copy(out=gtab, in_=bank(7)[:, 0:52]), r=["ps7"], w=["gains"])

    def a1_p1(i):
        s = i % 2

        def ld(E, i=i, s=s):
            return [E.dma_start(out=xs[s], in_=x[i * 128:(i + 1) * 128, :])]
        sc.op("sp", ld, w=[f"xs{s}"], dma=(f"xs{s}", 1))

        def sq(E, i=i, s=s):
            return E.activation(out=junk, in_=xs[s], func=AF.Square, accum_out=ssA[:, i:i + 1])
        sc.op("act", sq, r=[f"xs{s}"], w=[f"ss{i}"])

        def ln(E, i=i):
            return E.activation(out=rsA[:, i:i + 1], in_=ssA[:, i:i + 1], func=AF.Ln, scale=1.0 / D, bias=EPS)
        sc.op("act", ln, r=[f"ss{i}"], w=[f"rs{i}"])

        def ex(E, i=i):
            return E.activation(out=rsA[:, i:i + 1], in_=rsA[:, i:i + 1], func=AF.Exp, scale=-0.5)
        sc.op("act", ex, r=[f"rs{i}"], w=[f"rs{i}"])

        def nrm(E, i=i, s=s):
            return E.tensor_scalar(out=xn[s], in0=xs[s], scalar1=rsA[:, i:i + 1], scalar2=None, op0=ALU.mult)
        sc.op("dve", nrm, r=[f"xs{s}", f"rs{i}"], w=[f"xn{s}"])

    def a1_p2(i):
        s = i % 2
        b0 = 2 * s

        def tr(E, s=s, b0=b0):
            r = []
            for kc in range(8):
                r.append(E.transpose(out=bank(b0 + kc // 4)[:, (kc % 4) * 128:(kc % 4 + 1) * 128],
                                     in_=xn[s][:, kc * 128:(kc + 1) * 128], identity=identF))
            return r
        sc.op("pe", tr, r=[f"xn{s}", "consts"], w=[f"ps{b0}", f"ps{b0 + 1}"])

        def ev0(E, i=i, b0=b0):
            r = []
            for kc in range(4):
                r.append(E.tensor_scalar(out=hT[:, kc, i * 128:(i + 1) * 128],
                                         in0=bank(b0)[:, kc * 128:(kc + 1) * 128],
                                         scalar1=gmixC[:, kc:kc + 1], scalar2=None, op0=ALU.mult))
            return r
        sc.op("dve", ev0, r=[f"ps{b0}", "gains"], w=[f"hT{i}a"])

        def ev1(E, i=i, b0=b0):
            r = []
            for kc in range(4, 8):
                r.append(E.activation(out=hT[:, kc, i * 128:(i + 1) * 128],
                                      in_=bank(b0 + 1)[:, (kc - 4) * 128:(kc - 3) * 128],
                                      func=AF.Identity, scale=gmixC[:, kc:kc + 1]))
            return r
        sc.op("act", ev1, r=[f"ps{b0 + 1}", "gains"], w=[f"hT{i}b"])

    a1_p1(0)
    for i in range(NT):
        if i + 1 < NT:
            a1_p1(i + 1)
        a1_p2(i)

    def hkeys(c):
        return [f"hT{i}a" for i in range(4 * c, 4 * c + 4)] + [f"hT{i}b" for i in range(4 * c, 4 * c + 4)]

    def load_w_in(slot, blk):
        def f(E):
            return [E.dma_start(out=wsl[slot],
                                in_=w_in[:, blk * 512:(blk + 1) * 512].rearrange("(k p) f -> p k f", p=128))]
        sc.op("pool", f, w=[f"wsl{slot}"], dma=(f"wsl{slot}", 1))

    load_w_in(0, 3)
    load_w_in(1, 4)
    load_w_in(2, 5)
    pb = [4]

    def nextbank(lo=4, hi=8):
        b = pb[0]
        pb[0] = lo + (pb[0] - lo + 1) % (hi - lo)
        return b

    evtog = [0]

    def evac(out_ap, in_ap, r, w, scale=None):
        evtog[0] ^= 1
        if evtog[0]:
            def f(E):
                if scale is None:
                    return E.activation(out=out_ap, in_=in_ap, func=AF.Copy)
                return E.activation(out=out_ap, in_=in_ap, func=AF.Copy, scale=scale)
            sc.op("act", f, r=r, w=w)
        else:
            def f(E):
                if scale is None:
                    return E.tensor_copy(out=out_ap, in_=in_ap)
                return E.tensor_scalar(out=out_ap, in0=in_ap, scalar1=scale, scalar2=None, op0=ALU.mult)
            sc.op("dve", f, r=r, w=w)

    for slot, dstT, scl, nm in ((0, qT, 0.125, "qT"), (1, kT, None, "kT")):
        for c in range(4):
            for m in range(4):
                b = nextbank()

                def mm(E, slot=slot, c=c, m=m, b=b):
                    r = []
                    for kc in range(8):
                        r.append(E.matmul(out=bank(b), lhsT=wsl[slot][:, kc, m * 128:(m + 1) * 128],
                                          rhs=hT[:, kc, c * 512:(c + 1) * 512], start=(kc == 0), stop=(kc == 7)))
                    return r
                sc.op("pe", mm, r=hkeys(c) + [f"wsl{slot}"], w=[f"ps{b}"])
                evac(dstT[:, m, c * 512:(c + 1) * 512], bank(b), r=[f"ps{b}"], w=[f"{nm}{m}.{c}"], scale=scl)
    for i in range(NT):
        b = nextbank()

        def mm(E, i=i, b=b):
            r = []
            for kc in range(8):
                r.append(E.matmul(out=bank(b), lhsT=hT[:, kc, i * 128:(i + 1) * 128], rhs=wsl[2][:, kc, :],
                                  start=(kc == 0), stop=(kc == 7)))
            return r
        sc.op("pe", mm, r=[f"hT{i}a", f"hT{i}b", "wsl2"], w=[f"ps{b}"])
        evac(vtok[:, i, :], bank(b), r=[f"ps{b}"], w=[f"v{i}"])

    load_w_in(0, 1)
    load_w_in(1, 2)
    load_w_in(2, 0)
    conv_state = {}

    def conv_p1(it, c, m):
        cs = cset[it % 2]
        sfx = f"c{it % 2}"
        bC, bX, bB = nextbank(0, 8), nextbank(0, 8), nextbank(0, 8)
        conv_state[it] = bB
        for slot, b in ((0, bC), (1, bX), (2, bB)):
            def mm(E, slot=slot, c=c, m=m, b=b):
                r = []
                for kc in range(8):
                    r.append(E.matmul(out=bank(b), lhsT=wsl[slot][:, kc, m * 128:(m + 1) * 128],
                                      rhs=hT[:, kc, c * 512:(c + 1) * 512], start=(kc == 0), stop=(kc == 7)))
                return r
            sc.op("pe", mm, r=hkeys(c) + [f"wsl{slot}"], w=[f"ps{b}"])
        sc.op("act", lambda E, cs=cs, bC=bC: E.activation(out=cs["tc"], in_=bank(bC), func=AF.Copy),
              r=[f"ps{bC}"], w=["tc" + sfx])
        t0 = 2 + c * 512
        sc.op("dve", lambda E, cs=cs, bX=bX, m=m, t0=t0: E.tensor_tensor(
            out=uT[:, m, t0:t0 + 512], in0=bank(bX), in1=cs["tc"], op=ALU.mult),
            r=[f"ps{bX}", "tc" + sfx, "uhalo"], w=[f"u{m}.{c}"])

    def conv_p2(it, c, m):
        cs = cset[it % 2]
        sfx = f"c{it % 2}"
        bB = conv_state[it]
        bG = nextbank(0, 8)
        t0 = 2 + c * 512
        ukeys = [f"u{m}.{c}"] + ([f"u{m}.{c - 1}"] if c > 0 else ["uhalo"])
        sc.op("act", lambda E, cs=cs, m=m, t0=t0: E.activation(
            out=cs["t0"], in_=uT[:, m, t0:t0 + 512], func=AF.Identity, scale=convC[:, 2, m:m + 1]),
            r=ukeys + ["gains"], w=["t0" + sfx])
        sc.op("dve", lambda E, cs=cs, m=m, t0=t0: E.scalar_tensor_tensor(
            out=cs["t1"], in0=uT[:, m, t0 - 1:t0 + 511], scalar=convC[:, 1, m:m + 1], in1=cs["t0"],
            op0=ALU.mult, op1=ALU.add), r=ukeys + ["t0" + sfx, "gains"], w=["t1" + sfx])
        sc.op("dve", lambda E, cs=cs, m=m, t0=t0: E.scalar_tensor_tensor(
            out=cs["t0"], in0=uT[:, m, t0 - 2:t0 + 510], scalar=convC[:, 0, m:m + 1], in1=cs["t1"],
            op0=ALU.mult, op1=ALU.add), r=ukeys + ["t1" + sfx, "gains"], w=["t0" + sfx])
        sc.op("dve", lambda E, cs=cs, bB=bB: E.tensor_tensor(
            out=cs["yc"], in0=bank(bB), in1=cs["t0"], op=ALU.mult),
            r=[f"ps{bB}", "t0" + sfx], w=["yc" + sfx])
        sc.op("act", lambda E, cs=cs: E.activation(out=cs["sq"], in_=cs["yc"], func=AF.Square),
              r=["yc" + sfx], w=["sq" + sfx])
        sc.op("pe", lambda E, cs=cs, bG=bG: E.matmul(out=bank(bG), lhsT=bdiagB, rhs=cs["sq"],
                                                   start=True, stop=True),
              r=["sq" + sfx, "consts"], w=[f"ps{bG}"])
        sc.op("act", lambda E, cs=cs, bG=bG: E.activation(out=cs["rs"], in_=bank(bG), func=AF.Ln,
                                                         scale=1.0 / 64, bias=EPS),
              r=[f"ps{bG}"], w=["rs" + sfx])
        sc.op("act", lambda E, cs=cs: E.activation(out=cs["rs"], in_=cs["rs"], func=AF.Exp, scale=-0.5),
              r=["rs" + sfx], w=["rs" + sfx])
        sc.op("dve", lambda E, cs=cs, m=m, c=c: E.scalar_tensor_tensor(
            out=ymix[:, m, c * 512:(c + 1) * 512], in0=cs["yc"], scalar=gconvC[:, m:m + 1], in1=cs["rs"],
            op0=ALU.mult, op1=ALU.mult), r=["yc" + sfx, "rs" + sfx, "gains"], w=[f"ym{m}.{c}"])

    conv_its = [(c, m) for c in range(4) for m in range(4)]
    conv_p1(0, *conv_its[0])
    for it, (c, m) in enumerate(conv_its):
        if it + 1 < len(conv_its):
            conv_p1(it + 1, *conv_its[it + 1])
        conv_p2(it, c, m)

    cset_keys = [nm + f"c{j}" for j in range(2) for nm in ("tc", "t0", "t1", "yc", "sq", "rs")]

    def ld_wo(E):
        return [E.dma_start(out=wo_sl, in_=w_out.rearrange("(k p) f -> p k f", p=128))]
    sc.op("pool", ld_wo, w=["wo_sl"] + cset_keys, dma=("wo_sl", 1))

    conv_keys = ["xn0", "xn1"] + [f"u{m}.{c}" for m in range(4) for c in range(4)] + cset_keys
    BY = 6
    BG2 = 7
    first_att = [True]
    pending_gn = [None]
    for hp in range(4):
        for c in range(4):
            nkb = 4 * c + 4

            def emit_z(step, hp=hp, c=c, nkb=nkb):
                kb = nkb - 1 - step
                c0 = 128 * max(kb - 4 * c, 0)
                for hh in range(2):
                    P0 = 64 * hh
                    bz = 2 * (step % 2) + hh

                    def zmm(E, P0=P0, bz=bz, kb=kb, c0=c0):
                        return E.matmul(out=bank(bz)[:, c0:512], lhsT=kT[P0:P0 + 64, hp, kb * 128:(kb + 1) * 128],
                                        rhs=qT[P0:P0 + 64, hp, c * 512 + c0:(c + 1) * 512], start=True, stop=True)
                    sc.op("pe", zmm, r=[f"kT{hp}.{kb // 4}", f"qT{hp}.{c}"], w=[f"ps{bz}"])

            extra_w = conv_keys if first_att[0] else []
            first_att[0] = False
            sc.op("dve", lambda E: [E.memset(AT["sacc"][0], 0.0), E.memset(AT["sacc"][1], 0.0)],
                  w=["sacc0", "sacc1"] + extra_w)
            def stageA(step, c=c, nkb=nkb, extra_w=extra_w):
                kb = nkb - 1 - step
                r_ = kb - 4 * c
                par = step % 2
                c0 = 128 * max(r_, 0)
                tg = f"p{par}"
                e3 = step % 3
                ek = f"eb{e3}"
                zp = psum[:, 2 * par:2 * par + 2, c0:512]
                sc.op("act", lambda E: E.activation(out=AT["e"][e3][:, :, c0:512], in_=zp, func=AF.Exp),
                      r=[f"ps{2 * par}", f"ps{2 * par + 1}"], w=[ek] + extra_w)
                if r_ >= 0:
                    for hh in range(2):
                        sc.op("dve", lambda E, hh=hh: E.tensor_tensor(
                            out=AT["e"][e3][:, hh, c0:512], in0=AT["e"][e3][:, hh, c0:512], in1=maskB[:, r_, c0:512],
                            op=ALU.mult), r=[ek, "maskc"], w=[ek])
                sc.op("act", lambda E: E.activation(out=AT["sp"][par][:, :, c0:512], in_=AT["e"][e3][:, :, c0:512],
                                                    func=AF.Ln, bias=1.0), r=[ek], w=["sp" + tg])

            def stageB(step, c=c, nkb=nkb):
                kb = nkb - 1 - step
                par = step % 2
                c0 = 128 * max(kb - 4 * c, 0)
                tg = f"p{par}"
                for hh in range(2):
                    br = 4 + hh

                    def rmm(E, hh=hh, br=br):
                        r = [E.matmul(out=bank(br)[:, c0:512], lhsT=ntriB, rhs=AT["sp"][par][:, hh, c0:512], start=True,
                                      stop=(step == 0))]
                        if step > 0:
                            r.append(E.matmul(out=bank(br)[:, c0:512], lhsT=nonesB,
                                              rhs=AT["sacc"][(step - 1) % 2][:, hh, c0:512], start=False, stop=True))
                        return r
                    rk = ["sp" + tg, "consts"] + ([f"sacc{(step - 1) % 2}"] if step > 0 else [])
                    sc.op("pe", rmm, r=rk, w=[f"ps{br}"])
                if step < nkb - 1:
                    sc.op("dve", lambda E: E.tensor_tensor(
                        out=AT["sacc"][step % 2][:, :, c0:512], in0=AT["sacc"][(step - 1) % 2][:, :, c0:512],
                        in1=AT["sp"][par][:, :, c0:512], op=ALU.add),
                        r=["sp" + tg, f"sacc{(step - 1) % 2}"], w=[f"sacc{step % 2}"])

            def stageC1(step, c=c, nkb=nkb):
                kb = nkb - 1 - step
                par = step % 2
                c0 = 128 * max(kb - 4 * c, 0)
                tg = f"p{par}"
                if c0 > 0:
                    sc.op("dve", lambda E: E.memset(AT["p"][par][:, :, 0:c0], 0.0), w=["pl" + tg])
                sc.op("act", lambda E: E.activation(out=AT["p"][par][:, :, c0:512], in_=psum[:, 4:6, c0:512], func=AF.Exp),
                      r=["ps4", "ps5"], w=["p" + tg])
                e3 = step % 3
                sc.op("dve", lambda E: E.tensor_tensor(
                    out=AT["p"][par][:, :, c0:512], in0=AT["p"][par][:, :, c0:512], in1=AT["e"][e3][:, :, c0:512],
                    op=ALU.mult), r=["p" + tg, f"eb{e3}"], w=["p" + tg])

            def stageC2(step, hp=hp, c=c, nkb=nkb):
                kb = nkb - 1 - step
                par = step % 2
                c0 = 128 * max(kb - 4 * c, 0)
                tg = f"p{par}"
                for hh in range(2):
                    P0 = 64 * hh

                    def ymm(E, P0=P0, hh=hh):
                        h = 2 * hp + hh
                        return E.matmul(out=bank(BY)[P0:P0 + 64, :], lhsT=vtok[:, kb, h * 64:(h + 1) * 64],
                                        rhs=AT["p"][par][:, hh, :], start=(step == 0), stop=(step == nkb - 1))
                    sc.op("pe", ymm, r=["p" + tg, "pl" + tg, f"v{kb}"], w=[f"ps{BY}.{hh}"])

            emit_z(0)
            emit_z(1)
            stageA(0)
            stageB(0)
            if pending_gn[0] is not None:
                pending_gn[0]()
                pending_gn[0] = None
            for step in range(nkb):
                if step + 1 < nkb:
                    stageA(step + 1)
                if step + 2 < nkb:
                    emit_z(step + 2)
                stageC1(step)
                if step + 1 < nkb:
                    stageB(step + 1)
                stageC2(step)
            def gn_ops(hp=hp, c=c):
                sc.op("dve", lambda E: E.tensor_copy(out=gn["yraw"], in_=bank(BY)),
                      r=[f"ps{BY}.0", f"ps{BY}.1"], w=["gyraw"])
                sc.op("act", lambda E: E.activation(out=gn["sq"], in_=gn["yraw"], func=AF.Square),
                      r=["gyraw"], w=["gsq"])
                sc.op("pe", lambda E: E.matmul(out=bank(BG2), lhsT=bdiagB, rhs=gn["sq"], start=True, stop=True),
                      r=["gsq", "consts"], w=[f"ps{BG2}"])
                sc.op("act", lambda E: E.activation(out=gn["rs"], in_=bank(BG2), func=AF.Ln, scale=1.0 / 64, bias=EPS),
                      r=[f"ps{BG2}"], w=["grs"])
                sc.op("act", lambda E: E.activation(out=gn["rs"], in_=gn["rs"], func=AF.Exp, scale=-0.5),
                      r=["grs"], w=["grs"])
                sc.op("dve", lambda E, hp=hp, c=c: E.scalar_tensor_tensor(
                    out=ymix[:, 4 + hp, c * 512:(c + 1) * 512], in0=gn["yraw"], scalar=gsbC[:, hp:hp + 1], in1=gn["rs"],
                    op0=ALU.mult, op1=ALU.mult), r=["gyraw", "grs", "gains"], w=[f"ym{4 + hp}.{c}"])
            pending_gn[0] = gn_ops

    pending_gn[0]()

    all_h = [f"hT{i}a" for i in range(NT)] + [f"hT{i}b" for i in range(NT)]
    for i in range(NT):
        over = []
        if i < 8:
            over = all_h
        elif i < 14:
            over = [f"wsl{(i - 8) // 2}"]
        else:
            over = [f"xs{i - 14}"]

        def ldx(E, i=i):
            return [E.dma_start(out=X[:, i, :], in_=x[i * 128:(i + 1) * 128, :])]
        sc.op("sp", ldx, w=[f"X{i}"] + over, dma=(f"X{i}", 1))
    ymk = [f"ym{m}.{c}" for m in range(8) for c in range(4)]
    for i in range(NT):
        for hh in range(2):
            b = nextbank(6, 8)

            def mm(E, i=i, hh=hh, b=b):
                r = []
                for kc in range(8):
                    r.append(E.matmul(out=bank(b), lhsT=ymix[:, kc, i * 128:(i + 1) * 128],
                                      rhs=wo_sl[:, kc, hh * 512:(hh + 1) * 512], start=(kc == 0), stop=(kc == 7)))
                return r
            sc.op("pe", mm, r=[f"ym{m}.{i // 4}" for m in range(8)] + ["wo_sl"], w=[f"ps{b}"])
            sc.op("dve", lambda E, i=i, hh=hh, b=b: E.tensor_tensor(
                out=X[:, i, hh * 512:(hh + 1) * 512], in0=bank(b), in1=X[:, i, hh * 512:(hh + 1) * 512], op=ALU.add),
                r=[f"ps{b}", f"X{i}"], w=[f"X{i}"])

    KQ = [f"qT{m}.{c}" for m in range(4) for c in range(4)]
    KK = [f"kT{m}.{c}" for m in range(4) for c in range(4)]
    KV = [f"v{i}" for i in range(NT)]
    KATT = ["plp0", "plp1", "eb0", "eb1", "eb2", "spp0", "spp1", "pp0", "pp1", "sacc0", "sacc1", "gyraw", "gsq", "grs"]

    def norm_transpose(tag, src, src_keys, nbuf, nbuf_key, nbuf_extra, ss_col, rs_col, gC, dst_of_kc, dkey_a, dkey_b,
                       dst_extra, banks=None, defer=False):
        sc.op("act", lambda E: E.activation(out=junk2, in_=src, func=AF.Square, accum_out=ss_col),
              r=src_keys, w=["ss" + tag])
        sc.op("act", lambda E: E.activation(out=rs_col, in_=ss_col, func=AF.Ln, scale=1.0 / D, bias=EPS),
              r=["ss" + tag], w=["rs" + tag])
        sc.op("act", lambda E: E.activation(out=rs_col, in_=rs_col, func=AF.Exp, scale=-0.5),
              r=["rs" + tag], w=["rs" + tag])
        sc.op("dve", lambda E: E.tensor_scalar(out=nbuf, in0=src, scalar1=rs_col, scalar2=None, op0=ALU.mult),
              r=src_keys + ["rs" + tag], w=[nbuf_key] + nbuf_extra)
        if defer:
            return lambda: _norm_part2(nbuf, nbuf_key, gC, dst_of_kc, dkey_a, dkey_b, dst_extra, banks)
        _norm_part2(nbuf, nbuf_key, gC, dst_of_kc, dkey_a, dkey_b, dst_extra, banks)

    def _norm_part2(nbuf, nbuf_key, gC, dst_of_kc, dkey_a, dkey_b, dst_extra, banks):
        ba, bb = banks if banks is not None else (nextbank(0, 8), nextbank(0, 8))

        def tr(E):
            r = []
            for kc in range(8):
                bk = ba if kc < 4 else bb
                r.append(E.transpose(out=bank(bk)[:, (kc % 4) * 128:(kc % 4 + 1) * 128],
                                     in_=nbuf[:, kc * 128:(kc + 1) * 128], identity=identF))
            return r
        sc.op("pe", tr, r=[nbuf_key, "consts"], w=[f"ps{ba}", f"ps{bb}"])

        def ev0(E):
            return [E.tensor_scalar(out=dst_of_kc(kc), in0=bank(ba)[:, kc * 128:(kc + 1) * 128],
                                    scalar1=gC[:, kc:kc + 1], scalar2=None, op0=ALU.mult) for kc in range(4)]
        sc.op("dve", ev0, r=[f"ps{ba}", "gains"], w=[dkey_a] + dst_extra)

        def ev1(E):
            return [E.activation(out=dst_of_kc(kc), in_=bank(bb)[:, (kc - 4) * 128:(kc - 3) * 128],
                                 func=AF.Identity, scale=gC[:, kc:kc + 1]) for kc in range(4, 8)]
        sc.op("act", ev1, r=[f"ps{bb}", "gains"], w=[dkey_b] + dst_extra)

    if stop_after != "A":
        wkv = carve(76 * K, 32 * K, BF16, "p (k f) -> p k f", k=8)
        wq = carve(76 * K, 16 * K, BF16, "p (k f) -> p k f", k=8)
        wo = carve(92 * K, 16 * K, BF16, "p (k f) -> p k f", k=8)
        memnT = carve(108 * K, 4 * K, BF16, "p (k t) -> p k t", k=8)
        kTx = carve(112 * K, 4 * K, BF16, "p (k t) -> p k t", k=8)
        vx = carve(116 * K, 4 * K, BF16, "p (j f) -> p j f", j=2)
        h2T = carve(124 * K, 32 * K, BF16, "p (k t) -> p k t", k=8)
        xm = [carve(156 * K + j * 4 * K, 4 * K, F32) for j in range(2)]
        qTx = [carve(164 * K + j * 8 * K, 8 * K, BF16, "p (k t) -> p k t", k=8) for j in range(2)]
        oTx = [carve(180 * K + j * 8 * K, 8 * K, BF16, "p (k t) -> p k t", k=8) for j in range(2)]
        ET = [carve(196 * K + j * 2 * K, 2 * K, BF16, "p (m t) -> p m t", m=2) for j in range(2)]
        rden = [carve(200 * K + j * 2 * K, 2 * K, F32) for j in range(2)]
        junk2 = carve(204 * K, 2 * K, BF16)

        sc.op("pool", lambda E: [E.dma_start(out=wkv, in_=w_kv_mem.rearrange("(k p) f -> p k f", p=128))],
              w=["wkv"] + KQ + KK, dma=("wkv", 1))
        for j in range(2):
            sc.op("sp", lambda E, j=j: [E.dma_start(out=xm[j], in_=mem[j * 128:(j + 1) * 128, :])],
                  w=[f"xm{j}"] + KATT + ["wo_sl"], dma=(f"xm{j}", 1))
            norm_transpose(f"M{j}", xm[j], [f"xm{j}"], xm[j], f"xm{j}", [], ssM[:, j:j + 1], rsM[:, j:j + 1], gmemC,
                           lambda kc, j=j: memnT[:, kc, j * 128:(j + 1) * 128], f"memnT{j}a", f"memnT{j}b", KV)
        KMEM = [f"memnT{j}{ab}" for j in range(2) for ab in "ab"]
        for fc in range(8):
            b = nextbank(0, 8)

            def mm(E, fc=fc, b=b):
                return [E.matmul(out=bank(b)[:, 0:256], lhsT=wkv[:, kc, fc * 128:(fc + 1) * 128], rhs=memnT[:, kc, :],
                                 start=(kc == 0), stop=(kc == 7)) for kc in range(8)]
            sc.op("pe", mm, r=KMEM + ["wkv"], w=[f"ps{b}"])
            evac(kTx[:, fc, :], bank(b)[:, 0:256], r=[f"ps{b}"], w=[f"kTx{fc}"] + KV)
        for mt in range(2):
            for hh in range(2):
                b = nextbank(0, 8)

                def mm(E, mt=mt, hh=hh, b=b):
                    return [E.matmul(out=bank(b), lhsT=memnT[:, kc, mt * 128:(mt + 1) * 128],
                                     rhs=wkv[:, kc, 1024 + hh * 512:1024 + (hh + 1) * 512],
                                     start=(kc == 0), stop=(kc == 7)) for kc in range(8)]
                sc.op("pe", mm, r=KMEM + ["wkv"], w=[f"ps{b}"])
                evac(vx[:, mt, hh * 512:(hh + 1) * 512], bank(b), r=[f"ps{b}"], w=[f"vx{mt}.{hh}"] + KV)
        KKX = [f"kTx{fc}" for fc in range(8)]
        KVX = [f"vx{mt}.{hh}" for mt in range(2) for hh in range(2)]
        sc.op("pool", lambda E: [E.dma_start(out=wq, in_=w_q_mem.rearrange("(k p) f -> p k f", p=128))],
              w=["wq", "wkv"], dma=("wq", 1))
        sc.op("pool", lambda E: [E.dma_start(out=wo, in_=w_o_mem.rearrange("(k p) f -> p k f", p=128))],
              w=["wo", "wkv"], dma=("wo", 1))
        def bnorm(i):
            return norm_transpose(f"B{i}", X[:, i, :], [f"X{i}"], xm[i % 2], f"xm{i % 2}", [], ssA[:, i:i + 1], rsA[:, i:i + 1],
                                  gxatC, lambda kc, i=i: h2T[:, kc, i * 128:(i + 1) * 128], f"h2T{i}a", f"h2T{i}b",
                                  [f"ym{m}.{i // 4}" for m in range(8)], defer=True)
        p2 = bnorm(0)
        for i in range(NT):
            nxt2 = bnorm(i + 1) if i + 1 < NT else None
            p2()
            p2 = nxt2
        def b_qproj(c):
            cb = c % 2
            h2k = [f"h2T{i}{ab}" for i in range(4 * c, 4 * c + 4) for ab in "ab"]
            for fc in range(8):
                b = nextbank(0, 8)

                def mm(E, fc=fc, c=c, b=b):
                    return [E.matmul(out=bank(b), lhsT=wq[:, kc, fc * 128:(fc + 1) * 128],
                                     rhs=h2T[:, kc, c * 512:(c + 1) * 512], start=(kc == 0), stop=(kc == 7))
                            for kc in range(8)]
                sc.op("pe", mm, r=h2k + ["wq"], w=[f"ps{b}"])
                evac(qTx[cb][:, fc, :], bank(b), r=[f"ps{b}"], w=[f"qTx{cb}.{fc}"], scale=1.0 / 16)

        def b_att(c):
            cb = c % 2
            for h in range(4):
                eb = h % 2
                for mt in range(2):
                    b = nextbank(0, 8)

                    def mm(E, h=h, mt=mt, b=b, cb=cb):
                        return [E.matmul(out=bank(b), lhsT=kTx[:, 2 * h + dc, mt * 128:(mt + 1) * 128],
                                         rhs=qTx[cb][:, 2 * h + dc, :], start=(dc == 0), stop=(dc == 1))
                                for dc in range(2)]
                    sc.op("pe", mm, r=KKX + [f"qTx{cb}.{2 * h}", f"qTx{cb}.{2 * h + 1}"], w=[f"ps{b}"])
                    sc.op("act", lambda E, eb=eb, mt=mt, b=b: E.activation(out=ET[eb][:, mt, :], in_=bank(b), func=AF.Exp),
                          r=[f"ps{b}"], w=[f"ET{eb}.{mt}"])
                bd = nextbank(0, 8)
                sc.op("pe", lambda E, eb=eb, bd=bd: [E.matmul(out=bank(bd), lhsT=onesB, rhs=ET[eb][:, mt, :],
                                                             start=(mt == 0), stop=(mt == 1)) for mt in range(2)],
                      r=[f"ET{eb}.0", f"ET{eb}.1", "consts"], w=[f"ps{bd}"])
                sc.op("dve", lambda E, eb=eb, bd=bd: E.reciprocal(out=rden[eb], in_=bank(bd)),
                      r=[f"ps{bd}"], w=[f"rden{eb}"])
                for dc in range(2):
                    b = nextbank(0, 8)
                    sc.op("pe", lambda E, eb=eb, b=b, h=h, dc=dc: [
                        E.matmul(out=bank(b), lhsT=vx[:, mt, (2 * h + dc) * 128:(2 * h + dc + 1) * 128],
                                 rhs=ET[eb][:, mt, :], start=(mt == 0), stop=(mt == 1)) for mt in range(2)],
                        r=[f"ET{eb}.0", f"ET{eb}.1"] + KVX, w=[f"ps{b}"])
                    sc.op("dve", lambda E, eb=eb, b=b, h=h, dc=dc, cb=cb: E.tensor_tensor(
                        out=oTx[cb][:, 2 * h + dc, :], in0=bank(b), in1=rden[eb], op=ALU.mult),
                        r=[f"ps{b}", f"rden{eb}"], w=[f"oTx{cb}.{2 * h + dc}"])

        def b_oproj(c):
            cb = c % 2
            for ii in range(4):
                i = 4 * c + ii
                for hh in range(2):
                    b = nextbank(0, 8)
                    sc.op("pe", lambda E, ii=ii, hh=hh, b=b, cb=cb: [
                        E.matmul(out=bank(b), lhsT=oTx[cb][:, fc, ii * 128:(ii + 1) * 128],
                                 rhs=wo[:, fc, hh * 512:(hh + 1) * 512], start=(fc == 0), stop=(fc == 7))
                        for fc in range(8)],
                        r=[f"oTx{cb}.{fc}" for fc in range(8)] + ["wo"], w=[f"ps{b}"])
                    sc.op("dve", lambda E, i=i, hh=hh, b=b: E.tensor_tensor(
                        out=X[:, i, hh * 512:(hh + 1) * 512], in0=bank(b), in1=X[:, i, hh * 512:(hh + 1) * 512],
                        op=ALU.add), r=[f"ps{b}", f"X{i}"], w=[f"X{i}"])

        b_qproj(0)
        for c in range(4):
            b_att(c)
            if c + 1 < 4:
                b_qproj(c + 1)
            b_oproj(c)

    if stop_after in ("A", "B"):
        for i in range(NT):
            def st(E, i=i):
                return [E.dma_start(out=out[i * 128:(i + 1) * 128, :], in_=X[:, i, :])]
            sc.op("sp", st, r=[f"X{i}"], w=[f"out{i}"], dma=("out", 1))
    else:
        Xs_d = nc.dram_tensor("xs_scratch", [NE * CAP, D], BF16, kind="Internal").ap()
        Ys_d = nc.dram_tensor("ys_scratch", [NE * CAP, D], BF16, kind="Internal").ap()
        BIG = 1.0e6
        bc_reg = nc.gpsimd.to_reg(NE * CAP - 1)
        ring = [carve(76 * K + j * 16 * K, 16 * K, BF16, "p (k f) -> p k f", k=8) for j in range(5)]
        T0 = 156 * K
        idxT = carve(T0 + 44 * K, 256, I32, "p (i k) -> p i k", i=NT) if SCAT_TEST else None
        if SCAT_TEST:
            sc.op("pool", lambda E: E.iota(idxT, pattern=[[512, 16], [128, 4]], base=0, channel_multiplier=1), w=["idxT"])
        xn3 = [carve(T0 + j * 4 * K, 4 * K, F32) for j in range(2)]
        h3b = [carve(T0 + 8 * K + j * 2 * K, 2 * K, BF16) for j in range(4)]
        h3T = [carve(T0 + 16 * K + j * 4 * K, 4 * K, F32, "p (k t) -> p k t", k=8) for j in range(2)]
        o = T0 + 24 * K
        sm = {}
        for nm, nb, dt in (("lg", 128, F32), ("max8", 32, F32), ("idx8", 32, U32), ("idxf", 16, F32), ("nmax", 4, F32),
                           ("ex4", 16, F32), ("sum4", 4, F32), ("posS", 128, F32), ("oh", 128, F32), ("jr", 128, F32),
                           ("pk", 16, F32), ("okf", 16, F32), ("destf", 16, F32), ("Gd", 128, F32), ("GT", 512, F32)):
            sm[nm] = [carve(o + j * nb, nb, dt) for j in range(4)]
            o += 4 * nb
        o = (o + 63) // 64 * 64
        mskB = carve(o, 1024, BF16, "p (i e) -> p i e", i=NT); o += 1024
        b1st = carve(o, 8 * K, F32); o += 8 * K
        assert o <= ARENA
        xstage = [carve(T0 + j * 6 * K, 6 * K, BF16, "p (j d) -> p j d", j=3) for j in range(2)]
        XsT = [carve(T0 + 12 * K + j * 6 * K, 8 * CAP * 2, BF16, "p (k t) -> p k t", k=8) for j in range(2)]
        actT = carve(T0 + 24 * K, 8 * CAP * 2, BF16, "p (k t) -> p k t", k=8)
        RT = CAP - 256
        o = T0 + 30 * K
        hset = []
        for j in range(2):
            d = {}
            for nm in ("g", "sig", "l1", "gs"):
                d[nm] = carve(o, CAP * 4, F32); o += 1536
            hset.append(d)
        ystage = carve(o, 6 * K, BF16, "p (j d) -> p j d", j=3); o += 6 * K
        assert o <= ARENA
        grow = [[carve(T0 + (j * 4 + k) * 2 * K, 2 * K, BF16) for k in range(4)] for j in range(2)]
        acc = [carve(T0 + 16 * K + j * 4 * K, 4 * K, F32) for j in range(2)]
        ostage = [carve(T0 + 24 * K + j * 4 * K, 4 * K, F32) for j in range(2)]
        gfin = carve(T0 + 32 * K, 4 * K, F32)
        junk3 = carve(T0 + 36 * K, 2 * K, BF16)
        dg = [[carve(T0 + 38 * K + (j * 4 + k) * 256, 256, BF16) for k in range(4)] for j in range(2)]

        KB_ALL = ["wq", "wo", "wkv"] + KMEM + KKX + KVX + [f"h2T{i}{ab}" for i in range(NT) for ab in "ab"]
        KB_T = ["xm0", "xm1"] + [f"qTx{j}.{fc}" for j in range(2) for fc in range(8)] + \
            [f"oTx{j}.{fc}" for j in range(2) for fc in range(8)] + [f"ET{j}.{m}" for j in range(2) for m in range(2)] + \
            ["rden0", "rden1"]

        pieces = []
        for e in range(NE_RUN):
            pieces += [(e, "g"), (e, "l"), (e, "w2")]
        piece_slot = {}
        state = {"next": 0}

        def issue_piece():
            n = state["next"]
            if n >= len(pieces):
                return
            state["next"] += 1
            e, kind = pieces[n]
            slot = n % 5
            piece_slot[(e, kind)] = slot
            if kind == "g":
                src = w1[e, :, 0:1024]
            elif kind == "l":
                src = w1[e, :, 1024:2048]
            else:
                src = w2[e, :, :]
            extra = KB_ALL if n < 5 else []
            sc.op("pool", lambda E, slot=slot, src=src: [E.dma_start(out=ring[slot], in_=src.rearrange("(k p) f -> p k f", p=128))],
                  w=[f"ring{slot}"] + extra, dma=(f"ring{slot}", 1))

        if stop_after != "C1":
            for _ in range(5):
                issue_piece()

        def ld_moe_consts(E):
            r = []
            r.append(E.dma_start(out=wr, in_=w_router.rearrange("(k p) e -> p k e", p=128)))
            r.append(E.dma_start(out=brB, in_=b_router.partition_broadcast(128).rearrange("p o e -> p (o e)")))
            r.append(E.dma_start(out=b2S[0:32, :], in_=b2))
            r.append(E.dma_start(out=b1st[0:32, :], in_=b1))
            return r
        sc.op("sp", ld_moe_consts, w=["moec", "maskc", "b1st"] + KB_T, dma=("moec", 4))
        bt = nextbank(0, 8)
        sc.op("pe", lambda E: [E.transpose(out=bank(bt)[:, j * 32:(j + 1) * 32], in_=b1st[0:32, j * 128:(j + 1) * 128],
                                           identity=identF[0:32, 0:32]) for j in range(16)],
              r=["b1st", "consts"], w=[f"ps{bt}"])
        sc.op("dve", lambda E: E.tensor_copy(out=b1T, in_=bank(bt).rearrange("p (j e) -> p j e", j=16)),
              r=[f"ps{bt}"], w=["b1T", "iota"])
        sc.op("dve", lambda E: E.tensor_scalar(out=b1T[:, 8:16, :], in0=b1T[:, 8:16, :], scalar1=1.0, scalar2=None,
                                               op0=ALU.add), r=["b1T"], w=["b1T"])

        XSK = []
        def c1_tile(i):
            s_ = i % 2
            s4 = i % 4
            t = {k: v[s4] for k, v in sm.items()}
            tg = f"r{s4}"
            first = KB_T if i < 4 else []
            norm_transpose(f"C{i}", X[:, i, :], [f"X{i}"], xn3[s_], f"xn3{s_}", first, ssC[:, i:i + 1], rsC[:, i:i + 1],
                           gmoeC, lambda kc, s_=s_: h3T[s_][:, kc, :], f"h3T{s_}a", f"h3T{s_}b", first, banks=(2 * s4, 2 * s4 + 1))
            sc.op("dve", lambda E, s_=s_, s4=s4: E.tensor_copy(out=h3b[s4], in_=xn3[s_]), r=[f"xn3{s_}"], w=[f"h3b{s4}"] + first)
            bl = 2 * s4
            sc.op("pe", lambda E, s_=s_, bl=bl: [E.matmul(out=bank(bl)[:, 0:32], lhsT=h3T[s_][:, kc, :], rhs=wr[:, kc, :],
                                                         start=(kc == 0), stop=(kc == 7)) for kc in range(8)],
                  r=[f"h3T{s_}a", f"h3T{s_}b", "moec"], w=[f"ps{bl}"])
            yield
            sc.op("dve", lambda E, t=t, bl=bl: E.tensor_tensor(out=t["lg"], in0=bank(bl)[:, 0:32], in1=brB, op=ALU.add),
                  r=[f"ps{bl}", "moec"], w=["lg" + tg] + first)
            if C1_LEVEL >= 1:
                yield
                sc.op("dve", lambda E, t=t: E.max(out=t["max8"], in_=t["lg"]), r=["lg" + tg], w=["max8" + tg])
                yield
                sc.op("dve", lambda E, t=t: E.max_index(out=t["idx8"], in_max=t["max8"], in_values=t["lg"]),
                      r=["lg" + tg, "max8" + tg], w=["idx8" + tg])
                yield
                sc.op("dve", lambda E, t=t: E.tensor_scalar(out=t["nmax"], in0=t["max8"][:, 0:1], scalar1=-1.0, scalar2=None,
                                                            op0=ALU.mult), r=["max8" + tg], w=["nmax" + tg])
                yield
                sc.op("act", lambda E, t=t: E.activation(out=t["ex4"], in_=t["max8"][:, 0:4], func=AF.Exp, bias=t["nmax"],
                                                         accum_out=t["sum4"]),
                      r=["max8" + tg, "nmax" + tg], w=["ex4" + tg, "sum4" + tg])
                yield
                sc.op("dve", lambda E, t=t: E.reciprocal(out=t["sum4"], in_=t["sum4"]), r=["sum4" + tg], w=["sum4" + tg])
                yield
                sc.op("dve", lambda E, t=t, i=i: E.tensor_scalar(out=gatesA[:, i, :], in0=t["ex4"], scalar1=t["sum4"],
                                                                 scalar2=None, op0=ALU.mult),
                      r=["ex4" + tg, "sum4" + tg], w=[f"gate{i}"])
                yield
                sc.op("dve", lambda E, t=t, i=i: E.tensor_scalar(out=mskB[:, i, :], in0=t["lg"], scalar1=t["max8"][:, 3:4],
                                                                 scalar2=None, op0=ALU.is_ge),
                      r=["lg" + tg, "max8" + tg], w=[f"msk{i}"])
            if C1_LEVEL >= 2:
                bp = 2 * s4 + 1
                yield
                sc.op("pe", lambda E, i=i, bp=bp: [E.matmul(out=bank(bp)[:, 0:32], lhsT=triS, rhs=mskB[:, i, :], start=True,
                                                            stop=(i == 0))] +
                      [E.matmul(out=bank(bp)[:, 0:32], lhsT=onesB, rhs=mskB[:, i2, :], start=False, stop=(i2 == i - 1))
                       for i2 in range(i)],
                      r=[f"msk{i2}" for i2 in range(i + 1)] + ["consts"], w=[f"ps{bp}"])
                yield
                sc.op("dve", lambda E, t=t, bp=bp: E.tensor_copy(out=t["posS"], in_=bank(bp)[:, 0:32]),
                      r=[f"ps{bp}"], w=["posS" + tg])
                yield
                sc.op("dve", lambda E, t=t: E.tensor_copy(out=t["idxf"], in_=t["idx8"][:, 0:4]), r=["idx8" + tg], w=["idxf" + tg])
                for k in range(4):
                    yield
                    sc.op("dve", lambda E, t=t, k=k: E.scalar_tensor_tensor(
                        out=t["jr"], in0=iotaE, scalar=t["idxf"][:, k:k + 1], in1=t["posS"], op0=ALU.is_equal, op1=ALU.mult,
                        accum_out=t["pk"][:, k:k + 1]), r=["idxf" + tg, "posS" + tg, "consts"], w=["jr" + tg, f"pk{k}" + tg])
                yield
                sc.op("act", lambda E, t=t: E.activation(out=t["oh"], in_=t["lg"], func=AF.Exp, bias=t["nmax"]),
                      r=["lg" + tg, "nmax" + tg], w=["oh" + tg])
                yield
                sc.op("dve", lambda E, t=t: E.tensor_scalar(out=t["Gd"], in0=t["lg"], scalar1=t["max8"][:, 3:4], scalar2=None,
                                                            op0=ALU.is_ge), r=["lg" + tg, "max8" + tg], w=["Gd" + tg])
                yield
                sc.op("dve", lambda E, t=t: E.scalar_tensor_tensor(out=t["Gd"], in0=t["oh"], scalar=t["sum4"], in1=t["Gd"],
                                                                   op0=ALU.mult, op1=ALU.mult),
                      r=["oh" + tg, "sum4" + tg, "Gd" + tg], w=["Gd" + tg])
                pkk = [f"pk{k}" + tg for k in range(4)]
                yield
                sc.op("dve", lambda E, t=t: E.tensor_scalar(out=t["okf"], in0=t["pk"], scalar1=float(CAP), scalar2=None,
                                                            op0=ALU.is_lt), r=pkk, w=["okf" + tg])
                yield
                sc.op("dve", lambda E, t=t: E.scalar_tensor_tensor(out=t["destf"], in0=t["idxf"], scalar=float(CAP), in1=t["pk"],
                                                                   op0=ALU.mult, op1=ALU.add),
                      r=pkk + ["idxf" + tg], w=["destf" + tg])
                yield
                sc.op("dve", lambda E, t=t: E.scalar_tensor_tensor(out=t["destf"], in0=t["destf"], scalar=-BIG, in1=t["okf"],
                                                                   op0=ALU.add, op1=ALU.mult),
                      r=["destf" + tg, "okf" + tg], w=["destf" + tg])
                yield
                sc.op("dve", lambda E, t=t: E.tensor_scalar(out=t["destf"], in0=t["destf"], scalar1=BIG, scalar2=None,
                                                            op0=ALU.add), r=["destf" + tg], w=["destf" + tg])
                yield
                sc.op("dve", lambda E, t=t, i=i: E.tensor_copy(out=idxA[:, i, :], in_=t["destf"]),
                      r=["destf" + tg], w=[f"idx{i}"])
                yield
                sc.op("dve", lambda E, t=t, i=i: E.tensor_copy(out=t["jr"][:, 0:4], in_=idxA[:, i, :]),
                      r=[f"idx{i}"], w=["jr" + tg])
                yield
                sc.op("dve", lambda E, t=t, i=i: E.tensor_copy(out=t["jr"][:, 4:8], in_=t["jr"][:, 0:4]),
                      r=["jr" + tg], w=[f"idxfence{i}"])
            if C1_LEVEL >= 3:
                bg = 2 * s4
                yield
                sc.op("pe", lambda E, t=t, bg=bg: E.transpose(out=bank(bg)[0:32, 0:128], in_=t["Gd"], identity=identF),
                      r=["Gd" + tg, "consts"], w=[f"ps{bg}"])
                yield
                sc.op("act", lambda E, t=t, bg=bg: E.activation(out=t["GT"][0:32, :], in_=bank(bg)[0:32, 0:128], func=AF.Copy),
                      r=[f"ps{bg}"], w=["GT" + tg])
                for hh in range(2):
                    b = 2 * s4 + 1 - hh
                    yield
                    sc.op("pe", lambda E, t=t, b=b, hh=hh: E.matmul(out=bank(b), lhsT=t["GT"][0:32, :],
                                                                  rhs=b2S[0:32, hh * 512:(hh + 1) * 512], start=True, stop=True),
                          r=["GT" + tg, "moec"], w=[f"ps{b}"])
                    yield
                    sc.op("dve", lambda E, i=i, hh=hh, b=b: E.tensor_tensor(
                        out=X[:, i, hh * 512:(hh + 1) * 512], in0=bank(b), in1=X[:, i, hh * 512:(hh + 1) * 512], op=ALU.add),
                        r=[f"ps{b}", f"X{i}"], w=[f"X{i}"])
            if C1_LEVEL >= 4:
                for k in range(4):
                    def scat(E, i=i, k=k, s4=s4):
                        ia = idxT[:, i, k:k + 1] if SCAT_TEST else idxA[:, i, k:k + 1]
                        return [E.indirect_dma_start(out=Xs_d[:, :], out_offset=bass.IndirectOffsetOnAxis(ap=ia, axis=0),
                                                     in_=h3b[s4], in_offset=None, bounds_check=bc_reg, oob_is_err=False)]
                    yield
                    sc.op("pool", scat, r=[f"idx{i}", f"idxfence{i}", f"h3b{s4}", "idxT"], w=[f"Xs{i}.{k}"], dma=("scat", 1))
                    XSK.append(f"Xs{i}.{k}")
            if C1_LEVEL >= 2:
                yield
                sc.op("dve", lambda E, t=t, i=i: E.tensor_tensor(out=gatesA[:, i, :], in0=gatesA[:, i, :], in1=t["okf"], op=ALU.mult),
                      r=[f"gate{i}", "okf" + tg, "Gd" + tg], w=[f"gate{i}"])


        gens = [c1_tile(i) for i in range(NT)]
        active = []
        nxt = 0
        while nxt < NT or active:
            while len(active) < 4 and nxt < NT:
                active.append(gens[nxt]); nxt += 1
            for g in list(active):
                try:
                    next(g)
                except StopIteration:
                    active.remove(g)

        def phase_C23():
            C1K = ["xn30", "xn31", "h3b0", "h3b1", "h3b2", "h3b3", "h3T0a", "h3T0b", "h3T1a", "h3T1b", "b1st"] + \
                [nm + f"r{j}" for j in range(4) for nm in ("lg", "max8", "idx8", "idxf", "nmax", "ex4", "sum4", "posS", "oh", "jr",
                                                           "pk0", "pk1", "pk2", "pk3", "okf", "destf", "Gd", "GT")] + \
                [f"msk{i}" for i in range(NT)]
            bar_dummy = carve(T0 + 48 * K, 64, F32)
            sc.op("pool", lambda E: E.memset(bar_dummy, 0.0), w=C1K + ["bar1"])

            def load_xs(e):
                s_ = e % 2
                sc.op("act", lambda E, e=e, s_=s_: [
                    E.dma_start(out=xstage[s_][:, 0:2, :], in_=Xs_d[e * CAP:e * CAP + 256, :].rearrange("(j p) d -> p j d", p=128)),
                    E.dma_start(out=xstage[s_][0:RT, 2, :], in_=Xs_d[e * CAP + 256:(e + 1) * CAP, :])],
                    r=XSK + ["bar1"], w=[f"xstage{s_}"], dma=(f"xstage{s_}", 2))

            def xs_transposes(e):
                s_ = e % 2
                if C2_LEVEL < 1:
                    return
                for pr in range(4):
                    bk = pr % 2
                    bv = bank(bk).bitcast(BF16)

                    def tr(E, s_=s_, pr=pr, bv=bv):
                        r = []
                        for kl in range(2):
                            kc = 2 * pr + kl
                            for j in range(3):
                                rows = 128 if j < 2 else RT
                                r.append(E.transpose(out=bv[:, kl * CAP + j * 128:kl * CAP + j * 128 + rows],
                                                     in_=xstage[s_][0:rows, j, kc * 128:(kc + 1) * 128],
                                                     identity=identB[0:rows, 0:rows]))
                        return r
                    sc.op("pe", tr, r=[f"xstage{s_}", "consts", "bar1"], w=[f"ps{bk}"])
                    for kl in range(2):
                        kc = 2 * pr + kl
                        if C2_SUB < 1 or (C2_SUB < 2 and kl == 1):
                            continue
                        sc.op("dve", lambda E, s_=s_, kc=kc, kl=kl, bv=bv: E.tensor_scalar(
                            out=XsT[s_][:, kc, :], in0=bv[:, kl * CAP:(kl + 1) * CAP], scalar1=gmoeC[:, kc:kc + 1], scalar2=None,
                            op0=ALU.mult), r=[f"ps{bk}", "gains", "bar1"], w=[f"XsT{s_}.{kc}"])

            if NE_RUN > 0:
                load_xs(0)
                load_xs(1)
                xs_transposes(0)
            YSK = []
            for e in range(NE_RUN):
                s_ = e % 2
                sg, sl, s2 = piece_slot[(e, "g")], piece_slot[(e, "l")], piece_slot[(e, "w2")]
                xk = [f"XsT{s_}.{kc}" for kc in range(8)]
                for j8 in range(8 if C2_LEVEL >= 2 else 0):
                    H = hset[j8 % 2]
                    ht = f"h{j8 % 2}"
                    bG, bL = 2 + 2 * (j8 % 2), 3 + 2 * (j8 % 2)
                    sc.op("pe", lambda E, s_=s_, sg=sg, j8=j8, bG=bG: [
                        E.matmul(out=bank(bG)[:, 0:CAP], lhsT=ring[sg][:, kc, j8 * 128:(j8 + 1) * 128], rhs=XsT[s_][:, kc, :],
                                 start=(kc == 0), stop=(kc == 7)) for kc in range(8)],
                        r=xk + [f"ring{sg}"], w=[f"ps{bG}"])
                    sc.op("pe", lambda E, s_=s_, sl=sl, j8=j8, bL=bL: [
                        E.matmul(out=bank(bL)[:, 0:CAP], lhsT=ring[sl][:, kc, j8 * 128:(j8 + 1) * 128], rhs=XsT[s_][:, kc, :],
                                 start=(kc == 0), stop=(kc == 7)) for kc in range(8)],
                        r=xk + [f"ring{sl}"], w=[f"ps{bL}"])
                    sc.op("dve", lambda E, H=H, e=e, j8=j8, bG=bG: E.tensor_scalar(
                        out=H["g"], in0=bank(bG)[:, 0:CAP], scalar1=b1T[:, j8, e:e + 1], scalar2=7.0, op0=ALU.add, op1=ALU.min),
                        r=[f"ps{bG}", "b1T", "bar1"], w=["g" + ht])
                    sc.op("act", lambda E, H=H: E.activation(out=H["sig"], in_=H["g"], func=AF.Sigmoid, scale=1.702),
                          r=["g" + ht, "bar1"], w=["sig" + ht])
                    sc.op("dve", lambda E, H=H, e=e, j8=j8, bL=bL: E.tensor_scalar(
                        out=H["l1"], in0=bank(bL)[:, 0:CAP], scalar1=b1T[:, 8 + j8, e:e + 1], scalar2=8.0, op0=ALU.add, op1=ALU.min),
                        r=[f"ps{bL}", "b1T", "bar1"], w=["l1" + ht])
                    sc.op("dve", lambda E, H=H: E.tensor_tensor(out=H["gs"], in0=H["g"], in1=H["sig"], op=ALU.mult),
                          r=["g" + ht, "sig" + ht, "bar1"], w=["gs" + ht])
                    sc.op("dve", lambda E, H=H, j8=j8: E.scalar_tensor_tensor(
                        out=actT[:, j8, :], in0=H["l1"], scalar=-6.0, in1=H["gs"], op0=ALU.max, op1=ALU.mult),
                        r=["l1" + ht, "gs" + ht, "bar1"], w=[f"actT{j8}"])
                issue_piece()
                issue_piece()
                if e + 2 < NE_RUN:
                    load_xs(e + 2)
                if e + 1 < NE_RUN:
                    xs_transposes(e + 1)
                for j in range(3 if C2_LEVEL >= 3 else 0):
                    for hh in range(2):
                        bO = 6 + (2 * j + hh) % 2
                        rows = 128 if j < 2 else RT
                        sc.op("pe", lambda E, j=j, hh=hh, bO=bO, s2=s2, rows=rows: [
                            E.matmul(out=bank(bO)[0:rows, :], lhsT=actT[:, j8, j * 128:j * 128 + rows],
                                     rhs=ring[s2][:, j8, hh * 512:(hh + 1) * 512],
                                     start=(j8 == 0), stop=(j8 == 7)) for j8 in range(8)],
                            r=[f"actT{j8}" for j8 in range(8)] + [f"ring{s2}"], w=[f"ps{bO}"])
                        if hh == 0:
                            sc.op("act", lambda E, j=j, hh=hh, bO=bO, rows=rows: E.activation(
                                out=ystage[0:rows, j, hh * 512:(hh + 1) * 512], in_=bank(bO)[0:rows, :], func=AF.Copy),
                                r=[f"ps{bO}", "bar1"], w=[f"ystage{j}.{hh}"])
                        else:
                            sc.op("dve", lambda E, j=j, hh=hh, bO=bO, rows=rows: E.tensor_copy(
                                out=ystage[0:rows, j, hh * 512:(hh + 1) * 512], in_=bank(bO)[0:rows, :]),
                                r=[f"ps{bO}", "bar1"], w=[f"ystage{j}.{hh}"])
                issue_piece()
                if C2_LEVEL < 3:
                    continue
                sc.op("sp", lambda E, e=e: [
                    E.dma_start(out=Ys_d[e * CAP:e * CAP + 256, :].rearrange("(j p) d -> p j d", p=128), in_=ystage[:, 0:2, :]),
                    E.dma_start(out=Ys_d[e * CAP + 256:(e + 1) * CAP, :], in_=ystage[0:RT, 2, :])],
                    r=[f"ystage{j}.{hh}" for j in range(3) for hh in range(2)], w=[f"Ys{e}"], dma=("ys", 2))
                YSK.append(f"Ys{e}")

            if stop_after == "C2":
                for i in range(NT):
                    sc.op("sp", lambda E, i=i: [E.dma_start(out=out[i * 128:(i + 1) * 128, :], in_=X[:, i, :])],
                          r=[f"X{i}"] + YSK, w=[f"out{i}"], dma=("out", 1))
                return
            C2K = ["xstage0", "xstage1"] + [f"XsT{j}.{kc}" for j in range(2) for kc in range(8)] + [f"actT{j8}" for j8 in range(8)] + \
                [nm + f"h{j}" for j in range(2) for nm in ("g", "sig", "l1", "gs")] + [f"ystage{j}.{hh}" for j in range(3) for hh in range(2)]
            sc.op("sp", lambda E: [E.dma_start(out=gfin, in_=g_final.partition_broadcast(128).rearrange("p o d -> p (o d)"))],
                  w=["gfin"] + C2K, dma=("gfin", 1))
            sc.op("dve", lambda E: [E.memset(grow[j][k], 0.0) for j in range(2) for k in range(4)],
                  r=["gfin"], w=[f"grow{j}.{k}" for j in range(2) for k in range(4)])
            for i in range(NT):
                s_ = i % 2
                for k in range(4):
                    def gath(E, i=i, k=k, s_=s_):
                        return [E.indirect_dma_start(out=grow[s_][k], out_offset=None, in_=Ys_d[:, :],
                                                     in_offset=bass.IndirectOffsetOnAxis(ap=idxA[:, i, k:k + 1], axis=0),
                                                     bounds_check=bc_reg, oob_is_err=False)]
                    sc.op("pool", gath, r=YSK + [f"idx{i}"], w=[f"grow{s_}.{k}"], dma=(f"gath{s_}.{k}", 1))
                for k in range(4):
                    sc.op("act", lambda E, i=i, s_=s_, k=k: E.activation(out=dg[s_][k], in_=identB, func=AF.Identity,
                                                                        scale=gatesA[:, i, k:k + 1]),
                          r=[f"gate{i}", "consts", "gfin"], w=[f"dg{s_}.{k}"])
                for hh in range(2):
                    bq = 2 * s_ + hh
                    sc.op("pe", lambda E, s_=s_, hh=hh, bq=bq: [
                        E.matmul(out=bank(bq), lhsT=dg[s_][k], rhs=grow[s_][k][:, hh * 512:(hh + 1) * 512],
                                 start=(k == 0), stop=(k == 3)) for k in range(4)],
                        r=[f"dg{s_}.{k}" for k in range(4)] + [f"grow{s_}.{k}" for k in range(4)], w=[f"ps{bq}"])
                    sc.op("dve", lambda E, i=i, s_=s_, hh=hh, bq=bq: E.tensor_tensor(
                        out=acc[s_][:, hh * 512:(hh + 1) * 512], in0=bank(bq), in1=X[:, i, hh * 512:(hh + 1) * 512], op=ALU.add),
                        r=[f"ps{bq}", f"X{i}", "gfin"], w=[f"acc{s_}"] if hh == 0 else [f"acc{s_}b"])
                sc.op("act", lambda E, i=i, s_=s_: E.activation(out=junk3, in_=acc[s_], func=AF.Square, accum_out=ssA[:, i:i + 1]),
                      r=[f"acc{s_}", f"acc{s_}b", "gfin"], w=[f"ssF{i}"])
                sc.op("act", lambda E, i=i: E.activation(out=rsA[:, i:i + 1], in_=ssA[:, i:i + 1], func=AF.Ln, scale=1.0 / D, bias=EPS),
                      r=[f"ssF{i}"], w=[f"rsF{i}"])
                sc.op("act", lambda E, i=i: E.activation(out=rsA[:, i:i + 1], in_=rsA[:, i:i + 1], func=AF.Exp, scale=-0.5),
                      r=[f"rsF{i}"], w=[f"rsF{i}"])
                sc.op("dve", lambda E, i=i, s_=s_: E.scalar_tensor_tensor(out=ostage[s_], in0=acc[s_], scalar=rsA[:, i:i + 1], in1=gfin,
                                                                          op0=ALU.mult, op1=ALU.mult),
                      r=[f"acc{s_}", f"acc{s_}b", f"rsF{i}", "gfin"], w=[f"ostage{s_}"])
                sc.op("sp", lambda E, i=i, s_=s_: [E.dma_start(out=out[i * 128:(i + 1) * 128, :], in_=ostage[s_])],
                      r=[f"ostage{s_}"], w=[f"out{i}"], dma=("out", 1))

        if stop_after == "C1":
            dbg = nc.dram_tensor("dbg", [128, 128], F32, kind="ExternalOutput").ap()
            dbgS = carve(T0 + 40 * K, 512, F32)
            sc.op("dve", lambda E: [E.tensor_copy(out=dbgS[:, 0:64], in_=gatesA.rearrange("p i k -> p (i k)")),
                                    E.tensor_copy(out=dbgS[:, 64:128], in_=idxA.rearrange("p i k -> p (i k)"))],
                  r=[f"gate{i}" for i in range(NT)] + [f"idx{i}" for i in range(NT)] + XSK, w=["dbgS"])
            sc.op("sp", lambda E: [E.dma_start(out=dbg, in_=dbgS)], r=["dbgS"], w=["dbgout"], dma=("dbg", 1))
            for i in range(NT):
                sc.op("sp", lambda E, i=i: [E.dma_start(out=out[i * 128:(i + 1) * 128, :], in_=X[:, i, :])],
                      r=[f"X{i}", "dbgout"], w=[f"out{i}"], dma=("out", 1))
        else:
            phase_C23()
    sc.op("sp", lambda E: E.nop(), r=[f"out{i}" for i in range(NT)], w=["done"])
    nw = sc.emit()
    print(f"[kernel] ops={len(sc.ops)} waits={nw}")


_IN_NAMES = ["x", "mem", "g_mix", "w_in", "conv_w", "g_conv_out", "g_sb_out", "w_out", "g_xattn", "g_mem",
             "w_q_mem", "w_kv_mem", "w_o_mem", "g_moe", "w_router", "b_router", "w1", "b1", "w2", "b2", "g_final"]


def kernel(**inputs):
    nc = build_program()
    a = {k: np.ascontiguousarray(np.asarray(v, dtype=np.float32)) for k, v in inputs.items()}
    shared = {}
    for k in _IN_NAMES:
        if k in ("x", "mem"):
            continue
        v = a[k]
        if k == "g_final":
            v = v.reshape(1, D)
        elif v.shape[0] == 1:
            v = v[0]
            if v.ndim == 1:
                v = v.reshape(1, -1)
        shared[k] = np.ascontiguousarray(v)
    in_maps = []
    for c in range(NCORES):
        m = dict(shared)
        m["x"] = np.ascontiguousarray(a["x"][c])
        m["mem"] = np.ascontiguousarray(a["mem"][c])
        in_maps.append(m)
    res = run_bass_kernel_spmd(nc, in_maps, core_ids=list(range(NCORES)))
    return np.stack([np.asarray(r["out"], dtype=np.float32) for r in res.results], axis=0)
```

```python
import contextlib
import numpy as np
import concourse.bass as bass
import concourse.mybir as mybir
from concourse.bass_utils import run_bass_kernel_spmd

F32 = mybir.dt.float32
BF16 = mybir.dt.bfloat16
I32 = mybir.dt.int32
U32 = mybir.dt.uint32
U8 = mybir.dt.uint8
AF = mybir.ActivationFunctionType
ALU = mybir.AluOpType

NCORES = 8
S = 2048
D = 1024
NT = S // 128
MEM = 256
NE = 32
C1_LEVEL = 9
ATT_SPLIT = 0
C2_LEVEL = 9
C2_SUB = 9
NE_RUN = 32
SCAT_TEST = False
CAP = 384
EPS = 1e-5

ENGMAP = {"pe": "tensor", "act": "scalar", "dve": "vector", "pool": "gpsimd", "sp": "sync"}


class Sched:
    def __init__(self, nc, es):
        self.nc = nc
        self.es = es
        self.ops = []
        self.last_w = {}
        self.readers = {}
        self.eng_sem = {e: es.enter_context(nc.semaphore("sem_" + e)) for e in ENGMAP}
        self.dma_sem = {}
        self.dma_last = {}

    def op(self, eng, fn, r=(), w=(), dma=None):
        idx = len(self.ops)
        deps = set()
        for k in r:
            p = self.last_w.get(k)
            if p is not None:
                deps.add(p)
        for k in w:
            p = self.last_w.get(k)
            if p is not None:
                deps.add(p)
            deps |= self.readers.get(k, set())
        deps.discard(idx)
        for k in r:
            self.readers.setdefault(k, set()).add(idx)
        for k in w:
            self.last_w[k] = idx
            self.readers[k] = set()
        if dma is not None:
            name, n = dma
            if name not in self.dma_sem:
                self.dma_sem[name] = [self.es.enter_context(self.nc.semaphore("dq_" + name)), 0]
        self.ops.append(dict(eng=eng, fn=fn, deps=deps, dma=dma, token=None, needed=False))
        return idx

    def emit(self):
        ops = self.ops
        for op in ops:
            for d in op["deps"]:
                P = ops[d]
                if P["eng"] == "pe" and op["eng"] == "pe" and P["dma"] is None and op["dma"] is None:
                    continue
                P["needed"] = True
        cnt = {e: 0 for e in ENGMAP}
        for op in ops:
            if op["dma"] is not None:
                name, n = op["dma"]
                ent = self.dma_sem[name]
                ent[1] += 16 * n
                op["token"] = (ent[0], ent[1])
            elif op["needed"]:
                cnt[op["eng"]] += 1
                op["token"] = (self.eng_sem[op["eng"]], cnt[op["eng"]])
        waited = {e: {} for e in ENGMAP}
        nwaits = 0
        for op in ops:
            E = getattr(self.nc, ENGMAP[op["eng"]])
            need = {}
            for d in op["deps"]:
                P = ops[d]
                if P["token"] is None:
                    continue
                if P["eng"] == "pe" and op["eng"] == "pe" and P["dma"] is None and op["dma"] is None:
                    continue
                sem, c = P["token"]
                key = id(sem)
                if key not in need or need[key][1] < c:
                    need[key] = (sem, c)
            for key, (sem, c) in need.items():
                if waited[op["eng"]].get(key, 0) >= c:
                    continue
                E.wait_ge(sem, c)
                nwaits += 1
                waited[op["eng"]][key] = c
            res = op["fn"](E)
            if op["dma"] is not None:
                sem = op["token"][0]
                assert len(res) == op["dma"][1], (len(res), op["dma"])
                for ins in res:
                    ins.then_inc(sem, 16)
            elif op["token"] is not None:
                last = res[-1] if isinstance(res, (list, tuple)) else res
                last.then_inc(op["token"][0], 1)
        return nwaits


def build_program(stop_after=None, use=None):
    nc = bass.Bass("TRN2", target_bir_lowering=False)
    es = contextlib.ExitStack()
    with es:
        _build(nc, es, stop_after, use)
    return nc


def _build(nc, es, stop_after, use):
    def din(name, shape):
        if use is not None and name not in use:
            return None
        return nc.dram_tensor(name, list(shape), F32, kind="ExternalInput").ap()

    x = din("x", [S, D])
    mem = din("mem", [MEM, D])
    g_mix = din("g_mix", [1, D])
    w_in = din("w_in", [D, 3072])
    conv_w = din("conv_w", [3, 512])
    g_conv_out = din("g_conv_out", [1, 512])
    g_sb_out = din("g_sb_out", [1, 512])
    w_out = din("w_out", [D, D])
    g_xattn = din("g_xattn", [1, D])
    g_mem = din("g_mem", [1, D])
    w_q_mem = din("w_q_mem", [D, D])
    w_kv_mem = din("w_kv_mem", [D, 2 * D])
    w_o_mem = din("w_o_mem", [D, D])
    g_moe = din("g_moe", [1, D])
    w_router = din("w_router", [D, NE])
    b_router = din("b_router", [1, NE])
    w1 = din("w1", [max(NE_RUN, 1), D, 2 * D])
    b1 = din("b1", [NE, 2 * D])
    w2 = din("w2", [max(NE_RUN, 1), D, D])
    b2 = din("b2", [NE, D])
    g_final = din("g_final", [1, D])
    out = nc.dram_tensor("out", [S, D], F32, kind="ExternalOutput").ap()

    sc = Sched(nc, es)

    ARENA = 206 * 1024
    arena = es.enter_context(nc.sbuf_tensor("arena", [128, ARENA], U8))
    psum = es.enter_context(nc.psum_tensor("psum", [128, 8, 512], F32))

    def carve(off, nbytes, dt, pat=None, **kw):
        v = arena[:, off:off + nbytes].bitcast(dt)
        if pat is not None:
            v = v.rearrange(pat, **kw)
        return v

    def bank(b):
        return psum[:, b, :]

    K = 1024
    o = 0
    identF = carve(o, 512, F32); o += 512
    onesB = carve(o, 256, BF16); o += 256
    nonesB = carve(o, 256, BF16); o += 256
    ntriB = carve(o, 256, BF16); o += 256
    bdiagB = carve(o, 256, BF16); o += 256
    maskB = carve(o, 4096, BF16, "p (r f) -> p r f", r=4); o += 4096
    iotaI = carve(o, 2048, I32); o += 2048
    gtab = carve(o, 208, F32)
    gmixC = carve(o, 32, F32); o += 32
    gxatC = carve(o, 32, F32); o += 32
    gmemC = carve(o, 32, F32); o += 32
    gmoeC = carve(o, 32, F32); o += 32
    gconvC = carve(o, 16, F32); o += 16
    gsbC = carve(o, 16, F32); o += 16
    convC = carve(o, 48, F32, "p (k m) -> p k m", k=3); o += 48
    ssA = carve(o, 64, F32); o += 64
    rsA = carve(o, 64, F32); o += 64
    ssM = carve(o, 8, F32); o += 8
    rsM = carve(o, 8, F32); o += 8
    identB = carve(o, 256, BF16); o += 256
    triS = carve(o, 256, BF16); o += 256
    iotaE = carve(o, 128, F32); o += 128
    iotaEi = carve(o, 128, I32); o += 128
    brB = carve(o, 128, F32); o += 128
    wr = carve(o, 1024, F32, "p (k e) -> p k e", k=8); o += 1024
    gatesA = carve(o, 256, F32, "p (i k) -> p i k", i=NT); o += 256
    idxA = carve(o, 256, I32, "p (i k) -> p i k", i=NT); o += 256
    ssC = carve(o, 64, F32); o += 64
    rsC = carve(o, 64, F32); o += 64
    gst = carve(o, 512, F32); o += 512
    b1T = iotaI.bitcast(F32).rearrange("p (j e) -> p j e", j=16)
    b2S = maskB.rearrange("p r f -> p (r f)").bitcast(F32)
    assert o <= 12 * K
    XOFF = 12 * K
    X = carve(XOFF, 64 * K, F32, "p (i d) -> p i d", i=NT)
    hT = carve(XOFF, 32 * K, BF16, "p (k t) -> p k t", k=8)
    wsl = [carve(XOFF + 32 * K + j * 8 * K, 8 * K, BF16, "p (k f) -> p k f", k=8) for j in range(3)]
    xs = [carve(XOFF + 56 * K + j * 4 * K, 4 * K, F32) for j in range(2)]
    o = XOFF + 64 * K
    qT = carve(o, 16 * K, BF16, "p (m t) -> p m t", m=4); o += 16 * K
    kT = carve(o, 16 * K, BF16, "p (m t) -> p m t", m=4); o += 16 * K
    vtok = carve(o, 16 * K, BF16, "p (i f) -> p i f", i=NT); o += 16 * K
    ymix = carve(o, 32 * K, BF16, "p (k t) -> p k t", k=8); o += 32 * K
    TOFF = o
    assert TOFF == 156 * K
    xn = [carve(TOFF + j * 4 * K, 4 * K, F32) for j in range(2)]
    junk = carve(TOFF + 8 * K, 2 * K, BF16)
    uT = carve(TOFF + 10 * K, 4 * (S + 2) * 2, BF16, "p (m t) -> p m t", m=4)
    o = TOFF + 10 * K + 4 * (S + 2) * 2
    o = (o + 63) // 64 * 64
    cset = []
    for j in range(2):
        d = {}
        for nm, nb, dt in (("tc", 2 * K, F32), ("t0", 2 * K, F32), ("t1", 2 * K, F32), ("yc", 2 * K, F32),
                           ("sq", 1 * K, BF16), ("rs", 2 * K, F32)):
            d[nm] = carve(o, nb, dt); o += nb
        cset.append(d)
    assert o <= ARENA, o
    o = TOFF
    AT = {}
    AT["e"] = [carve(o + i * 2 * K, 2 * K, BF16, "p (h t) -> p h t", h=2) for i in range(3)]; o += 6 * K
    AT["sp"] = [carve(o + i * 2 * K, 2 * K, BF16, "p (h t) -> p h t", h=2) for i in range(2)]; o += 4 * K
    AT["sacc"] = [carve(o + i * 2 * K, 2 * K, BF16, "p (h t) -> p h t", h=2) for i in range(2)]; o += 4 * K
    AT["p"] = [carve(o + i * 2 * K, 2 * K, BF16, "p (h t) -> p h t", h=2) for i in range(2)]; o += 4 * K
    gn = {}
    gn["yraw"] = carve(o, 2 * K, F32); o += 2 * K
    gn["sq"] = carve(o, K, BF16); o += K
    gn["rs"] = carve(o, 2 * K, F32); o += 2 * K
    o = TOFF + 27 * K
    wo_sl = carve(o, 16 * K, BF16, "p (k f) -> p k f", k=8); o += 16 * K
    assert o <= ARENA

    def c_iota(E):
        return E.iota(iotaI, pattern=[[1, 512]], base=0, channel_multiplier=-1)
    sc.op("pool", c_iota, w=["iota"])

    def c_consts(E):
        r = []
        r.append(E.tensor_scalar(out=identF, in0=iotaI[:, 0:128], scalar1=0.0, scalar2=None, op0=ALU.is_equal))
        r.append(E.memset(onesB, 1.0))
        r.append(E.memset(nonesB, -1.0))
        r.append(E.tensor_scalar(out=ntriB, in0=iotaI[:, 0:128], scalar1=0.0, scalar2=-1.0,
                                 op0=ALU.is_le, op1=ALU.mult))
        r.append(E.memset(bdiagB[0:64, 0:64], 1.0))
        r.append(E.memset(bdiagB[0:64, 64:128], 0.0))
        r.append(E.memset(bdiagB[64:128, 0:64], 0.0))
        r.append(E.memset(bdiagB[64:128, 64:128], 1.0))
        for rr in range(4):
            r.append(E.tensor_scalar(out=maskB[:, rr, :], in0=iotaI, scalar1=float(128 * rr), scalar2=-29952.0,
                                     op0=ALU.is_le, op1=ALU.mult))
        r.append(E.memset(uT[:, :, 0:2], 0.0))
        r.append(E.tensor_scalar(out=triS, in0=iotaI[:, 0:128], scalar1=0.0, scalar2=None, op0=ALU.is_gt))
        r.append(E.tensor_copy(out=iotaE, in_=iotaEi))
        return r
    sc.op("pool", lambda E: E.iota(iotaEi, pattern=[[1, 32]], base=0, channel_multiplier=0), w=["iotaE"])
    sc.op("dve", c_consts, r=["iota", "iotaE"], w=["consts1", "uhalo", "maskc"])
    sc.op("dve", lambda E: E.tensor_copy(out=identB, in_=identF), r=["consts1"], w=["consts"])

    def c_gains(E):
        r = []
        srcs = [g_mix.rearrange("o (c p) -> (o c) p", p=128), g_xattn.rearrange("o (c p) -> (o c) p", p=128),
                g_mem.rearrange("o (c p) -> (o c) p", p=128), g_moe.rearrange("o (c p) -> (o c) p", p=128),
                g_conv_out.rearrange("o (c p) -> (o c) p", p=128), g_sb_out.rearrange("o (c p) -> (o c) p", p=128),
                conv_w.rearrange("k (m p) -> (k m) p", p=128)]
        row = 0
        for src in srcs:
            n = src.shape[0]
            r.append(E.dma_start(out=gst[row:row + n, :], in_=src))
            row += n
        return r
    sc.op("sp", c_gains, w=["gst"], dma=("gains", 7))
    sc.op("pe", lambda E: E.transpose(out=bank(7)[:, 0:52], in_=gst[0:52, :], identity=identF[0:52, 0:52]),
          r=["gst", "consts"], w=["ps7"])
    sc.op("dve", lambda E: E.tensor_copy(out=gtab, in_=bank(7)[:, 0:52]), r=["ps7"], w=["gains"])

    def a1_p1(i):
        s = i % 2

        def ld(E, i=i, s=s):
            return [E.dma_start(out=xs[s], in_=x[i * 128:(i + 1) * 128, :])]
        sc.op("sp", ld, w=[f"xs{s}"], dma=(f"xs{s}", 1))

        def sq(E, i=i, s=s):
            return E.activation(out=junk, in_=xs[s], func=AF.Square, accum_out=ssA[:, i:i + 1])
        sc.op("act", sq, r=[f"xs{s}"], w=[f"ss{i}"])

        def ln(E, i=i):
            return E.activation(out=rsA[:, i:i + 1], in_=ssA[:, i:i + 1], func=AF.Ln, scale=1.0 / D, bias=EPS)
        sc.op("act", ln, r=[f"ss{i}"], w=[f"rs{i}"])

        def ex(E, i=i):
            return E.activation(out=rsA[:, i:i + 1], in_=rsA[:, i:i + 1], func=AF.Exp, scale=-0.5)
        sc.op("act", ex, r=[f"rs{i}"], w=[f"rs{i}"])

        def nrm(E, i=i, s=s):
            return E.tensor_scalar(out=xn[s], in0=xs[s], scalar1=rsA[:, i:i + 1], scalar2=None, op0=ALU.mult)
        sc.op("dve", nrm, r=[f"xs{s}", f"rs{i}"], w=[f"xn{s}"])

    def a1_p2(i):
        s = i % 2
        b0 = 2 * s

        def tr(E, s=s, b0=b0):
            r = []
            for kc in range(8):
                r.append(E.transpose(out=bank(b0 + kc // 4)[:, (kc % 4) * 128:(kc % 4 + 1) * 128],
                                     in_=xn[s][:, kc * 128:(kc + 1) * 128], identity=identF))
            return r
        sc.op("pe", tr, r=[f"xn{s}", "consts"], w=[f"ps{b0}", f"ps{b0 + 1}"])

        def ev0(E, i=i, b0=b0):
            r = []
            for kc in range(4):
                r.append(E.tensor_scalar(out=hT[:, kc, i * 128:(i + 1) * 128],
                                         in0=bank(b0)[:, kc * 128:(kc + 1) * 128],
                                         scalar1=gmixC[:, kc:kc + 1], scalar2=None, op0=ALU.mult))
            return r
        sc.op("dve", ev0, r=[f"ps{b0}", "gains"], w=[f"hT{i}a"])

        def ev1(E, i=i, b0=b0):
            r = []
            for kc in range(4, 8):
                r.append(E.activation(out=hT[:, kc, i * 128:(i + 1) * 128],
                                      in_=bank(b0 + 1)[:, (kc - 4) * 128:(kc - 3) * 128],
                                      func=AF.Identity, scale=gmixC[:, kc:kc + 1]))
            return r
        sc.op("act", ev1, r=[f"ps{b0 + 1}", "gains"], w=[f"hT{i}b"])

    a1_p1(0)
    for i in range(NT):
        if i + 1 < NT:
            a1_p1(i + 1)
        a1_p2(i)

    def hkeys(c):
        return [f"hT{i}a" for i in range(4 * c, 4 * c + 4)] + [f"hT{i}b" for i in range(4 * c, 4 * c + 4)]

    def load_w_in(slot, blk):
        def f(E):
            return [E.dma_start(out=wsl[slot],
                                in_=w_in[:, blk * 512:(blk + 1) * 512].rearrange("(k p) f -> p k f", p=128))]
        sc.op("pool", f, w=[f"wsl{slot}"], dma=(f"wsl{slot}", 1))

    load_w_in(0, 3)
    load_w_in(1, 4)
    load_w_in(2, 5)
    pb = [4]

    def nextbank(lo=4, hi=8):
        b = pb[0]
        pb[0] = lo + (pb[0] - lo + 1) % (hi - lo)
        return b

    evtog = [0]

    def evac(out_ap, in_ap, r, w, scale=None):
        evtog[0] ^= 1
        if evtog[0]:
            def f(E):
                if scale is None:
                    return E.activation(out=out_ap, in_=in_ap, func=AF.Copy)
                return E.activation(out=out_ap, in_=in_ap, func=AF.Copy, scale=scale)
            sc.op("act", f, r=r, w=w)
        else:
            def f(E):
                if scale is None:
                    return E.tensor_copy(out=out_ap, in_=in_ap)
                return E.tensor_scalar(out=out_ap, in0=in_ap, scalar1=scale, scalar2=None, op0=ALU.mult)
            sc.op("dve", f, r=r, w=w)

    for slot, dstT, scl, nm in ((0, qT, 0.125, "qT"), (1, kT, None, "kT")):
        for c in range(4):
            for m in range(4):
                b = nextbank()

                def mm(E, slot=slot, c=c, m=m, b=b):
                    r = []
                    for kc in range(8):
                        r.append(E.matmul(out=bank(b), lhsT=wsl[slot][:, kc, m * 128:(m + 1) * 128],
                                          rhs=hT[:, kc, c * 512:(c + 1) * 512], start=(kc == 0), stop=(kc == 7)))
                    return r
                sc.op("pe", mm, r=hkeys(c) + [f"wsl{slot}"], w=[f"ps{b}"])
                evac(dstT[:, m, c * 512:(c + 1) * 512], bank(b), r=[f"ps{b}"], w=[f"{nm}{m}.{c}"], scale=scl)
    for i in range(NT):
        b = nextbank()

        def mm(E, i=i, b=b):
            r = []
            for kc in range(8):
                r.append(E.matmul(out=bank(b), lhsT=hT[:, kc, i * 128:(i + 1) * 128], rhs=wsl[2][:, kc, :],
                                  start=(kc == 0), stop=(kc == 7)))
            return r
        sc.op("pe", mm, r=[f"hT{i}a", f"hT{i}b", "wsl2"], w=[f"ps{b}"])
        evac(vtok[:, i, :], bank(b), r=[f"ps{b}"], w=[f"v{i}"])

    load_w_in(0, 1)
    load_w_in(1, 2)
    load_w_in(2, 0)
    conv_state = {}

    def conv_p1(it, c, m):
        cs = cset[it % 2]
        sfx = f"c{it % 2}"
        bC, bX, bB = nextbank(0, 8), nextbank(0, 8), nextbank(0, 8)
        conv_state[it] = bB
        for slot, b in ((0, bC), (1, bX), (2, bB)):
            def mm(E, slot=slot, c=c, m=m, b=b):
                r = []
                for kc in range(8):
                    r.append(E.matmul(out=bank(b), lhsT=wsl[slot][:, kc, m * 128:(m + 1) * 128],
                                      rhs=hT[:, kc, c * 512:(c + 1) * 512], start=(kc == 0), stop=(kc == 7)))
                return r
            sc.op("pe", mm, r=hkeys(c) + [f"wsl{slot}"], w=[f"ps{b}"])
        sc.op("act", lambda E, cs=cs, bC=bC: E.activation(out=cs["tc"], in_=bank(bC), func=AF.Copy),
              r=[f"ps{bC}"], w=["tc" + sfx])
        t0 = 2 + c * 512
        sc.op("dve", lambda E, cs=cs, bX=bX, m=m, t0=t0: E.tensor_tensor(
            out=uT[:, m, t0:t0 + 512], in0=bank(bX), in1=cs["tc"], op=ALU.mult),
            r=[f"ps{bX}", "tc" + sfx, "uhalo"], w=[f"u{m}.{c}"])

    def conv_p2(it, c, m):
        cs = cset[it % 2]
        sfx = f"c{it % 2}"
        bB = conv_state[it]
        bG = nextbank(0, 8)
        t0 = 2 + c * 512
        ukeys = [f"u{m}.{c}"] + ([f"u{m}.{c - 1}"] if c > 0 else ["uhalo"])
        sc.op("act", lambda E, cs=cs, m=m, t0=t0: E.activation(
            out=cs["t0"], in_=uT[:, m, t0:t0 + 512], func=AF.Identity, scale=convC[:, 2, m:m + 1]),
            r=ukeys + ["gains"], w=["t0" + sfx])
        sc.op("dve", lambda E, cs=cs, m=m, t0=t0: E.scalar_tensor_tensor(
            out=cs["t1"], in0=uT[:, m, t0 - 1:t0 + 511], scalar=convC[:, 1, m:m + 1], in1=cs["t0"],
            op0=ALU.mult, op1=ALU.add), r=ukeys + ["t0" + sfx, "gains"], w=["t1" + sfx])
        sc.op("dve", lambda E, cs=cs, m=m, t0=t0: E.scalar_tensor_tensor(
            out=cs["t0"], in0=uT[:, m, t0 - 2:t0 + 510], scalar=convC[:, 0, m:m + 1], in1=cs["t1"],
            op0=ALU.mult, op1=ALU.add), r=ukeys + ["t1" + sfx, "gains"], w=["t0" + sfx])
        sc.op("dve", lambda E, cs=cs, bB=bB: E.tensor_tensor(
            out=cs["yc"], in0=bank(bB), in1=cs["t0"], op=ALU.mult),
            r=[f"ps{bB}", "t0" + sfx], w=["yc" + sfx])
        sc.op("act", lambda E, cs=cs: E.activation(out=cs["sq"], in_=cs["yc"], func=AF.Square),
              r=["yc" + sfx], w=["sq" + sfx])
        sc.op("pe", lambda E, cs=cs, bG=bG: E.matmul(out=bank(bG), lhsT=bdiagB, rhs=cs["sq"],
                                                   start=True, stop=True),
              r=["sq" + sfx, "consts"], w=[f"ps{bG}"])
        sc.op("act", lambda E, cs=cs, bG=bG: E.activation(out=cs["rs"], in_=bank(bG), func=AF.Ln,
                                                         scale=1.0 / 64, bias=EPS),
              r=[f"ps{bG}"], w=["rs" + sfx])
        sc.op("act", lambda E, cs=cs: E.activation(out=cs["rs"], in_=cs["rs"], func=AF.Exp, scale=-0.5),
              r=["rs" + sfx], w=["rs" + sfx])
        sc.op("dve", lambda E, cs=cs, m=m, c=c: E.scalar_tensor_tensor(
            out=ymix[:, m, c * 512:(c + 1) * 512], in0=cs["yc"], scalar=gconvC[:, m:m + 1], in1=cs["rs"],
            op0=ALU.mult, op1=ALU.mult), r=["yc" + sfx, "rs" + sfx, "gains"], w=[f"ym{m}.{c}"])

    conv_its = [(c, m) for c in range(4) for m in range(4)]
    conv_p1(0, *conv_its[0])
    for it, (c, m) in enumerate(conv_its):
        if it + 1 < len(conv_its):
            conv_p1(it + 1, *conv_its[it + 1])
        conv_p2(it, c, m)

    cset_keys = [nm + f"c{j}" for j in range(2) for nm in ("tc", "t0", "t1", "yc", "sq", "rs")]

    def ld_wo(E):
        return [E.dma_start(out=wo_sl, in_=w_out.rearrange("(k p) f -> p k f", p=128))]
    sc.op("pool", ld_wo, w=["wo_sl"] + cset_keys, dma=("wo_sl", 1))

    conv_keys = ["xn0", "xn1"] + [f"u{m}.{c}" for m in range(4) for c in range(4)] + cset_keys
    BY = 6
    BG2 = 7
    first_att = [True]
    pending_gn = [None]
    for hp in range(4):
        for c in range(4):
            nkb = 4 * c + 4

            def emit_z(step, hp=hp, c=c, nkb=nkb):
                kb = nkb - 1 - step
                c0 = 128 * max(kb - 4 * c, 0)
                for hh in range(2):
                    P0 = 64 * hh
                    bz = 2 * (step % 2) + hh

                    r_ = kb - 4 * c

                    def zmm(E, P0=P0, bz=bz, kb=kb, c0=c0, r_=r_):
                        r = [E.matmul(out=bank(bz)[:, c0:512], lhsT=kT[P0:P0 + 64, hp, kb * 128:(kb + 1) * 128],
                                      rhs=qT[P0:P0 + 64, hp, c * 512 + c0:(c + 1) * 512], start=True, stop=(r_ < 0))]
                        if r_ >= 0:
                            r.append(E.matmul(out=bank(bz)[:, c0:512], lhsT=identB, rhs=maskB[:, r_, c0:512],
                                              start=False, stop=True))
                        return r
                    sc.op("pe", zmm, r=[f"kT{hp}.{kb // 4}", f"qT{hp}.{c}", "consts", "maskc"], w=[f"ps{bz}"])

            extra_w = conv_keys if first_att[0] else []
            first_att[0] = False
            sc.op("dve", lambda E: [E.memset(AT["sacc"][0], 0.0), E.memset(AT["sacc"][1], 0.0)],
                  w=["sacc0", "sacc1"] + extra_w)
            def stageA(step, c=c, nkb=nkb, extra_w=extra_w):
                kb = nkb - 1 - step
                r_ = kb - 4 * c
                par = step % 2
                c0 = 128 * max(r_, 0)
                tg = f"p{par}"
                e3 = step % 3
                ek = f"eb{e3}"
                zp = psum[:, 2 * par:2 * par + 2, c0:512]
                sc.op("act", lambda E: E.activation(out=AT["e"][e3][:, :, c0:512], in_=zp, func=AF.Exp),
                      r=[f"ps{2 * par}", f"ps{2 * par + 1}"], w=[ek] + extra_w)
                sc.op("act", lambda E: E.activation(out=AT["sp"][par][:, :, c0:512], in_=AT["e"][e3][:, :, c0:512],
                                                    func=AF.Ln, bias=1.0), r=[ek], w=["sp" + tg])

            def stageB(step, c=c, nkb=nkb):
                kb = nkb - 1 - step
                par = step % 2
                c0 = 128 * max(kb - 4 * c, 0)
                tg = f"p{par}"
                for hh in range(2):
                    br = 4 + hh

                    def rmm(E, hh=hh, br=br):
                        r = [E.matmul(out=bank(br)[:, c0:512], lhsT=ntriB, rhs=AT["sp"][par][:, hh, c0:512], start=True,
                                      stop=(step == 0))]
                        if step > 0:
                            r.append(E.matmul(out=bank(br)[:, c0:512], lhsT=nonesB,
                                              rhs=AT["sacc"][(step - 1) % 2][:, hh, c0:512], start=False, stop=True))
                        return r
                    rk = ["sp" + tg, "consts"] + ([f"sacc{(step - 1) % 2}"] if step > 0 else [])
                    sc.op("pe", rmm, r=rk, w=[f"ps{br}"])
                if step < nkb - 1:
                    sc.op("dve", lambda E: E.tensor_tensor(
                        out=AT["sacc"][step % 2][:, :, c0:512], in0=AT["sacc"][(step - 1) % 2][:, :, c0:512],
                        in1=AT["sp"][par][:, :, c0:512], op=ALU.add),
                        r=["sp" + tg, f"sacc{(step - 1) % 2}"], w=[f"sacc{step % 2}"])

            def stageC1(step, c=c, nkb=nkb):
                kb = nkb - 1 - step
                par = step % 2
                c0 = 128 * max(kb - 4 * c, 0)
                tg = f"p{par}"
                if c0 > 0:
                    sc.op("dve", lambda E: E.memset(AT["p"][par][:, :, 0:c0], 0.0), w=["pl" + tg])
                sc.op("act", lambda E: E.activation(out=AT["p"][par][:, :, c0:512], in_=psum[:, 4:6, c0:512], func=AF.Exp),
                      r=["ps4", "ps5"], w=["p" + tg])
                e3 = step % 3
                sc.op("dve", lambda E: E.tensor_tensor(
                    out=AT["p"][par][:, :, c0:512], in0=AT["p"][par][:, :, c0:512], in1=AT["e"][e3][:, :, c0:512],
                    op=ALU.mult), r=["p" + tg, f"eb{e3}"], w=["p" + tg])

            def stageC2(step, hp=hp, c=c, nkb=nkb):
                kb = nkb - 1 - step
                par = step % 2
                c0 = 128 * max(kb - 4 * c, 0)
                tg = f"p{par}"
                for hh in range(2):
                    P0 = 64 * hh

                    def ymm(E, P0=P0, hh=hh):
                        h = 2 * hp + hh
                        return E.matmul(out=bank(BY)[P0:P0 + 64, :], lhsT=vtok[:, kb, h * 64:(h + 1) * 64],
                                        rhs=AT["p"][par][:, hh, :], start=(step == 0), stop=(step == nkb - 1))
                    sc.op("pe", ymm, r=["p" + tg, "pl" + tg, f"v{kb}"], w=[f"ps{BY}.{hh}"])

            emit_z(0)
            emit_z(1)
            stageA(0)
            stageB(0)
            if pending_gn[0] is not None:
                pending_gn[0]()
                pending_gn[0] = None
            for step in range(nkb):
                if step + 1 < nkb:
                    stageA(step + 1)
                if step + 2 < nkb:
                    emit_z(step + 2)
                stageC1(step)
                if step + 1 < nkb:
                    stageB(step + 1)
                stageC2(step)
            def gn_ops(hp=hp, c=c):
                sc.op("dve", lambda E: E.tensor_copy(out=gn["yraw"], in_=bank(BY)),
                      r=[f"ps{BY}.0", f"ps{BY}.1"], w=["gyraw"])
                sc.op("act", lambda E: E.activation(out=gn["sq"], in_=gn["yraw"], func=AF.Square),
                      r=["gyraw"], w=["gsq"])
                sc.op("pe", lambda E: E.matmul(out=bank(BG2), lhsT=bdiagB, rhs=gn["sq"], start=True, stop=True),
                      r=["gsq", "consts"], w=[f"ps{BG2}"])
                sc.op("act", lambda E: E.activation(out=gn["rs"], in_=bank(BG2), func=AF.Ln, scale=1.0 / 64, bias=EPS),
                      r=[f"ps{BG2}"], w=["grs"])
                sc.op("act", lambda E: E.activation(out=gn["rs"], in_=gn["rs"], func=AF.Exp, scale=-0.5),
                      r=["grs"], w=["grs"])
                sc.op("dve", lambda E, hp=hp, c=c: E.scalar_tensor_tensor(
                    out=ymix[:, 4 + hp, c * 512:(c + 1) * 512], in0=gn["yraw"], scalar=gsbC[:, hp:hp + 1], in1=gn["rs"],
                    op0=ALU.mult, op1=ALU.mult), r=["gyraw", "grs", "gains"], w=[f"ym{4 + hp}.{c}"])
            pending_gn[0] = gn_ops

    pending_gn[0]()

    all_h = [f"hT{i}a" for i in range(NT)] + [f"hT{i}b" for i in range(NT)]
    for i in range(NT):
        over = []
        if i < 8:
            over = all_h
        elif i < 14:
            over = [f"wsl{(i - 8) // 2}"]
        else:
            over = [f"xs{i - 14}"]

        def ldx(E, i=i):
            return [E.dma_start(out=X[:, i, :], in_=x[i * 128:(i + 1) * 128, :])]
        sc.op("sp", ldx, w=[f"X{i}"] + over, dma=(f"X{i}", 1))
    ymk = [f"ym{m}.{c}" for m in range(8) for c in range(4)]
    for i in range(NT):
        for hh in range(2):
            b = nextbank(6, 8)

            def mm(E, i=i, hh=hh, b=b):
                r = []
                for kc in range(8):
                    r.append(E.matmul(out=bank(b), lhsT=ymix[:, kc, i * 128:(i + 1) * 128],
                                      rhs=wo_sl[:, kc, hh * 512:(hh + 1) * 512], start=(kc == 0), stop=(kc == 7)))
                return r
            sc.op("pe", mm, r=[f"ym{m}.{i // 4}" for m in range(8)] + ["wo_sl"], w=[f"ps{b}"])
            sc.op("dve", lambda E, i=i, hh=hh, b=b: E.tensor_tensor(
                out=X[:, i, hh * 512:(hh + 1) * 512], in0=bank(b), in1=X[:, i, hh * 512:(hh + 1) * 512], op=ALU.add),
                r=[f"ps{b}", f"X{i}"], w=[f"X{i}"])

    KQ = [f"qT{m}.{c}" for m in range(4) for c in range(4)]
    KK = [f"kT{m}.{c}" for m in range(4) for c in range(4)]
    KV = [f"v{i}" for i in range(NT)]
    KATT = ["plp0", "plp1", "eb0", "eb1", "eb2", "spp0", "spp1", "pp0", "pp1", "sacc0", "sacc1", "gyraw", "gsq", "grs"]

    def norm_transpose(tag, src, src_keys, nbuf, nbuf_key, nbuf_extra, ss_col, rs_col, gC, dst_of_kc, dkey_a, dkey_b,
                       dst_extra, banks=None, defer=False):
        sc.op("act", lambda E: E.activation(out=junk2, in_=src, func=AF.Square, accum_out=ss_col),
              r=src_keys, w=["ss" + tag])
        sc.op("act", lambda E: E.activation(out=rs_col, in_=ss_col, func=AF.Ln, scale=1.0 / D, bias=EPS),
              r=["ss" + tag], w=["rs" + tag])
        sc.op("act", lambda E: E.activation(out=rs_col, in_=rs_col, func=AF.Exp, scale=-0.5),
              r=["rs" + tag], w=["rs" + tag])
        sc.op("dve", lambda E: E.tensor_scalar(out=nbuf, in0=src, scalar1=rs_col, scalar2=None, op0=ALU.mult),
              r=src_keys + ["rs" + tag], w=[nbuf_key] + nbuf_extra)
        if defer:
            return lambda: _norm_part2(nbuf, nbuf_key, gC, dst_of_kc, dkey_a, dkey_b, dst_extra, banks)
        _norm_part2(nbuf, nbuf_key, gC, dst_of_kc, dkey_a, dkey_b, dst_extra, banks)

    def _norm_part2(nbuf, nbuf_key, gC, dst_of_kc, dkey_a, dkey_b, dst_extra, banks):
        ba, bb = banks if banks is not None else (nextbank(0, 8), nextbank(0, 8))

        def tr(E):
            r = []
            for kc in range(8):
                bk = ba if kc < 4 else bb
                r.append(E.transpose(out=bank(bk)[:, (kc % 4) * 128:(kc % 4 + 1) * 128],
                                     in_=nbuf[:, kc * 128:(kc + 1) * 128], identity=identF))
            return r
        sc.op("pe", tr, r=[nbuf_key, "consts"], w=[f"ps{ba}", f"ps{bb}"])

        def ev0(E):
            return [E.tensor_scalar(out=dst_of_kc(kc), in0=bank(ba)[:, kc * 128:(kc + 1) * 128],
                                    scalar1=gC[:, kc:kc + 1], scalar2=None, op0=ALU.mult) for kc in range(4)]
        sc.op("dve", ev0, r=[f"ps{ba}", "gains"], w=[dkey_a] + dst_extra)

        def ev1(E):
            return [E.activation(out=dst_of_kc(kc), in_=bank(bb)[:, (kc - 4) * 128:(kc - 3) * 128],
                                 func=AF.Identity, scale=gC[:, kc:kc + 1]) for kc in range(4, 8)]
        sc.op("act", ev1, r=[f"ps{bb}", "gains"], w=[dkey_b] + dst_extra)

    if stop_after != "A":
        wkv = carve(76 * K, 32 * K, BF16, "p (k f) -> p k f", k=8)
        wq = carve(76 * K, 16 * K, BF16, "p (k f) -> p k f", k=8)
        wo = carve(92 * K, 16 * K, BF16, "p (k f) -> p k f", k=8)
        memnT = carve(108 * K, 4 * K, BF16, "p (k t) -> p k t", k=8)
        kTx = carve(112 * K, 4 * K, BF16, "p (k t) -> p k t", k=8)
        vx = carve(116 * K, 4 * K, BF16, "p (j f) -> p j f", j=2)
        h2T = carve(124 * K, 32 * K, BF16, "p (k t) -> p k t", k=8)
        xm = [carve(156 * K + j * 4 * K, 4 * K, F32) for j in range(2)]
        qTx = [carve(164 * K + j * 8 * K, 8 * K, BF16, "p (k t) -> p k t", k=8) for j in range(2)]
        oTx = [carve(180 * K + j * 8 * K, 8 * K, BF16, "p (k t) -> p k t", k=8) for j in range(2)]
        ET = [carve(196 * K + j * 2 * K, 2 * K, BF16, "p (m t) -> p m t", m=2) for j in range(2)]
        rden = [carve(200 * K + j * 2 * K, 2 * K, F32) for j in range(2)]
        junk2 = carve(204 * K, 2 * K, BF16)

        sc.op("pool", lambda E: [E.dma_start(out=wkv, in_=w_kv_mem.rearrange("(k p) f -> p k f", p=128))],
              w=["wkv"] + KQ + KK, dma=("wkv", 1))
        for j in range(2):
            sc.op("sp", lambda E, j=j: [E.dma_start(out=xm[j], in_=mem[j * 128:(j + 1) * 128, :])],
                  w=[f"xm{j}"] + KATT + ["wo_sl"], dma=(f"xm{j}", 1))
            norm_transpose(f"M{j}", xm[j], [f"xm{j}"], xm[j], f"xm{j}", [], ssM[:, j:j + 1], rsM[:, j:j + 1], gmemC,
                           lambda kc, j=j: memnT[:, kc, j * 128:(j + 1) * 128], f"memnT{j}a", f"memnT{j}b", KV)
        KMEM = [f"memnT{j}{ab}" for j in range(2) for ab in "ab"]
        for fc in range(8):
            b = nextbank(0, 8)

            def mm(E, fc=fc, b=b):
                return [E.matmul(out=bank(b)[:, 0:256], lhsT=wkv[:, kc, fc * 128:(fc + 1) * 128], rhs=memnT[:, kc, :],
                                 start=(kc == 0), stop=(kc == 7)) for kc in range(8)]
            sc.op("pe", mm, r=KMEM + ["wkv"], w=[f"ps{b}"])
            evac(kTx[:, fc, :], bank(b)[:, 0:256], r=[f"ps{b}"], w=[f"kTx{fc}"] + KV)
        for mt in range(2):
            for hh in range(2):
                b = nextbank(0, 8)

                def mm(E, mt=mt, hh=hh, b=b):
                    return [E.matmul(out=bank(b), lhsT=memnT[:, kc, mt * 128:(mt + 1) * 128],
                                     rhs=wkv[:, kc, 1024 + hh * 512:1024 + (hh + 1) * 512],
                                     start=(kc == 0), stop=(kc == 7)) for kc in range(8)]
                sc.op("pe", mm, r=KMEM + ["wkv"], w=[f"ps{b}"])
                evac(vx[:, mt, hh * 512:(hh + 1) * 512], bank(b), r=[f"ps{b}"], w=[f"vx{mt}.{hh}"] + KV)
        KKX = [f"kTx{fc}" for fc in range(8)]
        KVX = [f"vx{mt}.{hh}" for mt in range(2) for hh in range(2)]
        sc.op("pool", lambda E: [E.dma_start(out=wq, in_=w_q_mem.rearrange("(k p) f -> p k f", p=128))],
              w=["wq", "wkv"], dma=("wq", 1))
        sc.op("pool", lambda E: [E.dma_start(out=wo, in_=w_o_mem.rearrange("(k p) f -> p k f", p=128))],
              w=["wo", "wkv"], dma=("wo", 1))
        def bnorm(i):
            return norm_transpose(f"B{i}", X[:, i, :], [f"X{i}"], xm[i % 2], f"xm{i % 2}", [], ssA[:, i:i + 1], rsA[:, i:i + 1],
                                  gxatC, lambda kc, i=i: h2T[:, kc, i * 128:(i + 1) * 128], f"h2T{i}a", f"h2T{i}b",
                                  [f"ym{m}.{i // 4}" for m in range(8)], defer=True)
        p2 = bnorm(0)
        for i in range(NT):
            nxt2 = bnorm(i + 1) if i + 1 < NT else None
            p2()
            p2 = nxt2
        def b_qproj(c):
            cb = c % 2
            h2k = [f"h2T{i}{ab}" for i in range(4 * c, 4 * c + 4) for ab in "ab"]
            for fc in range(8):
                b = nextbank(0, 8)

                def mm(E, fc=fc, c=c, b=b):
                    return [E.matmul(out=bank(b), lhsT=wq[:, kc, fc * 128:(fc + 1) * 128],
                                     rhs=h2T[:, kc, c * 512:(c + 1) * 512], start=(kc == 0), stop=(kc == 7))
                            for kc in range(8)]
                sc.op("pe", mm, r=h2k + ["wq"], w=[f"ps{b}"])
                evac(qTx[cb][:, fc, :], bank(b), r=[f"ps{b}"], w=[f"qTx{cb}.{fc}"], scale=1.0 / 16)

        def b_att(c):
            cb = c % 2
            for h in range(4):
                eb = h % 2
                for mt in range(2):
                    b = nextbank(0, 8)

                    def mm(E, h=h, mt=mt, b=b, cb=cb):
                        return [E.matmul(out=bank(b), lhsT=kTx[:, 2 * h + dc, mt * 128:(mt + 1) * 128],
                                         rhs=qTx[cb][:, 2 * h + dc, :], start=(dc == 0), stop=(dc == 1))
                                for dc in range(2)]
                    sc.op("pe", mm, r=KKX + [f"qTx{cb}.{2 * h}", f"qTx{cb}.{2 * h + 1}"], w=[f"ps{b}"])
                    sc.op("act", lambda E, eb=eb, mt=mt, b=b: E.activation(out=ET[eb][:, mt, :], in_=bank(b), func=AF.Exp),
                          r=[f"ps{b}"], w=[f"ET{eb}.{mt}"])
                bd = nextbank(0, 8)
                sc.op("pe", lambda E, eb=eb, bd=bd: [E.matmul(out=bank(bd), lhsT=onesB, rhs=ET[eb][:, mt, :],
                                                             start=(mt == 0), stop=(mt == 1)) for mt in range(2)],
                      r=[f"ET{eb}.0", f"ET{eb}.1", "consts"], w=[f"ps{bd}"])
                sc.op("dve", lambda E, eb=eb, bd=bd: E.reciprocal(out=rden[eb], in_=bank(bd)),
                      r=[f"ps{bd}"], w=[f"rden{eb}"])
                for dc in range(2):
                    b = nextbank(0, 8)
                    sc.op("pe", lambda E, eb=eb, b=b, h=h, dc=dc: [
                        E.matmul(out=bank(b), lhsT=vx[:, mt, (2 * h + dc) * 128:(2 * h + dc + 1) * 128],
                                 rhs=ET[eb][:, mt, :], start=(mt == 0), stop=(mt == 1)) for mt in range(2)],
                        r=[f"ET{eb}.0", f"ET{eb}.1"] + KVX, w=[f"ps{b}"])
                    sc.op("dve", lambda E, eb=eb, b=b, h=h, dc=dc, cb=cb: E.tensor_tensor(
                        out=oTx[cb][:, 2 * h + dc, :], in0=bank(b), in1=rden[eb], op=ALU.mult),
                        r=[f"ps{b}", f"rden{eb}"], w=[f"oTx{cb}.{2 * h + dc}"])

        def b_oproj(c):
            cb = c % 2
            for ii in range(4):
                i = 4 * c + ii
                for hh in range(2):
                    b = nextbank(0, 8)
                    sc.op("pe", lambda E, ii=ii, hh=hh, b=b, cb=cb: [
                        E.matmul(out=bank(b), lhsT=oTx[cb][:, fc, ii * 128:(ii + 1) * 128],
                                 rhs=wo[:, fc, hh * 512:(hh + 1) * 512], start=(fc == 0), stop=(fc == 7))
                        for fc in range(8)],
                        r=[f"oTx{cb}.{fc}" for fc in range(8)] + ["wo"], w=[f"ps{b}"])
                    sc.op("dve", lambda E, i=i, hh=hh, b=b: E.tensor_tensor(
                        out=X[:, i, hh * 512:(hh + 1) * 512], in0=bank(b), in1=X[:, i, hh * 512:(hh + 1) * 512],
                        op=ALU.add), r=[f"ps{b}", f"X{i}"], w=[f"X{i}"])

        b_qproj(0)
        for c in range(4):
            b_att(c)
            if c + 1 < 4:
                b_qproj(c + 1)
            b_oproj(c)

    if stop_after in ("A", "B"):
        for i in range(NT):
            def st(E, i=i):
                return [E.dma_start(out=out[i * 128:(i + 1) * 128, :], in_=X[:, i, :])]
            sc.op("sp", st, r=[f"X{i}"], w=[f"out{i}"], dma=("out", 1))
    else:
        Xs_d = nc.dram_tensor("xs_scratch", [NE * CAP, D], BF16, kind="Internal").ap()
        Ys_d = nc.dram_tensor("ys_scratch", [NE * CAP, D], BF16, kind="Internal").ap()
        BIG = 1.0e6
        bc_reg = nc.gpsimd.to_reg(NE * CAP - 1)
        ring = [carve(76 * K + j * 16 * K, 16 * K, BF16, "p (k f) -> p k f", k=8) for j in range(5)]
        T0 = 156 * K
        idxT = carve(T0 + 44 * K, 256, I32, "p (i k) -> p i k", i=NT) if SCAT_TEST else None
        if SCAT_TEST:
            sc.op("pool", lambda E: E.iota(idxT, pattern=[[512, 16], [128, 4]], base=0, channel_multiplier=1), w=["idxT"])
        xn3 = [carve(T0 + j * 4 * K, 4 * K, F32) for j in range(2)]
        h3b = [carve(T0 + 8 * K + j * 2 * K, 2 * K, BF16) for j in range(4)]
        h3T = [carve(T0 + 16 * K + j * 4 * K, 4 * K, F32, "p (k t) -> p k t", k=8) for j in range(2)]
        o = T0 + 24 * K
        sm = {}
        for nm, nb, dt in (("lg", 128, F32), ("max8", 32, F32), ("idx8", 32, U32), ("idxf", 16, F32), ("nmax", 4, F32),
                           ("ex4", 16, F32), ("sum4", 4, F32), ("posS", 128, F32), ("oh", 128, F32), ("jr", 128, F32),
                           ("pk", 16, F32), ("okf", 16, F32), ("destf", 16, F32), ("Gd", 128, F32), ("GT", 512, F32)):
            sm[nm] = [carve(o + j * nb, nb, dt) for j in range(4)]
            o += 4 * nb
        o = (o + 63) // 64 * 64
        mskB = carve(o, 1024, BF16, "p (i e) -> p i e", i=NT); o += 1024
        b1st = carve(o, 8 * K, F32); o += 8 * K
        assert o <= ARENA
        xstage = [carve(T0 + j * 6 * K, 6 * K, BF16, "p (j d) -> p j d", j=3) for j in range(2)]
        XsT = [carve(T0 + 12 * K + j * 6 * K, 6 * K, BF16, "p (k t) -> p k t", k=8) for j in range(2)]
        actT = carve(T0 + 24 * K, 6 * K, BF16, "p (k t) -> p k t", k=8)
        o = T0 + 30 * K
        hset = []
        for j in range(2):
            d = {}
            for nm in ("g", "sig", "l1", "gs"):
                d[nm] = carve(o, 1536, F32); o += 1536
            hset.append(d)
        ystage = carve(o, 6 * K, BF16, "p (j d) -> p j d", j=3); o += 6 * K
        assert o <= ARENA
        grow = [[carve(T0 + (j * 4 + k) * 2 * K, 2 * K, BF16) for k in range(4)] for j in range(2)]
        acc = [carve(T0 + 16 * K + j * 4 * K, 4 * K, F32) for j in range(2)]
        ostage = [carve(T0 + 24 * K + j * 4 * K, 4 * K, F32) for j in range(2)]
        gfin = carve(T0 + 32 * K, 4 * K, F32)
        junk3 = carve(T0 + 36 * K, 2 * K, BF16)
        dg = [[carve(T0 + 38 * K + (j * 4 + k) * 256, 256, BF16) for k in range(4)] for j in range(2)]

        KB_ALL = ["wq", "wo", "wkv"] + KMEM + KKX + KVX + [f"h2T{i}{ab}" for i in range(NT) for ab in "ab"]
        KB_T = ["xm0", "xm1"] + [f"qTx{j}.{fc}" for j in range(2) for fc in range(8)] + \
            [f"oTx{j}.{fc}" for j in range(2) for fc in range(8)] + [f"ET{j}.{m}" for j in range(2) for m in range(2)] + \
            ["rden0", "rden1"]

        pieces = []
        for e in range(NE_RUN):
            pieces += [(e, "g"), (e, "l"), (e, "w2")]
        piece_slot = {}
        state = {"next": 0}

        def issue_piece():
            n = state["next"]
            if n >= len(pieces):
                return
            state["next"] += 1
            e, kind = pieces[n]
            slot = n % 5
            piece_slot[(e, kind)] = slot
            if kind == "g":
                src = w1[e, :, 0:1024]
            elif kind == "l":
                src = w1[e, :, 1024:2048]
            else:
                src = w2[e, :, :]
            extra = KB_ALL if n < 5 else []
            sc.op("pool", lambda E, slot=slot, src=src: [E.dma_start(out=ring[slot], in_=src.rearrange("(k p) f -> p k f", p=128))],
                  w=[f"ring{slot}"] + extra, dma=(f"ring{slot}", 1))

        if stop_after != "C1":
            for _ in range(5):
                issue_piece()

        def ld_moe_consts(E):
            r = []
            r.append(E.dma_start(out=wr, in_=w_router.rearrange("(k p) e -> p k e", p=128)))
            r.append(E.dma_start(out=brB, in_=b_router.partition_broadcast(128).rearrange("p o e -> p (o e)")))
            r.append(E.dma_start(out=b2S[0:32, :], in_=b2))
            r.append(E.dma_start(out=b1st[0:32, :], in_=b1))
            return r
        sc.op("sp", ld_moe_consts, w=["moec", "maskc", "b1st"] + KB_T, dma=("moec", 4))
        bt = nextbank(0, 8)
        sc.op("pe", lambda E: [E.transpose(out=bank(bt)[:, j * 32:(j + 1) * 32], in_=b1st[0:32, j * 128:(j + 1) * 128],
                                           identity=identF[0:32, 0:32]) for j in range(16)],
              r=["b1st", "consts"], w=[f"ps{bt}"])
        sc.op("dve", lambda E: E.tensor_copy(out=b1T, in_=bank(bt).rearrange("p (j e) -> p j e", j=16)),
              r=[f"ps{bt}"], w=["b1T", "iota"])
        sc.op("dve", lambda E: E.tensor_scalar(out=b1T[:, 8:16, :], in0=b1T[:, 8:16, :], scalar1=1.0, scalar2=None,
                                               op0=ALU.add), r=["b1T"], w=["b1T"])

        XSK = []
        def c1_tile(i):
            s_ = i % 2
            s4 = i % 4
            t = {k: v[s4] for k, v in sm.items()}
            tg = f"r{s4}"
            first = KB_T if i < 4 else []
            norm_transpose(f"C{i}", X[:, i, :], [f"X{i}"], xn3[s_], f"xn3{s_}", first, ssC[:, i:i + 1], rsC[:, i:i + 1],
                           gmoeC, lambda kc, s_=s_: h3T[s_][:, kc, :], f"h3T{s_}a", f"h3T{s_}b", first, banks=(2 * s4, 2 * s4 + 1))
            sc.op("dve", lambda E, s_=s_, s4=s4: E.tensor_copy(out=h3b[s4], in_=xn3[s_]), r=[f"xn3{s_}"], w=[f"h3b{s4}"] + first)
            bl = 2 * s4
            sc.op("pe", lambda E, s_=s_, bl=bl: [E.matmul(out=bank(bl)[:, 0:32], lhsT=h3T[s_][:, kc, :], rhs=wr[:, kc, :],
                                                         start=(kc == 0), stop=(kc == 7)) for kc in range(8)],
                  r=[f"h3T{s_}a", f"h3T{s_}b", "moec"], w=[f"ps{bl}"])
            yield
            sc.op("dve", lambda E, t=t, bl=bl: E.tensor_tensor(out=t["lg"], in0=bank(bl)[:, 0:32], in1=brB, op=ALU.add),
                  r=[f"ps{bl}", "moec"], w=["lg" + tg] + first)
            if C1_LEVEL >= 1:
                yield
                sc.op("dve", lambda E, t=t: E.max(out=t["max8"], in_=t["lg"]), r=["lg" + tg], w=["max8" + tg])
                yield
                sc.op("dve", lambda E, t=t: E.max_index(out=t["idx8"], in_max=t["max8"], in_values=t["lg"]),
                      r=["lg" + tg, "max8" + tg], w=["idx8" + tg])
                yield
                sc.op("dve", lambda E, t=t: E.tensor_scalar(out=t["nmax"], in0=t["max8"][:, 0:1], scalar1=-1.0, scalar2=None,
                                                            op0=ALU.mult), r=["max8" + tg], w=["nmax" + tg])
                yield
                sc.op("act", lambda E, t=t: E.activation(out=t["ex4"], in_=t["max8"][:, 0:4], func=AF.Exp, bias=t["nmax"],
                                                         accum_out=t["sum4"]),
                      r=["max8" + tg, "nmax" + tg], w=["ex4" + tg, "sum4" + tg])
                yield
                sc.op("dve", lambda E, t=t: E.reciprocal(out=t["sum4"], in_=t["sum4"]), r=["sum4" + tg], w=["sum4" + tg])
                yield
                sc.op("dve", lambda E, t=t, i=i: E.tensor_scalar(out=gatesA[:, i, :], in0=t["ex4"], scalar1=t["sum4"],
                                                                 scalar2=None, op0=ALU.mult),
                      r=["ex4" + tg, "sum4" + tg], w=[f"gate{i}"])
                yield
                sc.op("dve", lambda E, t=t, i=i: E.tensor_scalar(out=mskB[:, i, :], in0=t["lg"], scalar1=t["max8"][:, 3:4],
                                                                 scalar2=None, op0=ALU.is_ge),
                      r=["lg" + tg, "max8" + tg], w=[f"msk{i}"])
            if C1_LEVEL >= 2:
                bp = 2 * s4 + 1
                yield
                sc.op("pe", lambda E, i=i, bp=bp: [E.matmul(out=bank(bp)[:, 0:32], lhsT=triS, rhs=mskB[:, i, :], start=True,
                                                            stop=(i == 0))] +
                      [E.matmul(out=bank(bp)[:, 0:32], lhsT=onesB, rhs=mskB[:, i2, :], start=False, stop=(i2 == i - 1))
                       for i2 in range(i)],
                      r=[f"msk{i2}" for i2 in range(i + 1)] + ["consts"], w=[f"ps{bp}"])
                yield
                sc.op("dve", lambda E, t=t, bp=bp: E.tensor_copy(out=t["posS"], in_=bank(bp)[:, 0:32]),
                      r=[f"ps{bp}"], w=["posS" + tg])
                yield
                sc.op("dve", lambda E, t=t: E.tensor_copy(out=t["idxf"], in_=t["idx8"][:, 0:4]), r=["idx8" + tg], w=["idxf" + tg])
                for k in range(4):
                    yield
                    sc.op("dve", lambda E, t=t, k=k: E.scalar_tensor_tensor(
                        out=t["jr"], in0=iotaE, scalar=t["idxf"][:, k:k + 1], in1=t["posS"], op0=ALU.is_equal, op1=ALU.mult,
                        accum_out=t["pk"][:, k:k + 1]), r=["idxf" + tg, "posS" + tg, "consts"], w=["jr" + tg, f"pk{k}" + tg])
                yield
                sc.op("act", lambda E, t=t: E.activation(out=t["oh"], in_=t["lg"], func=AF.Exp, bias=t["nmax"]),
                      r=["lg" + tg, "nmax" + tg], w=["oh" + tg])
                yield
                sc.op("dve", lambda E, t=t: E.tensor_scalar(out=t["Gd"], in0=t["lg"], scalar1=t["max8"][:, 3:4], scalar2=None,
                                                            op0=ALU.is_ge), r=["lg" + tg, "max8" + tg], w=["Gd" + tg])
                yield
                sc.op("dve", lambda E, t=t: E.scalar_tensor_tensor(out=t["Gd"], in0=t["oh"], scalar=t["sum4"], in1=t["Gd"],
                                                                   op0=ALU.mult, op1=ALU.mult),
                      r=["oh" + tg, "sum4" + tg, "Gd" + tg], w=["Gd" + tg])
                pkk = [f"pk{k}" + tg for k in range(4)]
                yield
                sc.op("dve", lambda E, t=t: E.tensor_scalar(out=t["okf"], in0=t["pk"], scalar1=float(CAP), scalar2=None,
                                                            op0=ALU.is_lt), r=pkk, w=["okf" + tg])
                yield
                sc.op("dve", lambda E, t=t: E.scalar_tensor_tensor(out=t["destf"], in0=t["idxf"], scalar=float(CAP), in1=t["pk"],
                                                                   op0=ALU.mult, op1=ALU.add),
                      r=pkk + ["idxf" + tg], w=["destf" + tg])
                yield
                sc.op("dve", lambda E, t=t: E.scalar_tensor_tensor(out=t["destf"], in0=t["destf"], scalar=-BIG, in1=t["okf"],
                                                                   op0=ALU.add, op1=ALU.mult),
                      r=["destf" + tg, "okf" + tg], w=["destf" + tg])
                yield
                sc.op("dve", lambda E, t=t: E.tensor_scalar(out=t["destf"], in0=t["destf"], scalar1=BIG, scalar2=None,
                                                            op0=ALU.add), r=["destf" + tg], w=["destf" + tg])
                yield
                sc.op("dve", lambda E, t=t, i=i: E.tensor_copy(out=idxA[:, i, :], in_=t["destf"]),
                      r=["destf" + tg], w=[f"idx{i}"])
                yield
                sc.op("dve", lambda E, t=t, i=i: E.tensor_copy(out=t["jr"][:, 0:4], in_=idxA[:, i, :]),
                      r=[f"idx{i}"], w=["jr" + tg])
                yield
                sc.op("dve", lambda E, t=t, i=i: E.tensor_copy(out=t["jr"][:, 4:8], in_=t["jr"][:, 0:4]),
                      r=["jr" + tg], w=[f"idxfence{i}"])
            if C1_LEVEL >= 3:
                bg = 2 * s4
                yield
                sc.op("pe", lambda E, t=t, bg=bg: E.transpose(out=bank(bg)[0:32, 0:128], in_=t["Gd"], identity=identF),
                      r=["Gd" + tg, "consts"], w=[f"ps{bg}"])
                yield
                sc.op("act", lambda E, t=t, bg=bg: E.activation(out=t["GT"][0:32, :], in_=bank(bg)[0:32, 0:128], func=AF.Copy),
                      r=[f"ps{bg}"], w=["GT" + tg])
                for hh in range(2):
                    b = 2 * s4 + 1 - hh
                    yield
                    sc.op("pe", lambda E, t=t, b=b, hh=hh: E.matmul(out=bank(b), lhsT=t["GT"][0:32, :],
                                                                  rhs=b2S[0:32, hh * 512:(hh + 1) * 512], start=True, stop=True),
                          r=["GT" + tg, "moec"], w=[f"ps{b}"])
                    yield
                    sc.op("dve", lambda E, i=i, hh=hh, b=b: E.tensor_tensor(
                        out=X[:, i, hh * 512:(hh + 1) * 512], in0=bank(b), in1=X[:, i, hh * 512:(hh + 1) * 512], op=ALU.add),
                        r=[f"ps{b}", f"X{i}"], w=[f"X{i}"])
            if C1_LEVEL >= 4:
                for k in range(4):
                    def scat(E, i=i, k=k, s4=s4):
                        ia = idxT[:, i, k:k + 1] if SCAT_TEST else idxA[:, i, k:k + 1]
                        return [E.indirect_dma_start(out=Xs_d[:, :], out_offset=bass.IndirectOffsetOnAxis(ap=ia, axis=0),
                                                     in_=h3b[s4], in_offset=None, bounds_check=bc_reg, oob_is_err=False)]
                    yield
                    sc.op("pool", scat, r=[f"idx{i}", f"idxfence{i}", f"h3b{s4}", "idxT"], w=[f"Xs{i}.{k}"], dma=("scat", 1))
                    XSK.append(f"Xs{i}.{k}")
            if C1_LEVEL >= 2:
                yield
                sc.op("dve", lambda E, t=t, i=i: E.tensor_tensor(out=gatesA[:, i, :], in0=gatesA[:, i, :], in1=t["okf"], op=ALU.mult),
                      r=[f"gate{i}", "okf" + tg, "Gd" + tg], w=[f"gate{i}"])


        gens = [c1_tile(i) for i in range(NT)]
        active = []
        nxt = 0
        while nxt < NT or active:
            while len(active) < 4 and nxt < NT:
                active.append(gens[nxt]); nxt += 1
            for g in list(active):
                try:
                    next(g)
                except StopIteration:
                    active.remove(g)

        def phase_C23():
            C1K = ["xn30", "xn31", "h3b0", "h3b1", "h3b2", "h3b3", "h3T0a", "h3T0b", "h3T1a", "h3T1b", "b1st"] + \
                [nm + f"r{j}" for j in range(4) for nm in ("lg", "max8", "idx8", "idxf", "nmax", "ex4", "sum4", "posS", "oh", "jr",
                                                           "pk0", "pk1", "pk2", "pk3", "okf", "destf", "Gd", "GT")] + \
                [f"msk{i}" for i in range(NT)]
            bar_dummy = carve(T0 + 48 * K, 64, F32)
            sc.op("pool", lambda E: E.memset(bar_dummy, 0.0), w=C1K + ["bar1"])

            def load_xs(e):
                s_ = e % 2
                sc.op("act", lambda E, e=e, s_=s_: [E.dma_start(out=xstage[s_], in_=Xs_d[e * CAP:(e + 1) * CAP, :].rearrange(
                    "(j p) d -> p j d", p=128))], r=XSK + ["bar1"], w=[f"xstage{s_}"], dma=(f"xstage{s_}", 1))

            def xs_transposes(e):
                s_ = e % 2
                if C2_LEVEL < 1:
                    return
                for pr in range(4):
                    bk = pr % 2
                    bv = bank(bk).bitcast(BF16)

                    def tr(E, s_=s_, pr=pr, bv=bv):
                        r = []
                        for kl in range(2):
                            kc = 2 * pr + kl
                            for j in range(3):
                                r.append(E.transpose(out=bv[:, kl * 384 + j * 128:kl * 384 + (j + 1) * 128],
                                                     in_=xstage[s_][:, j, kc * 128:(kc + 1) * 128], identity=identB))
                        return r
                    sc.op("pe", tr, r=[f"xstage{s_}", "consts", "bar1"], w=[f"ps{bk}"])
                    for kl in range(2):
                        kc = 2 * pr + kl
                        if C2_SUB < 1 or (C2_SUB < 2 and kl == 1):
                            continue
                        sc.op("dve", lambda E, s_=s_, kc=kc, kl=kl, bv=bv: E.tensor_scalar(
                            out=XsT[s_][:, kc, :], in0=bv[:, kl * 384:(kl + 1) * 384], scalar1=gmoeC[:, kc:kc + 1], scalar2=None,
                            op0=ALU.mult), r=[f"ps{bk}", "gains", "bar1"], w=[f"XsT{s_}.{kc}"])

            if NE_RUN > 0:
                load_xs(0)
                load_xs(1)
                xs_transposes(0)
            YSK = []
            for e in range(NE_RUN):
                s_ = e % 2
                sg, sl, s2 = piece_slot[(e, "g")], piece_slot[(e, "l")], piece_slot[(e, "w2")]
                xk = [f"XsT{s_}.{kc}" for kc in range(8)]
                for j8 in range(8 if C2_LEVEL >= 2 else 0):
                    H = hset[j8 % 2]
                    ht = f"h{j8 % 2}"
                    bG, bL = 2 + 2 * (j8 % 2), 3 + 2 * (j8 % 2)
                    sc.op("pe", lambda E, s_=s_, sg=sg, j8=j8, bG=bG: [
                        E.matmul(out=bank(bG)[:, 0:CAP], lhsT=ring[sg][:, kc, j8 * 128:(j8 + 1) * 128], rhs=XsT[s_][:, kc, :],
                                 start=(kc == 0), stop=(kc == 7)) for kc in range(8)],
                        r=xk + [f"ring{sg}"], w=[f"ps{bG}"])
                    sc.op("pe", lambda E, s_=s_, sl=sl, j8=j8, bL=bL: [
                        E.matmul(out=bank(bL)[:, 0:CAP], lhsT=ring[sl][:, kc, j8 * 128:(j8 + 1) * 128], rhs=XsT[s_][:, kc, :],
                                 start=(kc == 0), stop=(kc == 7)) for kc in range(8)],
                        r=xk + [f"ring{sl}"], w=[f"ps{bL}"])
                    sc.op("dve", lambda E, H=H, e=e, j8=j8, bG=bG: E.tensor_scalar(
                        out=H["g"], in0=bank(bG)[:, 0:CAP], scalar1=b1T[:, j8, e:e + 1], scalar2=7.0, op0=ALU.add, op1=ALU.min),
                        r=[f"ps{bG}", "b1T", "bar1"], w=["g" + ht])
                    sc.op("act", lambda E, H=H: E.activation(out=H["sig"], in_=H["g"], func=AF.Sigmoid, scale=1.702),
                          r=["g" + ht, "bar1"], w=["sig" + ht])
                    sc.op("dve", lambda E, H=H, e=e, j8=j8, bL=bL: E.tensor_scalar(
                        out=H["l1"], in0=bank(bL)[:, 0:CAP], scalar1=b1T[:, 8 + j8, e:e + 1], scalar2=8.0, op0=ALU.add, op1=ALU.min),
                        r=[f"ps{bL}", "b1T", "bar1"], w=["l1" + ht])
                    sc.op("dve", lambda E, H=H: E.tensor_tensor(out=H["gs"], in0=H["g"], in1=H["sig"], op=ALU.mult),
                          r=["g" + ht, "sig" + ht, "bar1"], w=["gs" + ht])
                    sc.op("dve", lambda E, H=H, j8=j8: E.scalar_tensor_tensor(
                        out=actT[:, j8, :], in0=H["l1"], scalar=-6.0, in1=H["gs"], op0=ALU.max, op1=ALU.mult),
                        r=["l1" + ht, "gs" + ht, "bar1"], w=[f"actT{j8}"])
                issue_piece()
                issue_piece()
                if e + 2 < NE_RUN:
                    load_xs(e + 2)
                if e + 1 < NE_RUN:
                    xs_transposes(e + 1)
                for j in range(3 if C2_LEVEL >= 3 else 0):
                    for hh in range(2):
                        bO = 6 + (2 * j + hh) % 2
                        sc.op("pe", lambda E, j=j, hh=hh, bO=bO, s2=s2: [
                            E.matmul(out=bank(bO), lhsT=actT[:, j8, j * 128:(j + 1) * 128], rhs=ring[s2][:, j8, hh * 512:(hh + 1) * 512],
                                     start=(j8 == 0), stop=(j8 == 7)) for j8 in range(8)],
                            r=[f"actT{j8}" for j8 in range(8)] + [f"ring{s2}"], w=[f"ps{bO}"])
                        if hh == 0:
                            sc.op("act", lambda E, j=j, hh=hh, bO=bO: E.activation(out=ystage[:, j, hh * 512:(hh + 1) * 512],
                                                                                  in_=bank(bO), func=AF.Copy),
                                  r=[f"ps{bO}", "bar1"], w=[f"ystage{j}.{hh}"])
                        else:
                            sc.op("dve", lambda E, j=j, hh=hh, bO=bO: E.tensor_copy(out=ystage[:, j, hh * 512:(hh + 1) * 512],
                                                                                   in_=bank(bO)),
                                  r=[f"ps{bO}", "bar1"], w=[f"ystage{j}.{hh}"])
                issue_piece()
                if C2_LEVEL < 3:
                    continue
                sc.op("sp", lambda E, e=e: [E.dma_start(out=Ys_d[e * CAP:(e + 1) * CAP, :].rearrange("(j p) d -> p j d", p=128),
                                                        in_=ystage)],
                      r=[f"ystage{j}.{hh}" for j in range(3) for hh in range(2)], w=[f"Ys{e}"], dma=("ys", 1))
                YSK.append(f"Ys{e}")

            if stop_after == "C2":
                for i in range(NT):
                    sc.op("sp", lambda E, i=i: [E.dma_start(out=out[i * 128:(i + 1) * 128, :], in_=X[:, i, :])],
                          r=[f"X{i}"] + YSK, w=[f"out{i}"], dma=("out", 1))
                return
            C2K = ["xstage0", "xstage1"] + [f"XsT{j}.{kc}" for j in range(2) for kc in range(8)] + [f"actT{j8}" for j8 in range(8)] + \
                [nm + f"h{j}" for j in range(2) for nm in ("g", "sig", "l1", "gs")] + [f"ystage{j}.{hh}" for j in range(3) for hh in range(2)]
            sc.op("sp", lambda E: [E.dma_start(out=gfin, in_=g_final.partition_broadcast(128).rearrange("p o d -> p (o d)"))],
                  w=["gfin"] + C2K, dma=("gfin", 1))
            sc.op("dve", lambda E: [E.memset(grow[j][k], 0.0) for j in range(2) for k in range(4)],
                  r=["gfin"], w=[f"grow{j}.{k}" for j in range(2) for k in range(4)])
            for i in range(NT):
                s_ = i % 2
                for k in range(4):
                    def gath(E, i=i, k=k, s_=s_):
                        return [E.indirect_dma_start(out=grow[s_][k], out_offset=None, in_=Ys_d[:, :],
                                                     in_offset=bass.IndirectOffsetOnAxis(ap=idxA[:, i, k:k + 1], axis=0),
                                                     bounds_check=bc_reg, oob_is_err=False)]
                    sc.op("pool", gath, r=YSK + [f"idx{i}"], w=[f"grow{s_}.{k}"], dma=(f"gath{s_}.{k}", 1))
                for k in range(4):
                    sc.op("act", lambda E, i=i, s_=s_, k=k: E.activation(out=dg[s_][k], in_=identB, func=AF.Identity,
                                                                        scale=gatesA[:, i, k:k + 1]),
                          r=[f"gate{i}", "consts", "gfin"], w=[f"dg{s_}.{k}"])
                for hh in range(2):
                    bq = 2 * s_ + hh
                    sc.op("pe", lambda E, s_=s_, hh=hh, bq=bq: [
                        E.matmul(out=bank(bq), lhsT=dg[s_][k], rhs=grow[s_][k][:, hh * 512:(hh + 1) * 512],
                                 start=(k == 0), stop=(k == 3)) for k in range(4)],
                        r=[f"dg{s_}.{k}" for k in range(4)] + [f"grow{s_}.{k}" for k in range(4)], w=[f"ps{bq}"])
                    sc.op("dve", lambda E, i=i, s_=s_, hh=hh, bq=bq: E.tensor_tensor(
                        out=acc[s_][:, hh * 512:(hh + 1) * 512], in0=bank(bq), in1=X[:, i, hh * 512:(hh + 1) * 512], op=ALU.add),
                        r=[f"ps{bq}", f"X{i}", "gfin"], w=[f"acc{s_}"] if hh == 0 else [f"acc{s_}b"])
                sc.op("act", lambda E, i=i, s_=s_: E.activation(out=junk3, in_=acc[s_], func=AF.Square, accum_out=ssA[:, i:i + 1]),
                      r=[f"acc{s_}", f"acc{s_}b", "gfin"], w=[f"ssF{i}"])
                sc.op("act", lambda E, i=i: E.activation(out=rsA[:, i:i + 1], in_=ssA[:, i:i + 1], func=AF.Ln, scale=1.0 / D, bias=EPS),
                      r=[f"ssF{i}"], w=[f"rsF{i}"])
                sc.op("act", lambda E, i=i: E.activation(out=rsA[:, i:i + 1], in_=rsA[:, i:i + 1], func=AF.Exp, scale=-0.5),
                      r=[f"rsF{i}"], w=[f"rsF{i}"])
                sc.op("dve", lambda E, i=i, s_=s_: E.scalar_tensor_tensor(out=ostage[s_], in0=acc[s_], scalar=rsA[:, i:i + 1], in1=gfin,
                                                                          op0=ALU.mult, op1=ALU.mult),
                      r=[f"acc{s_}", f"acc{s_}b", f"rsF{i}", "gfin"], w=[f"ostage{s_}"])
                sc.op("sp", lambda E, i=i, s_=s_: [E.dma_start(out=out[i * 128:(i + 1) * 128, :], in_=ostage[s_])],
                      r=[f"ostage{s_}"], w=[f"out{i}"], dma=("out", 1))

        if stop_after == "C1":
            dbg = nc.dram_tensor("dbg", [128, 128], F32, kind="ExternalOutput").ap()
            dbgS = carve(T0 + 40 * K, 512, F32)
            sc.op("dve", lambda E: [E.tensor_copy(out=dbgS[:, 0:64], in_=gatesA.rearrange("p i k -> p (i k)")),
                                    E.tensor_copy(out=dbgS[:, 64:128], in_=idxA.rearrange("p i k -> p (i k)"))],
                  r=[f"gate{i}" for i in range(NT)] + [f"idx{i}" for i in range(NT)] + XSK, w=["dbgS"])
            sc.op("sp", lambda E: [E.dma_start(out=dbg, in_=dbgS)], r=["dbgS"], w=["dbgout"], dma=("dbg", 1))
            for i in range(NT):
                sc.op("sp", lambda E, i=i: [E.dma_start(out=out[i * 128:(i + 1) * 128, :], in_=X[:, i, :])],
                      r=[f"X{i}", "dbgout"], w=[f"out{i}"], dma=("out", 1))
        else:
            phase_C23()
    sc.op("sp", lambda E: E.nop(), r=[f"out{i}" for i in range(NT)], w=["done"])
    nw = sc.emit()
    print(f"[kernel] ops={len(sc.ops)} waits={nw}")


_IN_NAMES = ["x", "mem", "g_mix", "w_in", "conv_w", "g_conv_out", "g_sb_out", "w_out", "g_xattn", "g_mem",
             "w_q_mem", "w_kv_mem", "w_o_mem", "g_moe", "w_router", "b_router", "w1", "b1", "w2", "b2", "g_final"]


def kernel(**inputs):
    nc = build_program()
    a = {k: np.ascontiguousarray(np.asarray(v, dtype=np.float32)) for k, v in inputs.items()}
    shared = {}
    for k in _IN_NAMES:
        if k in ("x", "mem"):
            continue
        v = a[k]
        if k == "g_final":
            v = v.reshape(1, D)
        elif v.shape[0] == 1:
            v = v[0]
            if v.ndim == 1:
                v = v.reshape(1, -1)
        shared[k] = np.ascontiguousarray(v)
    in_maps = []
    for c in range(NCORES):
        m = dict(shared)
        m["x"] = np.ascontiguousarray(a["x"][c])
        m["mem"] = np.ascontiguousarray(a["mem"][c])
        in_maps.append(m)
    res = run_bass_kernel_spmd(nc, in_maps, core_ids=list(range(NCORES)))
    return np.stack([np.asarray(r["out"], dtype=np.float32) for r in res.results], axis=0)
```

```python
import contextlib
import numpy as np
import concourse.bass as bass
import concourse.mybir as mybir
from concourse.bass_utils import run_bass_kernel_spmd

F32 = mybir.dt.float32
BF16 = mybir.dt.bfloat16
I32 = mybir.dt.int32
U32 = mybir.dt.uint32
U8 = mybir.dt.uint8
AF = mybir.ActivationFunctionType
ALU = mybir.AluOpType

NCORES = 8
S = 2048
D = 1024
NT = S // 128
MEM = 256
NE = 32
C1_LEVEL = 9
ATT_SPLIT = 0
C2_LEVEL = 9
C2_SUB = 9
NE_RUN = 32
SCAT_TEST = False
CAP = 352
EPS = 1e-5

ENGMAP = {"pe": "tensor", "act": "scalar", "dve": "vector", "pool": "gpsimd", "sp": "sync"}


class Sched:
    def __init__(self, nc, es):
        self.nc = nc
        self.es = es
        self.ops = []
        self.last_w = {}
        self.readers = {}
        self.eng_sem = {e: es.enter_context(nc.semaphore("sem_" + e)) for e in ENGMAP}
        self.dma_sem = {}
        self.dma_last = {}

    def op(self, eng, fn, r=(), w=(), dma=None):
        idx = len(self.ops)
        deps = set()
        for k in r:
            p = self.last_w.get(k)
            if p is not None:
                deps.add(p)
        for k in w:
            p = self.last_w.get(k)
            if p is not None:
                deps.add(p)
            deps |= self.readers.get(k, set())
        deps.discard(idx)
        for k in r:
            self.readers.setdefault(k, set()).add(idx)
        for k in w:
            self.last_w[k] = idx
            self.readers[k] = set()
        if dma is not None:
            name, n = dma
            if name not in self.dma_sem:
                self.dma_sem[name] = [self.es.enter_context(self.nc.semaphore("dq_" + name)), 0]
        self.ops.append(dict(eng=eng, fn=fn, deps=deps, dma=dma, token=None, needed=False))
        return idx

    def emit(self):
        ops = self.ops
        for op in ops:
            for d in op["deps"]:
                P = ops[d]
                if P["eng"] == "pe" and op["eng"] == "pe" and P["dma"] is None and op["dma"] is None:
                    continue
                P["needed"] = True
        cnt = {e: 0 for e in ENGMAP}
        for op in ops:
            if op["dma"] is not None:
                name, n = op["dma"]
                ent = self.dma_sem[name]
                ent[1] += 16 * n
                op["token"] = (ent[0], ent[1])
            elif op["needed"]:
                cnt[op["eng"]] += 1
                op["token"] = (self.eng_sem[op["eng"]], cnt[op["eng"]])
        waited = {e: {} for e in ENGMAP}
        nwaits = 0
        for op in ops:
            E = getattr(self.nc, ENGMAP[op["eng"]])
            need = {}
            for d in op["deps"]:
                P = ops[d]
                if P["token"] is None:
                    continue
                if P["eng"] == "pe" and op["eng"] == "pe" and P["dma"] is None and op["dma"] is None:
                    continue
                sem, c = P["token"]
                key = id(sem)
                if key not in need or need[key][1] < c:
                    need[key] = (sem, c)
            for key, (sem, c) in need.items():
                if waited[op["eng"]].get(key, 0) >= c:
                    continue
                E.wait_ge(sem, c)
                nwaits += 1
                waited[op["eng"]][key] = c
            res = op["fn"](E)
            if op["dma"] is not None:
                sem = op["token"][0]
                assert len(res) == op["dma"][1], (len(res), op["dma"])
                for ins in res:
                    ins.then_inc(sem, 16)
            elif op["token"] is not None:
                last = res[-1] if isinstance(res, (list, tuple)) else res
                last.then_inc(op["token"][0], 1)
        return nwaits


def build_program(stop_after=None, use=None):
    nc = bass.Bass("TRN2", target_bir_lowering=False)
    es = contextlib.ExitStack()
    with es:
        _build(nc, es, stop_after, use)
    return nc


def _build(nc, es, stop_after, use):
    def din(name, shape):
        if use is not None and name not in use:
            return None
        return nc.dram_tensor(name, list(shape), F32, kind="ExternalInput").ap()

    x = din("x", [S, D])
    mem = din("mem", [MEM, D])
    g_mix = din("g_mix", [1, D])
    w_in = din("w_in", [D, 3072])
    conv_w = din("conv_w", [3, 512])
    g_conv_out = din("g_conv_out", [1, 512])
    g_sb_out = din("g_sb_out", [1, 512])
    w_out = din("w_out", [D, D])
    g_xattn = din("g_xattn", [1, D])
    g_mem = din("g_mem", [1, D])
    w_q_mem = din("w_q_mem", [D, D])
    w_kv_mem = din("w_kv_mem", [D, 2 * D])
    w_o_mem = din("w_o_mem", [D, D])
    g_moe = din("g_moe", [1, D])
    w_router = din("w_router", [D, NE])
    b_router = din("b_router", [1, NE])
    w1 = din("w1", [max(NE_RUN, 1), D, 2 * D])
    b1 = din("b1", [NE, 2 * D])
    w2 = din("w2", [max(NE_RUN, 1), D, D])
    b2 = din("b2", [NE, D])
    g_final = din("g_final", [1, D])
    out = nc.dram_tensor("out", [S, D], F32, kind="ExternalOutput").ap()

    sc = Sched(nc, es)

    ARENA = 206 * 1024
    arena = es.enter_context(nc.sbuf_tensor("arena", [128, ARENA], U8))
    psum = es.enter_context(nc.psum_tensor("psum", [128, 8, 512], F32))

    def carve(off, nbytes, dt, pat=None, **kw):
        v = arena[:, off:off + nbytes].bitcast(dt)
        if pat is not None:
            v = v.rearrange(pat, **kw)
        return v

    def bank(b):
        return psum[:, b, :]

    K = 1024
    o = 0
    identF = carve(o, 512, F32); o += 512
    onesB = carve(o, 256, BF16); o += 256
    nonesB = carve(o, 256, BF16); o += 256
    ntriB = carve(o, 256, BF16); o += 256
    bdiagB = carve(o, 256, BF16); o += 256
    maskB = carve(o, 4096, BF16, "p (r f) -> p r f", r=4); o += 4096
    iotaI = carve(o, 2048, I32); o += 2048
    gtab = carve(o, 208, F32)
    gmixC = carve(o, 32, F32); o += 32
    gxatC = carve(o, 32, F32); o += 32
    gmemC = carve(o, 32, F32); o += 32
    gmoeC = carve(o, 32, F32); o += 32
    gconvC = carve(o, 16, F32); o += 16
    gsbC = carve(o, 16, F32); o += 16
    convC = carve(o, 48, F32, "p (k m) -> p k m", k=3); o += 48
    ssA = carve(o, 64, F32); o += 64
    rsA = carve(o, 64, F32); o += 64
    ssM = carve(o, 8, F32); o += 8
    rsM = carve(o, 8, F32); o += 8
    identB = carve(o, 256, BF16); o += 256
    triS = carve(o, 256, BF16); o += 256
    iotaE = carve(o, 128, F32); o += 128
    iotaEi = carve(o, 128, I32); o += 128
    brB = carve(o, 128, F32); o += 128
    wr = carve(o, 1024, F32, "p (k e) -> p k e", k=8); o += 1024
    gatesA = carve(o, 256, F32, "p (i k) -> p i k", i=NT); o += 256
    idxA = carve(o, 256, I32, "p (i k) -> p i k", i=NT); o += 256
    ssC = carve(o, 64, F32); o += 64
    rsC = carve(o, 64, F32); o += 64
    gst = carve(o, 512, F32); o += 512
    b1T = iotaI.bitcast(F32).rearrange("p (j e) -> p j e", j=16)
    b2S = maskB.rearrange("p r f -> p (r f)").bitcast(F32)
    assert o <= 12 * K
    XOFF = 12 * K
    X = carve(XOFF, 64 * K, F32, "p (i d) -> p i d", i=NT)
    hT = carve(XOFF, 32 * K, BF16, "p (k t) -> p k t", k=8)
    wsl = [carve(XOFF + 32 * K + j * 8 * K, 8 * K, BF16, "p (k f) -> p k f", k=8) for j in range(3)]
    xs = [carve(XOFF + 56 * K + j * 4 * K, 4 * K, F32) for j in range(2)]
    o = XOFF + 64 * K
    qT = carve(o, 16 * K, BF16, "p (m t) -> p m t", m=4); o += 16 * K
    kT = carve(o, 16 * K, BF16, "p (m t) -> p m t", m=4); o += 16 * K
    vtok = carve(o, 16 * K, BF16, "p (i f) -> p i f", i=NT); o += 16 * K
    ymix = carve(o, 32 * K, BF16, "p (k t) -> p k t", k=8); o += 32 * K
    TOFF = o
    assert TOFF == 156 * K
    xn = [carve(TOFF + j * 4 * K, 4 * K, F32) for j in range(2)]
    junk = carve(TOFF + 8 * K, 2 * K, BF16)
    uT = carve(TOFF + 10 * K, 4 * (S + 2) * 2, BF16, "p (m t) -> p m t", m=4)
    o = TOFF + 10 * K + 4 * (S + 2) * 2
    o = (o + 63) // 64 * 64
    cset = []
    for j in range(2):
        d = {}
        for nm, nb, dt in (("tc", 2 * K, F32), ("t0", 2 * K, F32), ("t1", 2 * K, F32), ("yc", 2 * K, F32),
                           ("sq", 1 * K, BF16), ("rs", 2 * K, F32)):
            d[nm] = carve(o, nb, dt); o += nb
        cset.append(d)
    assert o <= ARENA, o
    o = TOFF
    AT = {}
    AT["e"] = [carve(o + i * 2 * K, 2 * K, BF16, "p (h t) -> p h t", h=2) for i in range(3)]; o += 6 * K
    AT["sp"] = [carve(o + i * 2 * K, 2 * K, BF16, "p (h t) -> p h t", h=2) for i in range(2)]; o += 4 * K
    AT["sacc"] = [carve(o + i * 2 * K, 2 * K, BF16, "p (h t) -> p h t", h=2) for i in range(2)]; o += 4 * K
    AT["p"] = [carve(o + i * 2 * K, 2 * K, BF16, "p (h t) -> p h t", h=2) for i in range(2)]; o += 4 * K
    gn = {}
    gn["yraw"] = carve(o, 2 * K, F32); o += 2 * K
    gn["sq"] = carve(o, K, BF16); o += K
    gn["rs"] = carve(o, 2 * K, F32); o += 2 * K
    o = TOFF + 27 * K
    wo_sl = carve(o, 16 * K, BF16, "p (k f) -> p k f", k=8); o += 16 * K
    assert o <= ARENA

    def c_iota(E):
        return E.iota(iotaI, pattern=[[1, 512]], base=0, channel_multiplier=-1)
    sc.op("pool", c_iota, w=["iota"])

    def c_consts(E):
        r = []
        r.append(E.tensor_scalar(out=identF, in0=iotaI[:, 0:128], scalar1=0.0, scalar2=None, op0=ALU.is_equal))
        r.append(E.memset(onesB, 1.0))
        r.append(E.memset(nonesB, -1.0))
        r.append(E.tensor_scalar(out=ntriB, in0=iotaI[:, 0:128], scalar1=0.0, scalar2=-1.0,
                                 op0=ALU.is_le, op1=ALU.mult))
        r.append(E.memset(bdiagB[0:64, 0:64], 1.0))
        r.append(E.memset(bdiagB[0:64, 64:128], 0.0))
        r.append(E.memset(bdiagB[64:128, 0:64], 0.0))
        r.append(E.memset(bdiagB[64:128, 64:128], 1.0))
        for rr in range(4):
            r.append(E.tensor_scalar(out=maskB[:, rr, :], in0=iotaI, scalar1=float(128 * rr), scalar2=-29952.0,
                                     op0=ALU.is_le, op1=ALU.mult))
        r.append(E.memset(uT[:, :, 0:2], 0.0))
        r.append(E.tensor_scalar(out=triS, in0=iotaI[:, 0:128], scalar1=0.0, scalar2=None, op0=ALU.is_gt))
        r.append(E.tensor_copy(out=iotaE, in_=iotaEi))
        return r
    sc.op("pool", lambda E: E.iota(iotaEi, pattern=[[1, 32]], base=0, channel_multiplier=0), w=["iotaE"])
    sc.op("dve", c_consts, r=["iota", "iotaE"], w=["consts1", "uhalo", "maskc"])
    sc.op("dve", lambda E: E.tensor_copy(out=identB, in_=identF), r=["consts1"], w=["consts"])

    def c_gains(E):
        r = []
        srcs = [g_mix.rearrange("o (c p) -> (o c) p", p=128), g_xattn.rearrange("o (c p) -> (o c) p", p=128),
                g_mem.rearrange("o (c p) -> (o c) p", p=128), g_moe.rearrange("o (c p) -> (o c) p", p=128),
                g_conv_out.rearrange("o (c p) -> (o c) p", p=128), g_sb_out.rearrange("o (c p) -> (o c) p", p=128),
                conv_w.rearrange("k (m p) -> (k m) p", p=128)]
        row = 0
        for src in srcs:
            n = src.shape[0]
            r.append(E.dma_start(out=gst[row:row + n, :], in_=src))
            row += n
        return r
    sc.op("sp", c_gains, w=["gst"], dma=("gains", 7))
    sc.op("pe", lambda E: E.transpose(out=bank(7)[:, 0:52], in_=gst[0:52, :], identity=identF[0:52, 0:52]),
          r=["gst", "consts"], w=["ps7"])
    sc.op("dve", lambda E: E.tensor_copy(out=gtab, in_=bank(7)[:, 0:52]), r=["ps7"], w=["gains"])

    def a1_p1(i):
        s = i % 2

        def ld(E, i=i, s=s):
            return [E.dma_start(out=xs[s], in_=x[i * 128:(i + 1) * 128, :])]
        sc.op("sp", ld, w=[f"xs{s}"], dma=(f"xs{s}", 1))

        def sq(E, i=i, s=s):
            return E.activation(out=junk, in_=xs[s], func=AF.Square, accum_out=ssA[:, i:i + 1])
        sc.op("act", sq, r=[f"xs{s}"], w=[f"ss{i}"])

        def ln(E, i=i):
            return E.activation(out=rsA[:, i:i + 1], in_=ssA[:, i:i + 1], func=AF.Ln, scale=1.0 / D, bias=EPS)
        sc.op("act", ln, r=[f"ss{i}"], w=[f"rs{i}"])

        def ex(E, i=i):
            return E.activation(out=rsA[:, i:i + 1], in_=rsA[:, i:i + 1], func=AF.Exp, scale=-0.5)
        sc.op("act", ex, r=[f"rs{i}"], w=[f"rs{i}"])

        def nrm(E, i=i, s=s):
            return E.tensor_scalar(out=xn[s], in0=xs[s], scalar1=rsA[:, i:i + 1], scalar2=None, op0=ALU.mult)
        sc.op("dve", nrm, r=[f"xs{s}", f"rs{i}"], w=[f"xn{s}"])

    def a1_p2(i):
        s = i % 2
        b0 = 2 * s

        def tr(E, s=s, b0=b0):
            r = []
            for kc in range(8):
                r.append(E.transpose(out=bank(b0 + kc // 4)[:, (kc % 4) * 128:(kc % 4 + 1) * 128],
                                     in_=xn[s][:, kc * 128:(kc + 1) * 128], identity=identF))
            return r
        sc.op("pe", tr, r=[f"xn{s}", "consts"], w=[f"ps{b0}", f"ps{b0 + 1}"])

        def ev0(E, i=i, b0=b0):
            r = []
            for kc in range(4):
                r.append(E.tensor_scalar(out=hT[:, kc, i * 128:(i + 1) * 128],
                                         in0=bank(b0)[:, kc * 128:(kc + 1) * 128],
                                         scalar1=gmixC[:, kc:kc + 1], scalar2=None, op0=ALU.mult))
            return r
        sc.op("dve", ev0, r=[f"ps{b0}", "gains"], w=[f"hT{i}a"])

        def ev1(E, i=i, b0=b0):
            r = []
            for kc in range(4, 8):
                r.append(E.activation(out=hT[:, kc, i * 128:(i + 1) * 128],
                                      in_=bank(b0 + 1)[:, (kc - 4) * 128:(kc - 3) * 128],
                                      func=AF.Identity, scale=gmixC[:, kc:kc + 1]))
            return r
        sc.op("act", ev1, r=[f"ps{b0 + 1}", "gains"], w=[f"hT{i}b"])

    a1_p1(0)
    for i in range(NT):
        if i + 1 < NT:
            a1_p1(i + 1)
        a1_p2(i)

    def hkeys(c):
        return [f"hT{i}a" for i in range(4 * c, 4 * c + 4)] + [f"hT{i}b" for i in range(4 * c, 4 * c + 4)]

    def load_w_in(slot, blk):
        def f(E):
            return [E.dma_start(out=wsl[slot],
                                in_=w_in[:, blk * 512:(blk + 1) * 512].rearrange("(k p) f -> p k f", p=128))]
        sc.op("pool", f, w=[f"wsl{slot}"], dma=(f"wsl{slot}", 1))

    load_w_in(0, 3)
    load_w_in(1, 4)
    load_w_in(2, 5)
    pb = [4]

    def nextbank(lo=4, hi=8):
        b = pb[0]
        pb[0] = lo + (pb[0] - lo + 1) % (hi - lo)
        return b

    evtog = [0]

    def evac(out_ap, in_ap, r, w, scale=None):
        evtog[0] ^= 1
        if evtog[0]:
            def f(E):
                if scale is None:
                    return E.activation(out=out_ap, in_=in_ap, func=AF.Copy)
                return E.activation(out=out_ap, in_=in_ap, func=AF.Copy, scale=scale)
            sc.op("act", f, r=r, w=w)
        else:
            def f(E):
                if scale is None:
                    return E.tensor_copy(out=out_ap, in_=in_ap)
                return E.tensor_scalar(out=out_ap, in0=in_ap, scalar1=scale, scalar2=None, op0=ALU.mult)
            sc.op("dve", f, r=r, w=w)

    for slot, dstT, scl, nm in ((0, qT, 0.125, "qT"), (1, kT, None, "kT")):
        for c in range(4):
            for m in range(4):
                b = nextbank()

                def mm(E, slot=slot, c=c, m=m, b=b):
                    r = []
                    for kc in range(8):
                        r.append(E.matmul(out=bank(b), lhsT=wsl[slot][:, kc, m * 128:(m + 1) * 128],
                                          rhs=hT[:, kc, c * 512:(c + 1) * 512], start=(kc == 0), stop=(kc == 7)))
                    return r
                sc.op("pe", mm, r=hkeys(c) + [f"wsl{slot}"], w=[f"ps{b}"])
                evac(dstT[:, m, c * 512:(c + 1) * 512], bank(b), r=[f"ps{b}"], w=[f"{nm}{m}.{c}"], scale=scl)
    for i in range(NT):
        b = nextbank()

        def mm(E, i=i, b=b):
            r = []
            for kc in range(8):
                r.append(E.matmul(out=bank(b), lhsT=hT[:, kc, i * 128:(i + 1) * 128], rhs=wsl[2][:, kc, :],
                                  start=(kc == 0), stop=(kc == 7)))
            return r
        sc.op("pe", mm, r=[f"hT{i}a", f"hT{i}b", "wsl2"], w=[f"ps{b}"])
        evac(vtok[:, i, :], bank(b), r=[f"ps{b}"], w=[f"v{i}"])

    load_w_in(0, 1)
    load_w_in(1, 2)
    load_w_in(2, 0)
    conv_state = {}

    def conv_p1(it, c, m):
        cs = cset[it % 2]
        sfx = f"c{it % 2}"
        bC, bX, bB = nextbank(0, 8), nextbank(0, 8), nextbank(0, 8)
        conv_state[it] = bB
        for slot, b in ((0, bC), (1, bX), (2, bB)):
            def mm(E, slot=slot, c=c, m=m, b=b):
                r = []
                for kc in range(8):
                    r.append(E.matmul(out=bank(b), lhsT=wsl[slot][:, kc, m * 128:(m + 1) * 128],
                                      rhs=hT[:, kc, c * 512:(c + 1) * 512], start=(kc == 0), stop=(kc == 7)))
                return r
            sc.op("pe", mm, r=hkeys(c) + [f"wsl{slot}"], w=[f"ps{b}"])
        sc.op("act", lambda E, cs=cs, bC=bC: E.activation(out=cs["tc"], in_=bank(bC), func=AF.Copy),
              r=[f"ps{bC}"], w=["tc" + sfx])
        t0 = 2 + c * 512
        sc.op("dve", lambda E, cs=cs, bX=bX, m=m, t0=t0: E.tensor_tensor(
            out=uT[:, m, t0:t0 + 512], in0=bank(bX), in1=cs["tc"], op=ALU.mult),
            r=[f"ps{bX}", "tc" + sfx, "uhalo"], w=[f"u{m}.{c}"])

    def conv_p2(it, c, m):
        cs = cset[it % 2]
        sfx = f"c{it % 2}"
        bB = conv_state[it]
        bG = nextbank(0, 8)
        t0 = 2 + c * 512
        ukeys = [f"u{m}.{c}"] + ([f"u{m}.{c - 1}"] if c > 0 else ["uhalo"])
        sc.op("act", lambda E, cs=cs, m=m, t0=t0: E.activation(
            out=cs["t0"], in_=uT[:, m, t0:t0 + 512], func=AF.Identity, scale=convC[:, 2, m:m + 1]),
            r=ukeys + ["gains"], w=["t0" + sfx])
        sc.op("dve", lambda E, cs=cs, m=m, t0=t0: E.scalar_tensor_tensor(
            out=cs["t1"], in0=uT[:, m, t0 - 1:t0 + 511], scalar=convC[:, 1, m:m + 1], in1=cs["t0"],
            op0=ALU.mult, op1=ALU.add), r=ukeys + ["t0" + sfx, "gains"], w=["t1" + sfx])
        sc.op("dve", lambda E, cs=cs, m=m, t0=t0: E.scalar_tensor_tensor(
            out=cs["t0"], in0=uT[:, m, t0 - 2:t0 + 510], scalar=convC[:, 0, m:m + 1], in1=cs["t1"],
            op0=ALU.mult, op1=ALU.add), r=ukeys + ["t1" + sfx, "gains"], w=["t0" + sfx])
        sc.op("dve", lambda E, cs=cs, bB=bB: E.tensor_tensor(
            out=cs["yc"], in0=bank(bB), in1=cs["t0"], op=ALU.mult),
            r=[f"ps{bB}", "t0" + sfx], w=["yc" + sfx])
        sc.op("act", lambda E, cs=cs: E.activation(out=cs["sq"], in_=cs["yc"], func=AF.Square),
              r=["yc" + sfx], w=["sq" + sfx])
        sc.op("pe", lambda E, cs=cs, bG=bG: E.matmul(out=bank(bG), lhsT=bdiagB, rhs=cs["sq"],
                                                   start=True, stop=True),
              r=["sq" + sfx, "consts"], w=[f"ps{bG}"])
        sc.op("act", lambda E, cs=cs, bG=bG: E.activation(out=cs["rs"], in_=bank(bG), func=AF.Ln,
                                                         scale=1.0 / 64, bias=EPS),
              r=[f"ps{bG}"], w=["rs" + sfx])
        sc.op("act", lambda E, cs=cs: E.activation(out=cs["rs"], in_=cs["rs"], func=AF.Exp, scale=-0.5),
              r=["rs" + sfx], w=["rs" + sfx])
        sc.op("dve", lambda E, cs=cs, m=m, c=c: E.scalar_tensor_tensor(
            out=ymix[:, m, c * 512:(c + 1) * 512], in0=cs["yc"], scalar=gconvC[:, m:m + 1], in1=cs["rs"],
            op0=ALU.mult, op1=ALU.mult), r=["yc" + sfx, "rs" + sfx, "gains"], w=[f"ym{m}.{c}"])

    conv_its = [(c, m) for c in range(4) for m in range(4)]
    conv_p1(0, *conv_its[0])
    for it, (c, m) in enumerate(conv_its):
        if it + 1 < len(conv_its):
            conv_p1(it + 1, *conv_its[it + 1])
        conv_p2(it, c, m)

    cset_keys = [nm + f"c{j}" for j in range(2) for nm in ("tc", "t0", "t1", "yc", "sq", "rs")]

    def ld_wo(E):
        return [E.dma_start(out=wo_sl, in_=w_out.rearrange("(k p) f -> p k f", p=128))]
    sc.op("pool", ld_wo, w=["wo_sl"] + cset_keys, dma=("wo_sl", 1))

    conv_keys = ["xn0", "xn1"] + [f"u{m}.{c}" for m in range(4) for c in range(4)] + cset_keys
    BY = 6
    BG2 = 7
    first_att = [True]
    pending_gn = [None]
    for hp in range(4):
        for c in range(4):
            nkb = 4 * c + 4

            def emit_z(step, hp=hp, c=c, nkb=nkb):
                kb = nkb - 1 - step
                c0 = 128 * max(kb - 4 * c, 0)
                for hh in range(2):
                    P0 = 64 * hh
                    bz = 2 * (step % 2) + hh

                    r_ = kb - 4 * c

                    def zmm(E, P0=P0, bz=bz, kb=kb, c0=c0, r_=r_):
                        r = [E.matmul(out=bank(bz)[:, c0:512], lhsT=kT[P0:P0 + 64, hp, kb * 128:(kb + 1) * 128],
                                      rhs=qT[P0:P0 + 64, hp, c * 512 + c0:(c + 1) * 512], start=True, stop=(r_ < 0))]
                        if r_ >= 0:
                            r.append(E.matmul(out=bank(bz)[:, c0:512], lhsT=identB, rhs=maskB[:, r_, c0:512],
                                              start=False, stop=True))
                        return r
                    sc.op("pe", zmm, r=[f"kT{hp}.{kb // 4}", f"qT{hp}.{c}", "consts", "maskc"], w=[f"ps{bz}"])

            extra_w = conv_keys if first_att[0] else []
            first_att[0] = False
            sc.op("dve", lambda E: [E.memset(AT["sacc"][0], 0.0), E.memset(AT["sacc"][1], 0.0)],
                  w=["sacc0", "sacc1"] + extra_w)
            def stageA(step, c=c, nkb=nkb, extra_w=extra_w):
                kb = nkb - 1 - step
                r_ = kb - 4 * c
                par = step % 2
                c0 = 128 * max(r_, 0)
                tg = f"p{par}"
                e3 = step % 3
                ek = f"eb{e3}"
                zp = psum[:, 2 * par:2 * par + 2, c0:512]
                sc.op("act", lambda E: E.activation(out=AT["e"][e3][:, :, c0:512], in_=zp, func=AF.Exp),
                      r=[f"ps{2 * par}", f"ps{2 * par + 1}"], w=[ek] + extra_w)
                sc.op("act", lambda E: E.activation(out=AT["sp"][par][:, :, c0:512], in_=AT["e"][e3][:, :, c0:512],
                                                    func=AF.Ln, bias=1.0), r=[ek], w=["sp" + tg])

            def stageB(step, c=c, nkb=nkb):
                kb = nkb - 1 - step
                par = step % 2
                c0 = 128 * max(kb - 4 * c, 0)
                tg = f"p{par}"
                for hh in range(2):
                    br = 4 + hh

                    def rmm(E, hh=hh, br=br):
                        r = [E.matmul(out=bank(br)[:, c0:512], lhsT=ntriB, rhs=AT["sp"][par][:, hh, c0:512], start=True,
                                      stop=(step == 0))]
                        if step > 0:
                            r.append(E.matmul(out=bank(br)[:, c0:512], lhsT=nonesB,
                                              rhs=AT["sacc"][(step - 1) % 2][:, hh, c0:512], start=False, stop=True))
                        return r
                    rk = ["sp" + tg, "consts"] + ([f"sacc{(step - 1) % 2}"] if step > 0 else [])
                    sc.op("pe", rmm, r=rk, w=[f"ps{br}"])
                if step < nkb - 1:
                    sc.op("dve", lambda E: E.tensor_tensor(
                        out=AT["sacc"][step % 2][:, :, c0:512], in0=AT["sacc"][(step - 1) % 2][:, :, c0:512],
                        in1=AT["sp"][par][:, :, c0:512], op=ALU.add),
                        r=["sp" + tg, f"sacc{(step - 1) % 2}"], w=[f"sacc{step % 2}"])

            def stageC1(step, c=c, nkb=nkb):
                kb = nkb - 1 - step
                par = step % 2
                c0 = 128 * max(kb - 4 * c, 0)
                tg = f"p{par}"
                if c0 > 0:
                    sc.op("dve", lambda E: E.memset(AT["p"][par][:, :, 0:c0], 0.0), w=["pl" + tg])
                sc.op("act", lambda E: E.activation(out=AT["p"][par][:, :, c0:512], in_=psum[:, 4:6, c0:512], func=AF.Exp),
                      r=["ps4", "ps5"], w=["p" + tg])
                e3 = step % 3
                sc.op("dve", lambda E: E.tensor_tensor(
                    out=AT["p"][par][:, :, c0:512], in0=AT["p"][par][:, :, c0:512], in1=AT["e"][e3][:, :, c0:512],
                    op=ALU.mult), r=["p" + tg, f"eb{e3}"], w=["p" + tg])

            def stageC2(step, hp=hp, c=c, nkb=nkb):
                kb = nkb - 1 - step
                par = step % 2
                c0 = 128 * max(kb - 4 * c, 0)
                tg = f"p{par}"
                for hh in range(2):
                    P0 = 64 * hh

                    def ymm(E, P0=P0, hh=hh):
                        h = 2 * hp + hh
                        return E.matmul(out=bank(BY)[P0:P0 + 64, :], lhsT=vtok[:, kb, h * 64:(h + 1) * 64],
                                        rhs=AT["p"][par][:, hh, :], start=(step == 0), stop=(step == nkb - 1))
                    sc.op("pe", ymm, r=["p" + tg, "pl" + tg, f"v{kb}"], w=[f"ps{BY}.{hh}"])

            emit_z(0)
            emit_z(1)
            stageA(0)
            stageB(0)
            if pending_gn[0] is not None:
                pending_gn[0]()
                pending_gn[0] = None
            for step in range(nkb):
                if step + 1 < nkb:
                    stageA(step + 1)
                if step + 2 < nkb:
                    emit_z(step + 2)
                stageC1(step)
                if step + 1 < nkb:
                    stageB(step + 1)
                stageC2(step)
            def gn_ops(hp=hp, c=c):
                sc.op("dve", lambda E: E.tensor_copy(out=gn["yraw"], in_=bank(BY)),
                      r=[f"ps{BY}.0", f"ps{BY}.1"], w=["gyraw"])
                sc.op("act", lambda E: E.activation(out=gn["sq"], in_=gn["yraw"], func=AF.Square),
                      r=["gyraw"], w=["gsq"])
                sc.op("pe", lambda E: E.matmul(out=bank(BG2), lhsT=bdiagB, rhs=gn["sq"], start=True, stop=True),
                      r=["gsq", "consts"], w=[f"ps{BG2}"])
                sc.op("act", lambda E: E.activation(out=gn["rs"], in_=bank(BG2), func=AF.Ln, scale=1.0 / 64, bias=EPS),
                      r=[f"ps{BG2}"], w=["grs"])
                sc.op("act", lambda E: E.activation(out=gn["rs"], in_=gn["rs"], func=AF.Exp, scale=-0.5),
                      r=["grs"], w=["grs"])
                sc.op("dve", lambda E, hp=hp, c=c: E.scalar_tensor_tensor(
                    out=ymix[:, 4 + hp, c * 512:(c + 1) * 512], in0=gn["yraw"], scalar=gsbC[:, hp:hp + 1], in1=gn["rs"],
                    op0=ALU.mult, op1=ALU.mult), r=["gyraw", "grs", "gains"], w=[f"ym{4 + hp}.{c}"])
            pending_gn[0] = gn_ops

    pending_gn[0]()

    all_h = [f"hT{i}a" for i in range(NT)] + [f"hT{i}b" for i in range(NT)]
    for i in range(NT):
        over = []
        if i < 8:
            over = all_h
        elif i < 14:
            over = [f"wsl{(i - 8) // 2}"]
        else:
            over = [f"xs{i - 14}"]

        def ldx(E, i=i):
            return [E.dma_start(out=X[:, i, :], in_=x[i * 128:(i + 1) * 128, :])]
        sc.op("sp", ldx, w=[f"X{i}"] + over, dma=(f"X{i}", 1))
    ymk = [f"ym{m}.{c}" for m in range(8) for c in range(4)]
    for i in range(NT):
        for hh in range(2):
            b = nextbank(6, 8)

            def mm(E, i=i, hh=hh, b=b):
                r = []
                for kc in range(8):
                    r.append(E.matmul(out=bank(b), lhsT=ymix[:, kc, i * 128:(i + 1) * 128],
                                      rhs=wo_sl[:, kc, hh * 512:(hh + 1) * 512], start=(kc == 0), stop=(kc == 7)))
                return r
            sc.op("pe", mm, r=[f"ym{m}.{i // 4}" for m in range(8)] + ["wo_sl"], w=[f"ps{b}"])
            sc.op("dve", lambda E, i=i, hh=hh, b=b: E.tensor_tensor(
                out=X[:, i, hh * 512:(hh + 1) * 512], in0=bank(b), in1=X[:, i, hh * 512:(hh + 1) * 512], op=ALU.add),
                r=[f"ps{b}", f"X{i}"], w=[f"X{i}"])

    KQ = [f"qT{m}.{c}" for m in range(4) for c in range(4)]
    KK = [f"kT{m}.{c}" for m in range(4) for c in range(4)]
    KV = [f"v{i}" for i in range(NT)]
    KATT = ["plp0", "plp1", "eb0", "eb1", "eb2", "spp0", "spp1", "pp0", "pp1", "sacc0", "sacc1", "gyraw", "gsq", "grs"]

    def norm_transpose(tag, src, src_keys, nbuf, nbuf_key, nbuf_extra, ss_col, rs_col, gC, dst_of_kc, dkey_a, dkey_b,
                       dst_extra, banks=None, defer=False):
        sc.op("act", lambda E: E.activation(out=junk2, in_=src, func=AF.Square, accum_out=ss_col),
              r=src_keys, w=["ss" + tag])
        sc.op("act", lambda E: E.activation(out=rs_col, in_=ss_col, func=AF.Ln, scale=1.0 / D, bias=EPS),
              r=["ss" + tag], w=["rs" + tag])
        sc.op("act", lambda E: E.activation(out=rs_col, in_=rs_col, func=AF.Exp, scale=-0.5),
              r=["rs" + tag], w=["rs" + tag])
        sc.op("dve", lambda E: E.tensor_scalar(out=nbuf, in0=src, scalar1=rs_col, scalar2=None, op0=ALU.mult),
              r=src_keys + ["rs" + tag], w=[nbuf_key] + nbuf_extra)
        if defer:
            return lambda: _norm_part2(nbuf, nbuf_key, gC, dst_of_kc, dkey_a, dkey_b, dst_extra, banks)
        _norm_part2(nbuf, nbuf_key, gC, dst_of_kc, dkey_a, dkey_b, dst_extra, banks)

    def _norm_part2(nbuf, nbuf_key, gC, dst_of_kc, dkey_a, dkey_b, dst_extra, banks):
        ba, bb = banks if banks is not None else (nextbank(0, 8), nextbank(0, 8))

        def tr(E):
            r = []
            for kc in range(8):
                bk = ba if kc < 4 else bb
                r.append(E.transpose(out=bank(bk)[:, (kc % 4) * 128:(kc % 4 + 1) * 128],
                                     in_=nbuf[:, kc * 128:(kc + 1) * 128], identity=identF))
            return r
        sc.op("pe", tr, r=[nbuf_key, "consts"], w=[f"ps{ba}", f"ps{bb}"])

        def ev0(E):
            return [E.tensor_scalar(out=dst_of_kc(kc), in0=bank(ba)[:, kc * 128:(kc + 1) * 128],
                                    scalar1=gC[:, kc:kc + 1], scalar2=None, op0=ALU.mult) for kc in range(4)]
        sc.op("dve", ev0, r=[f"ps{ba}", "gains"], w=[dkey_a] + dst_extra)

        def ev1(E):
            return [E.activation(out=dst_of_kc(kc), in_=bank(bb)[:, (kc - 4) * 128:(kc - 3) * 128],
                                 func=AF.Identity, scale=gC[:, kc:kc + 1]) for kc in range(4, 8)]
        sc.op("act", ev1, r=[f"ps{bb}", "gains"], w=[dkey_b] + dst_extra)

    if stop_after != "A":
        wkv = carve(76 * K, 32 * K, BF16, "p (k f) -> p k f", k=8)
        wq = carve(76 * K, 16 * K, BF16, "p (k f) -> p k f", k=8)
        wo = carve(92 * K, 16 * K, BF16, "p (k f) -> p k f", k=8)
        memnT = carve(108 * K, 4 * K, BF16, "p (k t) -> p k t", k=8)
        kTx = carve(112 * K, 4 * K, BF16, "p (k t) -> p k t", k=8)
        vx = carve(116 * K, 4 * K, BF16, "p (j f) -> p j f", j=2)
        h2T = carve(124 * K, 32 * K, BF16, "p (k t) -> p k t", k=8)
        xm = [carve(156 * K + j * 4 * K, 4 * K, F32) for j in range(2)]
        qTx = [carve(164 * K + j * 8 * K, 8 * K, BF16, "p (k t) -> p k t", k=8) for j in range(2)]
        oTx = [carve(180 * K + j * 8 * K, 8 * K, BF16, "p (k t) -> p k t", k=8) for j in range(2)]
        ET = [carve(196 * K + j * 2 * K, 2 * K, BF16, "p (m t) -> p m t", m=2) for j in range(2)]
        rden = [carve(200 * K + j * 2 * K, 2 * K, F32) for j in range(2)]
        junk2 = carve(204 * K, 2 * K, BF16)

        sc.op("pool", lambda E: [E.dma_start(out=wkv, in_=w_kv_mem.rearrange("(k p) f -> p k f", p=128))],
              w=["wkv"] + KQ + KK, dma=("wkv", 1))
        for j in range(2):
            sc.op("sp", lambda E, j=j: [E.dma_start(out=xm[j], in_=mem[j * 128:(j + 1) * 128, :])],
                  w=[f"xm{j}"] + KATT + ["wo_sl"], dma=(f"xm{j}", 1))
            norm_transpose(f"M{j}", xm[j], [f"xm{j}"], xm[j], f"xm{j}", [], ssM[:, j:j + 1], rsM[:, j:j + 1], gmemC,
                           lambda kc, j=j: memnT[:, kc, j * 128:(j + 1) * 128], f"memnT{j}a", f"memnT{j}b", KV)
        KMEM = [f"memnT{j}{ab}" for j in range(2) for ab in "ab"]
        for fc in range(8):
            b = nextbank(0, 8)

            def mm(E, fc=fc, b=b):
                return [E.matmul(out=bank(b)[:, 0:256], lhsT=wkv[:, kc, fc * 128:(fc + 1) * 128], rhs=memnT[:, kc, :],
                                 start=(kc == 0), stop=(kc == 7)) for kc in range(8)]
            sc.op("pe", mm, r=KMEM + ["wkv"], w=[f"ps{b}"])
            evac(kTx[:, fc, :], bank(b)[:, 0:256], r=[f"ps{b}"], w=[f"kTx{fc}"] + KV)
        for mt in range(2):
            for hh in range(2):
                b = nextbank(0, 8)

                def mm(E, mt=mt, hh=hh, b=b):
                    return [E.matmul(out=bank(b), lhsT=memnT[:, kc, mt * 128:(mt + 1) * 128],
                                     rhs=wkv[:, kc, 1024 + hh * 512:1024 + (hh + 1) * 512],
                                     start=(kc == 0), stop=(kc == 7)) for kc in range(8)]
                sc.op("pe", mm, r=KMEM + ["wkv"], w=[f"ps{b}"])
                evac(vx[:, mt, hh * 512:(hh + 1) * 512], bank(b), r=[f"ps{b}"], w=[f"vx{mt}.{hh}"] + KV)
        KKX = [f"kTx{fc}" for fc in range(8)]
        KVX = [f"vx{mt}.{hh}" for mt in range(2) for hh in range(2)]
        sc.op("pool", lambda E: [E.dma_start(out=wq, in_=w_q_mem.rearrange("(k p) f -> p k f", p=128))],
              w=["wq", "wkv"], dma=("wq", 1))
        sc.op("pool", lambda E: [E.dma_start(out=wo, in_=w_o_mem.rearrange("(k p) f -> p k f", p=128))],
              w=["wo", "wkv"], dma=("wo", 1))
        def bnorm(i):
            return norm_transpose(f"B{i}", X[:, i, :], [f"X{i}"], xm[i % 2], f"xm{i % 2}", [], ssA[:, i:i + 1], rsA[:, i:i + 1],
                                  gxatC, lambda kc, i=i: h2T[:, kc, i * 128:(i + 1) * 128], f"h2T{i}a", f"h2T{i}b",
                                  [f"ym{m}.{i // 4}" for m in range(8)], defer=True)
        p2 = bnorm(0)
        for i in range(NT):
            nxt2 = bnorm(i + 1) if i + 1 < NT else None
            p2()
            p2 = nxt2
        def b_qproj(c):
            cb = c % 2
            h2k = [f"h2T{i}{ab}" for i in range(4 * c, 4 * c + 4) for ab in "ab"]
            for fc in range(8):
                b = nextbank(0, 8)

                def mm(E, fc=fc, c=c, b=b):
                    return [E.matmul(out=bank(b), lhsT=wq[:, kc, fc * 128:(fc + 1) * 128],
                                     rhs=h2T[:, kc, c * 512:(c + 1) * 512], start=(kc == 0), stop=(kc == 7))
                            for kc in range(8)]
                sc.op("pe", mm, r=h2k + ["wq"], w=[f"ps{b}"])
                evac(qTx[cb][:, fc, :], bank(b), r=[f"ps{b}"], w=[f"qTx{cb}.{fc}"], scale=1.0 / 16)

        def b_att(c):
            cb = c % 2
            for h in range(4):
                eb = h % 2
                for mt in range(2):
                    b = nextbank(0, 8)

                    def mm(E, h=h, mt=mt, b=b, cb=cb):
                        return [E.matmul(out=bank(b), lhsT=kTx[:, 2 * h + dc, mt * 128:(mt + 1) * 128],
                                         rhs=qTx[cb][:, 2 * h + dc, :], start=(dc == 0), stop=(dc == 1))
                                for dc in range(2)]
                    sc.op("pe", mm, r=KKX + [f"qTx{cb}.{2 * h}", f"qTx{cb}.{2 * h + 1}"], w=[f"ps{b}"])
                    sc.op("act", lambda E, eb=eb, mt=mt, b=b: E.activation(out=ET[eb][:, mt, :], in_=bank(b), func=AF.Exp),
                          r=[f"ps{b}"], w=[f"ET{eb}.{mt}"])
                bd = nextbank(0, 8)
                sc.op("pe", lambda E, eb=eb, bd=bd: [E.matmul(out=bank(bd), lhsT=onesB, rhs=ET[eb][:, mt, :],
                                                             start=(mt == 0), stop=(mt == 1)) for mt in range(2)],
                      r=[f"ET{eb}.0", f"ET{eb}.1", "consts"], w=[f"ps{bd}"])
                sc.op("dve", lambda E, eb=eb, bd=bd: E.reciprocal(out=rden[eb], in_=bank(bd)),
                      r=[f"ps{bd}"], w=[f"rden{eb}"])
                for dc in range(2):
                    b = nextbank(0, 8)
                    sc.op("pe", lambda E, eb=eb, b=b, h=h, dc=dc: [
                        E.matmul(out=bank(b), lhsT=vx[:, mt, (2 * h + dc) * 128:(2 * h + dc + 1) * 128],
                                 rhs=ET[eb][:, mt, :], start=(mt == 0), stop=(mt == 1)) for mt in range(2)],
                        r=[f"ET{eb}.0", f"ET{eb}.1"] + KVX, w=[f"ps{b}"])
                    sc.op("dve", lambda E, eb=eb, b=b, h=h, dc=dc, cb=cb: E.tensor_tensor(
                        out=oTx[cb][:, 2 * h + dc, :], in0=bank(b), in1=rden[eb], op=ALU.mult),
                        r=[f"ps{b}", f"rden{eb}"], w=[f"oTx{cb}.{2 * h + dc}"])

        def b_oproj(c):
            cb = c % 2
            for ii in range(4):
                i = 4 * c + ii
                for hh in range(2):
                    b = nextbank(0, 8)
                    sc.op("pe", lambda E, ii=ii, hh=hh, b=b, cb=cb: [
                        E.matmul(out=bank(b), lhsT=oTx[cb][:, fc, ii * 128:(ii + 1) * 128],
                                 rhs=wo[:, fc, hh * 512:(hh + 1) * 512], start=(fc == 0), stop=(fc == 7))
                        for fc in range(8)],
                        r=[f"oTx{cb}.{fc}" for fc in range(8)] + ["wo"], w=[f"ps{b}"])
                    sc.op("dve", lambda E, i=i, hh=hh, b=b: E.tensor_tensor(
                        out=X[:, i, hh * 512:(hh + 1) * 512], in0=bank(b), in1=X[:, i, hh * 512:(hh + 1) * 512],
                        op=ALU.add), r=[f"ps{b}", f"X{i}"], w=[f"X{i}"])

        b_qproj(0)
        for c in range(4):
            b_att(c)
            if c + 1 < 4:
                b_qproj(c + 1)
            b_oproj(c)

    if stop_after in ("A", "B"):
        for i in range(NT):
            def st(E, i=i):
                return [E.dma_start(out=out[i * 128:(i + 1) * 128, :], in_=X[:, i, :])]
            sc.op("sp", st, r=[f"X{i}"], w=[f"out{i}"], dma=("out", 1))
    else:
        Xs_d = nc.dram_tensor("xs_scratch", [NE * CAP, D], BF16, kind="Internal").ap()
        Ys_d = nc.dram_tensor("ys_scratch", [NE * CAP, D], BF16, kind="Internal").ap()
        BIG = 1.0e6
        bc_reg = nc.gpsimd.to_reg(NE * CAP - 1)
        ring = [carve(76 * K + j * 16 * K, 16 * K, BF16, "p (k f) -> p k f", k=8) for j in range(5)]
        T0 = 156 * K
        idxT = carve(T0 + 44 * K, 256, I32, "p (i k) -> p i k", i=NT) if SCAT_TEST else None
        if SCAT_TEST:
            sc.op("pool", lambda E: E.iota(idxT, pattern=[[512, 16], [128, 4]], base=0, channel_multiplier=1), w=["idxT"])
        xn3 = [carve(T0 + j * 4 * K, 4 * K, F32) for j in range(2)]
        h3b = [carve(T0 + 8 * K + j * 2 * K, 2 * K, BF16) for j in range(4)]
        h3T = [carve(T0 + 16 * K + j * 4 * K, 4 * K, F32, "p (k t) -> p k t", k=8) for j in range(2)]
        o = T0 + 24 * K
        sm = {}
        for nm, nb, dt in (("lg", 128, F32), ("max8", 32, F32), ("idx8", 32, U32), ("idxf", 16, F32), ("nmax", 4, F32),
                           ("ex4", 16, F32), ("sum4", 4, F32), ("posS", 128, F32), ("oh", 128, F32), ("jr", 128, F32),
                           ("pk", 16, F32), ("okf", 16, F32), ("destf", 16, F32), ("Gd", 128, F32), ("GT", 512, F32)):
            sm[nm] = [carve(o + j * nb, nb, dt) for j in range(4)]
            o += 4 * nb
        o = (o + 63) // 64 * 64
        mskB = carve(o, 1024, BF16, "p (i e) -> p i e", i=NT); o += 1024
        b1st = carve(o, 8 * K, F32); o += 8 * K
        assert o <= ARENA
        xstage = [carve(T0 + j * 6 * K, 6 * K, BF16, "p (j d) -> p j d", j=3) for j in range(2)]
        XsT = [carve(T0 + 12 * K + j * 6 * K, 8 * CAP * 2, BF16, "p (k t) -> p k t", k=8) for j in range(2)]
        actT = carve(T0 + 24 * K, 8 * CAP * 2, BF16, "p (k t) -> p k t", k=8)
        RT = CAP - 256
        o = T0 + 30 * K
        hset = []
        for j in range(2):
            d = {}
            for nm in ("g", "sig", "l1", "gs"):
                d[nm] = carve(o, CAP * 4, F32); o += 1536
            hset.append(d)
        ystage = carve(o, 6 * K, BF16, "p (j d) -> p j d", j=3); o += 6 * K
        assert o <= ARENA
        grow = [[carve(T0 + (j * 4 + k) * 2 * K, 2 * K, BF16) for k in range(4)] for j in range(2)]
        acc = [carve(T0 + 16 * K + j * 4 * K, 4 * K, F32) for j in range(2)]
        ostage = [carve(T0 + 24 * K + j * 4 * K, 4 * K, F32) for j in range(2)]
        gfin = carve(T0 + 32 * K, 4 * K, F32)
        junk3 = carve(T0 + 36 * K, 2 * K, BF16)
        dg = [[carve(T0 + 38 * K + (j * 4 + k) * 256, 256, BF16) for k in range(4)] for j in range(2)]

        KB_ALL = ["wq", "wo", "wkv"] + KMEM + KKX + KVX + [f"h2T{i}{ab}" for i in range(NT) for ab in "ab"]
        KB_T = ["xm0", "xm1"] + [f"qTx{j}.{fc}" for j in range(2) for fc in range(8)] + \
            [f"oTx{j}.{fc}" for j in range(2) for fc in range(8)] + [f"ET{j}.{m}" for j in range(2) for m in range(2)] + \
            ["rden0", "rden1"]

        pieces = []
        for e in range(NE_RUN):
            pieces += [(e, "g"), (e, "l"), (e, "w2")]
        piece_slot = {}
        state = {"next": 0}

        def issue_piece():
            n = state["next"]
            if n >= len(pieces):
                return
            state["next"] += 1
            e, kind = pieces[n]
            slot = n % 5
            piece_slot[(e, kind)] = slot
            if kind == "g":
                src = w1[e, :, 0:1024]
            elif kind == "l":
                src = w1[e, :, 1024:2048]
            else:
                src = w2[e, :, :]
            extra = KB_ALL if n < 5 else []
            sc.op("pool", lambda E, slot=slot, src=src: [E.dma_start(out=ring[slot], in_=src.rearrange("(k p) f -> p k f", p=128))],
                  w=[f"ring{slot}"] + extra, dma=(f"ring{slot}", 1))

        if stop_after != "C1":
            for _ in range(5):
                issue_piece()

        def ld_moe_consts(E):
            r = []
            r.append(E.dma_start(out=wr, in_=w_router.rearrange("(k p) e -> p k e", p=128)))
            r.append(E.dma_start(out=brB, in_=b_router.partition_broadcast(128).rearrange("p o e -> p (o e)")))
            r.append(E.dma_start(out=b2S[0:32, :], in_=b2))
            r.append(E.dma_start(out=b1st[0:32, :], in_=b1))
            return r
        sc.op("sp", ld_moe_consts, w=["moec", "maskc", "b1st"] + KB_T, dma=("moec", 4))
        bt = nextbank(0, 8)
        sc.op("pe", lambda E: [E.transpose(out=bank(bt)[:, j * 32:(j + 1) * 32], in_=b1st[0:32, j * 128:(j + 1) * 128],
                                           identity=identF[0:32, 0:32]) for j in range(16)],
              r=["b1st", "consts"], w=[f"ps{bt}"])
        sc.op("dve", lambda E: E.tensor_copy(out=b1T, in_=bank(bt).rearrange("p (j e) -> p j e", j=16)),
              r=[f"ps{bt}"], w=["b1T", "iota"])
        sc.op("dve", lambda E: E.tensor_scalar(out=b1T[:, 8:16, :], in0=b1T[:, 8:16, :], scalar1=1.0, scalar2=None,
                                               op0=ALU.add), r=["b1T"], w=["b1T"])

        XSK = []
        def c1_tile(i):
            s_ = i % 2
            s4 = i % 4
            t = {k: v[s4] for k, v in sm.items()}
            tg = f"r{s4}"
            first = KB_T if i < 4 else []
            norm_transpose(f"C{i}", X[:, i, :], [f"X{i}"], xn3[s_], f"xn3{s_}", first, ssC[:, i:i + 1], rsC[:, i:i + 1],
                           gmoeC, lambda kc, s_=s_: h3T[s_][:, kc, :], f"h3T{s_}a", f"h3T{s_}b", first, banks=(2 * s4, 2 * s4 + 1))
            sc.op("dve", lambda E, s_=s_, s4=s4: E.tensor_copy(out=h3b[s4], in_=xn3[s_]), r=[f"xn3{s_}"], w=[f"h3b{s4}"] + first)
            bl = 2 * s4
            sc.op("pe", lambda E, s_=s_, bl=bl: [E.matmul(out=bank(bl)[:, 0:32], lhsT=h3T[s_][:, kc, :], rhs=wr[:, kc, :],
                                                         start=(kc == 0), stop=(kc == 7)) for kc in range(8)],
                  r=[f"h3T{s_}a", f"h3T{s_}b", "moec"], w=[f"ps{bl}"])
            yield
            sc.op("dve", lambda E, t=t, bl=bl: E.tensor_tensor(out=t["lg"], in0=bank(bl)[:, 0:32], in1=brB, op=ALU.add),
                  r=[f"ps{bl}", "moec"], w=["lg" + tg] + first)
            if C1_LEVEL >= 1:
                yield
                sc.op("dve", lambda E, t=t: E.max(out=t["max8"], in_=t["lg"]), r=["lg" + tg], w=["max8" + tg])
                yield
                sc.op("dve", lambda E, t=t: E.max_index(out=t["idx8"], in_max=t["max8"], in_values=t["lg"]),
                      r=["lg" + tg, "max8" + tg], w=["idx8" + tg])
                yield
                sc.op("dve", lambda E, t=t: E.tensor_scalar(out=t["nmax"], in0=t["max8"][:, 0:1], scalar1=-1.0, scalar2=None,
                                                            op0=ALU.mult), r=["max8" + tg], w=["nmax" + tg])
                yield
                sc.op("act", lambda E, t=t: E.activation(out=t["ex4"], in_=t["max8"][:, 0:4], func=AF.Exp, bias=t["nmax"],
                                                         accum_out=t["sum4"]),
                      r=["max8" + tg, "nmax" + tg], w=["ex4" + tg, "sum4" + tg])
                yield
                sc.op("dve", lambda E, t=t: E.reciprocal(out=t["sum4"], in_=t["sum4"]), r=["sum4" + tg], w=["sum4" + tg])
                yield
                sc.op("dve", lambda E, t=t, i=i: E.tensor_scalar(out=gatesA[:, i, :], in0=t["ex4"], scalar1=t["sum4"],
                                                                 scalar2=None, op0=ALU.mult),
                      r=["ex4" + tg, "sum4" + tg], w=[f"gate{i}"])
                yield
                sc.op("dve", lambda E, t=t, i=i: E.tensor_scalar(out=mskB[:, i, :], in0=t["lg"], scalar1=t["max8"][:, 3:4],
                                                                 scalar2=None, op0=ALU.is_ge),
                      r=["lg" + tg, "max8" + tg], w=[f"msk{i}"])
            if C1_LEVEL >= 2:
                bp = 2 * s4 + 1
                yield
                sc.op("pe", lambda E, i=i, bp=bp: [E.matmul(out=bank(bp)[:, 0:32], lhsT=triS, rhs=mskB[:, i, :], start=True,
                                                            stop=(i == 0))] +
                      [E.matmul(out=bank(bp)[:, 0:32], lhsT=onesB, rhs=mskB[:, i2, :], start=False, stop=(i2 == i - 1))
                       for i2 in range(i)],
                      r=[f"msk{i2}" for i2 in range(i + 1)] + ["consts"], w=[f"ps{bp}"])
                yield
                sc.op("dve", lambda E, t=t, bp=bp: E.tensor_copy(out=t["posS"], in_=bank(bp)[:, 0:32]),
                      r=[f"ps{bp}"], w=["posS" + tg])
                yield
                sc.op("dve", lambda E, t=t: E.tensor_copy(out=t["idxf"], in_=t["idx8"][:, 0:4]), r=["idx8" + tg], w=["idxf" + tg])
                for k in range(4):
                    yield
                    sc.op("dve", lambda E, t=t, k=k: E.scalar_tensor_tensor(
                        out=t["jr"], in0=iotaE, scalar=t["idxf"][:, k:k + 1], in1=t["posS"], op0=ALU.is_equal, op1=ALU.mult,
                        accum_out=t["pk"][:, k:k + 1]), r=["idxf" + tg, "posS" + tg, "consts"], w=["jr" + tg, f"pk{k}" + tg])
                yield
                sc.op("act", lambda E, t=t: E.activation(out=t["oh"], in_=t["lg"], func=AF.Exp, bias=t["nmax"]),
                      r=["lg" + tg, "nmax" + tg], w=["oh" + tg])
                yield
                sc.op("dve", lambda E, t=t: E.tensor_scalar(out=t["Gd"], in0=t["lg"], scalar1=t["max8"][:, 3:4], scalar2=None,
                                                            op0=ALU.is_ge), r=["lg" + tg, "max8" + tg], w=["Gd" + tg])
                yield
                sc.op("dve", lambda E, t=t: E.scalar_tensor_tensor(out=t["Gd"], in0=t["oh"], scalar=t["sum4"], in1=t["Gd"],
                                                                   op0=ALU.mult, op1=ALU.mult),
                      r=["oh" + tg, "sum4" + tg, "Gd" + tg], w=["Gd" + tg])
                pkk = [f"pk{k}" + tg for k in range(4)]
                yield
                sc.op("dve", lambda E, t=t: E.tensor_scalar(out=t["okf"], in0=t["pk"], scalar1=float(CAP), scalar2=None,
                                                            op0=ALU.is_lt), r=pkk, w=["okf" + tg])
                yield
                sc.op("dve", lambda E, t=t: E.scalar_tensor_tensor(out=t["destf"], in0=t["idxf"], scalar=float(CAP), in1=t["pk"],
                                                                   op0=ALU.mult, op1=ALU.add),
                      r=pkk + ["idxf" + tg], w=["destf" + tg])
                yield
                sc.op("dve", lambda E, t=t: E.scalar_tensor_tensor(out=t["destf"], in0=t["destf"], scalar=-BIG, in1=t["okf"],
                                                                   op0=ALU.add, op1=ALU.mult),
                      r=["destf" + tg, "okf" + tg], w=["destf" + tg])
                yield
                sc.op("dve", lambda E, t=t: E.tensor_scalar(out=t["destf"], in0=t["destf"], scalar1=BIG, scalar2=None,
                                                            op0=ALU.add), r=["destf" + tg], w=["destf" + tg])
                yield
                sc.op("dve", lambda E, t=t, i=i: E.tensor_copy(out=idxA[:, i, :], in_=t["destf"]),
                      r=["destf" + tg], w=[f"idx{i}"])
                yield
                sc.op("dve", lambda E, t=t, i=i: E.tensor_copy(out=t["jr"][:, 0:4], in_=idxA[:, i, :]),
                      r=[f"idx{i}"], w=["jr" + tg])
                yield
                sc.op("dve", lambda E, t=t, i=i: E.tensor_copy(out=t["jr"][:, 4:8], in_=t["jr"][:, 0:4]),
                      r=["jr" + tg], w=[f"idxfence{i}"])
            if C1_LEVEL >= 3:
                bg = 2 * s4
                yield
                sc.op("pe", lambda E, t=t, bg=bg: E.transpose(out=bank(bg)[0:32, 0:128], in_=t["Gd"], identity=identF),
                      r=["Gd" + tg, "consts"], w=[f"ps{bg}"])
                yield
                sc.op("act", lambda E, t=t, bg=bg: E.activation(out=t["GT"][0:32, :], in_=bank(bg)[0:32, 0:128], func=AF.Copy),
                      r=[f"ps{bg}"], w=["GT" + tg])
                for hh in range(2):
                    b = 2 * s4 + 1 - hh
                    yield
                    sc.op("pe", lambda E, t=t, b=b, hh=hh: E.matmul(out=bank(b), lhsT=t["GT"][0:32, :],
                                                                  rhs=b2S[0:32, hh * 512:(hh + 1) * 512], start=True, stop=True),
                          r=["GT" + tg, "moec"], w=[f"ps{b}"])
                    yield
                    sc.op("dve", lambda E, i=i, hh=hh, b=b: E.tensor_tensor(
                        out=X[:, i, hh * 512:(hh + 1) * 512], in0=bank(b), in1=X[:, i, hh * 512:(hh + 1) * 512], op=ALU.add),
                        r=[f"ps{b}", f"X{i}"], w=[f"X{i}"])
            if C1_LEVEL >= 4:
                for k in range(4):
                    def scat(E, i=i, k=k, s4=s4):
                        ia = idxT[:, i, k:k + 1] if SCAT_TEST else idxA[:, i, k:k + 1]
                        return [E.indirect_dma_start(out=Xs_d[:, :], out_offset=bass.IndirectOffsetOnAxis(ap=ia, axis=0),
                                                     in_=h3b[s4], in_offset=None, bounds_check=bc_reg, oob_is_err=False)]
                    yield
                    sc.op("pool", scat, r=[f"idx{i}", f"idxfence{i}", f"h3b{s4}", "idxT"], w=[f"Xs{i}.{k}"], dma=(f"scat{s4}", 1))
                    XSK.append(f"Xs{i}.{k}")
            if C1_LEVEL >= 2:
                yield
                sc.op("dve", lambda E, t=t, i=i: E.tensor_tensor(out=gatesA[:, i, :], in0=gatesA[:, i, :], in1=t["okf"], op=ALU.mult),
                      r=[f"gate{i}", "okf" + tg, "Gd" + tg], w=[f"gate{i}"])


        gens = [c1_tile(i) for i in range(NT)]
        active = []
        nxt = 0
        while nxt < NT or active:
            while len(active) < 4 and nxt < NT:
                active.append(gens[nxt]); nxt += 1
            for g in list(active):
                try:
                    next(g)
                except StopIteration:
                    active.remove(g)

        def phase_C23():
            C1K = ["xn30", "xn31", "h3b0", "h3b1", "h3b2", "h3b3", "h3T0a", "h3T0b", "h3T1a", "h3T1b", "b1st"] + \
                [nm + f"r{j}" for j in range(4) for nm in ("lg", "max8", "idx8", "idxf", "nmax", "ex4", "sum4", "posS", "oh", "jr",
                                                           "pk0", "pk1", "pk2", "pk3", "okf", "destf", "Gd", "GT")] + \
                [f"msk{i}" for i in range(NT)]
            bar_dummy = carve(T0 + 48 * K, 64, F32)
            sc.op("pool", lambda E: E.memset(bar_dummy, 0.0), w=C1K + ["bar1"])

            def load_xs(e):
                s_ = e % 2
                sc.op("act", lambda E, e=e, s_=s_: [
                    E.dma_start(out=xstage[s_][:, 0:2, :], in_=Xs_d[e * CAP:e * CAP + 256, :].rearrange("(j p) d -> p j d", p=128)),
                    E.dma_start(out=xstage[s_][0:RT, 2, :], in_=Xs_d[e * CAP + 256:(e + 1) * CAP, :])],
                    r=XSK + ["bar1"], w=[f"xstage{s_}"], dma=(f"xstage{s_}", 2))

            def xs_transposes(e):
                s_ = e % 2
                if C2_LEVEL < 1:
                    return
                for pr in range(4):
                    bk = pr % 2
                    bv = bank(bk).bitcast(BF16)

                    def tr(E, s_=s_, pr=pr, bv=bv):
                        r = []
                        for kl in range(2):
                            kc = 2 * pr + kl
                            for j in range(3):
                                rows = 128 if j < 2 else RT
                                r.append(E.transpose(out=bv[:, kl * CAP + j * 128:kl * CAP + j * 128 + rows],
                                                     in_=xstage[s_][0:rows, j, kc * 128:(kc + 1) * 128],
                                                     identity=identB[0:rows, 0:rows]))
                        return r
                    sc.op("pe", tr, r=[f"xstage{s_}", "consts", "bar1"], w=[f"ps{bk}"])
                    for kl in range(2):
                        kc = 2 * pr + kl
                        if C2_SUB < 1 or (C2_SUB < 2 and kl == 1):
                            continue
                        sc.op("dve", lambda E, s_=s_, kc=kc, kl=kl, bv=bv: E.tensor_scalar(
                            out=XsT[s_][:, kc, :], in0=bv[:, kl * CAP:(kl + 1) * CAP], scalar1=gmoeC[:, kc:kc + 1], scalar2=None,
                            op0=ALU.mult), r=[f"ps{bk}", "gains", "bar1"], w=[f"XsT{s_}.{kc}"])

            if NE_RUN > 0:
                load_xs(0)
                load_xs(1)
                xs_transposes(0)
            YSK = []
            for e in range(NE_RUN):
                s_ = e % 2
                sg, sl, s2 = piece_slot[(e, "g")], piece_slot[(e, "l")], piece_slot[(e, "w2")]
                xk = [f"XsT{s_}.{kc}" for kc in range(8)]
                for j8 in range(8 if C2_LEVEL >= 2 else 0):
                    H = hset[j8 % 2]
                    ht = f"h{j8 % 2}"
                    bG, bL = 2 + 2 * (j8 % 2), 3 + 2 * (j8 % 2)
                    sc.op("pe", lambda E, s_=s_, sg=sg, j8=j8, bG=bG: [
                        E.matmul(out=bank(bG)[:, 0:CAP], lhsT=ring[sg][:, kc, j8 * 128:(j8 + 1) * 128], rhs=XsT[s_][:, kc, :],
                                 start=(kc == 0), stop=(kc == 7)) for kc in range(8)],
                        r=xk + [f"ring{sg}"], w=[f"ps{bG}"])
                    sc.op("pe", lambda E, s_=s_, sl=sl, j8=j8, bL=bL: [
                        E.matmul(out=bank(bL)[:, 0:CAP], lhsT=ring[sl][:, kc, j8 * 128:(j8 + 1) * 128], rhs=XsT[s_][:, kc, :],
                                 start=(kc == 0), stop=(kc == 7)) for kc in range(8)],
                        r=xk + [f"ring{sl}"], w=[f"ps{bL}"])
                    sc.op("dve", lambda E, H=H, e=e, j8=j8, bG=bG: E.tensor_scalar(
                        out=H["g"], in0=bank(bG)[:, 0:CAP], scalar1=b1T[:, j8, e:e + 1], scalar2=7.0, op0=ALU.add, op1=ALU.min),
                        r=[f"ps{bG}", "b1T", "bar1"], w=["g" + ht])
                    sc.op("act", lambda E, H=H: E.activation(out=H["sig"], in_=H["g"], func=AF.Sigmoid, scale=1.702),
                          r=["g" + ht, "bar1"], w=["sig" + ht])
                    sc.op("dve", lambda E, H=H, e=e, j8=j8, bL=bL: E.tensor_scalar(
                        out=H["l1"], in0=bank(bL)[:, 0:CAP], scalar1=b1T[:, 8 + j8, e:e + 1], scalar2=8.0, op0=ALU.add, op1=ALU.min),
                        r=[f"ps{bL}", "b1T", "bar1"], w=["l1" + ht])
                    sc.op("dve", lambda E, H=H: E.tensor_tensor(out=H["gs"], in0=H["g"], in1=H["sig"], op=ALU.mult),
                          r=["g" + ht, "sig" + ht, "bar1"], w=["gs" + ht])
                    sc.op("dve", lambda E, H=H, j8=j8: E.scalar_tensor_tensor(
                        out=actT[:, j8, :], in0=H["l1"], scalar=-6.0, in1=H["gs"], op0=ALU.max, op1=ALU.mult),
                        r=["l1" + ht, "gs" + ht, "bar1"], w=[f"actT{j8}"])
                issue_piece()
                issue_piece()
                if e + 2 < NE_RUN:
                    load_xs(e + 2)
                if e + 1 < NE_RUN:
                    xs_transposes(e + 1)
                for j in range(3 if C2_LEVEL >= 3 else 0):
                    for hh in range(2):
                        bO = 6 + (2 * j + hh) % 2
                        rows = 128 if j < 2 else RT
                        sc.op("pe", lambda E, j=j, hh=hh, bO=bO, s2=s2, rows=rows: [
                            E.matmul(out=bank(bO)[0:rows, :], lhsT=actT[:, j8, j * 128:j * 128 + rows],
                                     rhs=ring[s2][:, j8, hh * 512:(hh + 1) * 512],
                                     start=(j8 == 0), stop=(j8 == 7)) for j8 in range(8)],
                            r=[f"actT{j8}" for j8 in range(8)] + [f"ring{s2}"], w=[f"ps{bO}"])
                        if hh == 0:
                            sc.op("act", lambda E, j=j, hh=hh, bO=bO, rows=rows: E.activation(
                                out=ystage[0:rows, j, hh * 512:(hh + 1) * 512], in_=bank(bO)[0:rows, :], func=AF.Copy),
                                r=[f"ps{bO}", "bar1"], w=[f"ystage{j}.{hh}"])
                        else:
                            sc.op("dve", lambda E, j=j, hh=hh, bO=bO, rows=rows: E.tensor_copy(
                                out=ystage[0:rows, j, hh * 512:(hh + 1) * 512], in_=bank(bO)[0:rows, :]),
                                r=[f"ps{bO}", "bar1"], w=[f"ystage{j}.{hh}"])
                issue_piece()
                if C2_LEVEL < 3:
                    continue
                sc.op("sp", lambda E, e=e: [
                    E.dma_start(out=Ys_d[e * CAP:e * CAP + 256, :].rearrange("(j p) d -> p j d", p=128), in_=ystage[:, 0:2, :]),
                    E.dma_start(out=Ys_d[e * CAP + 256:(e + 1) * CAP, :], in_=ystage[0:RT, 2, :])],
                    r=[f"ystage{j}.{hh}" for j in range(3) for hh in range(2)], w=[f"Ys{e}"], dma=("ys", 2))
                YSK.append(f"Ys{e}")

            if stop_after == "C2":
                for i in range(NT):
                    sc.op("sp", lambda E, i=i: [E.dma_start(out=out[i * 128:(i + 1) * 128, :], in_=X[:, i, :])],
                          r=[f"X{i}"] + YSK, w=[f"out{i}"], dma=("out", 1))
                return
            C2K = ["xstage0", "xstage1"] + [f"XsT{j}.{kc}" for j in range(2) for kc in range(8)] + [f"actT{j8}" for j8 in range(8)] + \
                [nm + f"h{j}" for j in range(2) for nm in ("g", "sig", "l1", "gs")] + [f"ystage{j}.{hh}" for j in range(3) for hh in range(2)]
            sc.op("sp", lambda E: [E.dma_start(out=gfin, in_=g_final.partition_broadcast(128).rearrange("p o d -> p (o d)"))],
                  w=["gfin"] + C2K, dma=("gfin", 1))
            sc.op("dve", lambda E: [E.memset(grow[j][k], 0.0) for j in range(2) for k in range(4)],
                  r=["gfin"], w=[f"grow{j}.{k}" for j in range(2) for k in range(4)])
            for i in range(NT):
                s_ = i % 2
                for k in range(4):
                    def gath(E, i=i, k=k, s_=s_):
                        return [E.indirect_dma_start(out=grow[s_][k], out_offset=None, in_=Ys_d[:, :],
                                                     in_offset=bass.IndirectOffsetOnAxis(ap=idxA[:, i, k:k + 1], axis=0),
                                                     bounds_check=bc_reg, oob_is_err=False)]
                    sc.op("pool", gath, r=YSK + [f"idx{i}"], w=[f"grow{s_}.{k}"], dma=(f"gath{s_}.{k}", 1))
                for k in range(4):
                    sc.op("act", lambda E, i=i, s_=s_, k=k: E.activation(out=dg[s_][k], in_=identB, func=AF.Identity,
                                                                        scale=gatesA[:, i, k:k + 1]),
                          r=[f"gate{i}", "consts", "gfin"], w=[f"dg{s_}.{k}"])
                for hh in range(2):
                    bq = 2 * s_ + hh
                    sc.op("pe", lambda E, s_=s_, hh=hh, bq=bq: [
                        E.matmul(out=bank(bq), lhsT=dg[s_][k], rhs=grow[s_][k][:, hh * 512:(hh + 1) * 512],
                                 start=(k == 0), stop=(k == 3)) for k in range(4)],
                        r=[f"dg{s_}.{k}" for k in range(4)] + [f"grow{s_}.{k}" for k in range(4)], w=[f"ps{bq}"])
                    sc.op("dve", lambda E, i=i, s_=s_, hh=hh, bq=bq: E.tensor_tensor(
                        out=acc[s_][:, hh * 512:(hh + 1) * 512], in0=bank(bq), in1=X[:, i, hh * 512:(hh + 1) * 512], op=ALU.add),
                        r=[f"ps{bq}", f"X{i}", "gfin"], w=[f"acc{s_}"] if hh == 0 else [f"acc{s_}b"])
                sc.op("act", lambda E, i=i, s_=s_: E.activation(out=junk3, in_=acc[s_], func=AF.Square, accum_out=ssA[:, i:i + 1]),
                      r=[f"acc{s_}", f"acc{s_}b", "gfin"], w=[f"ssF{i}"])
                sc.op("act", lambda E, i=i: E.activation(out=rsA[:, i:i + 1], in_=ssA[:, i:i + 1], func=AF.Ln, scale=1.0 / D, bias=EPS),
                      r=[f"ssF{i}"], w=[f"rsF{i}"])
                sc.op("act", lambda E, i=i: E.activation(out=rsA[:, i:i + 1], in_=rsA[:, i:i + 1], func=AF.Exp, scale=-0.5),
                      r=[f"rsF{i}"], w=[f"rsF{i}"])
                sc.op("dve", lambda E, i=i, s_=s_: E.scalar_tensor_tensor(out=ostage[s_], in0=acc[s_], scalar=rsA[:, i:i + 1], in1=gfin,
                                                                          op0=ALU.mult, op1=ALU.mult),
                      r=[f"acc{s_}", f"acc{s_}b", f"rsF{i}", "gfin"], w=[f"ostage{s_}"])
                sc.op("sp", lambda E, i=i, s_=s_: [E.dma_start(out=out[i * 128:(i + 1) * 128, :], in_=ostage[s_])],
                      r=[f"ostage{s_}"], w=[f"out{i}"], dma=(f"out{s_}", 1))

        if stop_after == "C1":
            dbg = nc.dram_tensor("dbg", [128, 128], F32, kind="ExternalOutput").ap()
            dbgS = carve(T0 + 40 * K, 512, F32)
            sc.op("dve", lambda E: [E.tensor_copy(out=dbgS[:, 0:64], in_=gatesA.rearrange("p i k -> p (i k)")),
                                    E.tensor_copy(out=dbgS[:, 64:128], in_=idxA.rearrange("p i k -> p (i k)"))],
                  r=[f"gate{i}" for i in range(NT)] + [f"idx{i}" for i in range(NT)] + XSK, w=["dbgS"])
            sc.op("sp", lambda E: [E.dma_start(out=dbg, in_=dbgS)], r=["dbgS"], w=["dbgout"], dma=("dbg", 1))
            for i in range(NT):
                sc.op("sp", lambda E, i=i: [E.dma_start(out=out[i * 128:(i + 1) * 128, :], in_=X[:, i, :])],
                      r=[f"X{i}", "dbgout"], w=[f"out{i}"], dma=("out", 1))
        else:
            phase_C23()
    sc.op("sp", lambda E: E.nop(), r=[f"out{i}" for i in range(NT)], w=["done"])
    nw = sc.emit()
    print(f"[kernel] ops={len(sc.ops)} waits={nw}")


_IN_NAMES = ["x", "mem", "g_mix", "w_in", "conv_w", "g_conv_out", "g_sb_out", "w_out", "g_xattn", "g_mem",
             "w_q_mem", "w_kv_mem", "w_o_mem", "g_moe", "w_router", "b_router", "w1", "b1", "w2", "b2", "g_final"]


def kernel(**inputs):
    nc = build_program()
    a = {k: np.ascontiguousarray(np.asarray(v, dtype=np.float32)) for k, v in inputs.items()}
    shared = {}
    for k in _IN_NAMES:
        if k in ("x", "mem"):
            continue
        v = a[k]
        if k == "g_final":
            v = v.reshape(1, D)
        elif v.shape[0] == 1:
            v = v[0]
            if v.ndim == 1:
                v = v.reshape(1, -1)
        shared[k] = np.ascontiguousarray(v)
    in_maps = []
    for c in range(NCORES):
        m = dict(shared)
        m["x"] = np.ascontiguousarray(a["x"][c])
        m["mem"] = np.ascontiguousarray(a["mem"][c])
        in_maps.append(m)
    res = run_bass_kernel_spmd(nc, in_maps, core_ids=list(range(NCORES)))
    return np.stack([np.asarray(r["out"], dtype=np.float32) for r in res.results], axis=0)
```
